# Optimizing a Trainium2 kernel written in Bass

```python
import jax
import jax.numpy as jnp
from jax import lax
import numpy as np

D_MODEL = 1024
BATCH = 16
SEQ = 2048
DEPTH = 4

GRID_W = 64
CTX_LEN = 256
HG_HEADS = 4
HG_HEAD_DIM = 128
HG_WIDTH = HG_HEADS * HG_HEAD_DIM
DN_HEADS = 4
DN_HEAD_DIM = 128
DN_WIDTH = DN_HEADS * DN_HEAD_DIM
MIX_WIDTH = HG_WIDTH + DN_WIDTH
HG_CHUNK = 32
DN_CHUNK = 64
CONV_SIZE = 3
N_GROUPS = 4
EXPERTS_PER_GROUP = 4
N_EXPERTS = N_GROUPS * EXPERTS_PER_GROUP
TOP_K_IN_GROUP = 2
EXPERT_FF = 512
NORM_EPS = 1e-6
IN_SPLITS = [HG_WIDTH] * 5 + [DN_WIDTH] * 4 + [DN_HEADS] * 4
IN_COLS = sum(IN_SPLITS)

kernel_name = 'hybrid_hgrn2_gdn_hmoe_dit'


def rms_norm(x, w):
    x32 = x.astype(jnp.float32)
    y = x32 * lax.rsqrt(jnp.mean(x32 * x32, axis=-1, keepdims=True) + NORM_EPS)
    return (y * w.astype(jnp.float32)).astype(x.dtype)


def modulate(h, shift, scale):
    return h * (1 + scale) + shift


def l2_normalize(t):
    t32 = t.astype(jnp.float32)
    return (t32 * lax.rsqrt(jnp.sum(t32 * t32, axis=-1, keepdims=True) + NORM_EPS)).astype(t.dtype)


def to_heads(t, n_heads):
    b, length, width = t.shape
    return t.reshape(b, length, n_heads, width // n_heads).transpose(0, 2, 1, 3)


def gated_head_norm(o, gate, w):
    y = rms_norm(o, w) * jax.nn.silu(gate)
    b, h, length, d = y.shape
    return y.transpose(0, 2, 1, 3).reshape(b, length, h * d)


def split_in_proj(p):
    bounds = [int(v) for v in np.cumsum(IN_SPLITS)[:-1]]
    return jnp.split(p, bounds, axis=-1)


def gla_chunk_scan(q, k, v, log_f, s0):
    b, h, length, dk = q.shape
    dv = v.shape[-1]
    n = length // HG_CHUNK
    incl = jnp.tril(jnp.ones((HG_CHUNK, HG_CHUNK), dtype=bool))[:, :, None]

    def to_chunks(t):
        return jnp.moveaxis(t.reshape(b, h, n, HG_CHUNK, t.shape[-1]), 2, 0)

    def step(state, inp):
        qc, kc, vc, fc = inp
        cum = jnp.cumsum(fc, axis=2)
        diff = cum[:, :, :, None, :] - cum[:, :, None, :, :]
        decay = jnp.where(incl, jnp.exp(jnp.where(incl, diff, 0.0)), 0.0)
        attn = jnp.einsum('bhtd,bhsd,bhtsd->bhts', qc, kc, decay)
        o = jnp.einsum('bhtd,bhde->bhte', qc * jnp.exp(cum), state) + jnp.einsum('bhts,bhse->bhte', attn, vc)
        last = cum[:, :, -1, :]
        k_dec = kc * jnp.exp(last[:, :, None, :] - cum)
        state = jnp.exp(last)[..., None] * state + jnp.einsum('bhsd,bhse->bhde', k_dec, vc)
        return state, o

    state, o = lax.scan(step, s0, (to_chunks(q), to_chunks(k), to_chunks(v), to_chunks(log_f)))
    return jnp.moveaxis(o, 0, 2).reshape(b, h, length, dv), state


def gated_delta_chunk_scan(q, k, v, beta, log_a, s0):
    b, h, length, dk = q.shape
    dv = v.shape[-1]
    n = length // DN_CHUNK
    dt = q.dtype
    qn = q.reshape(b, h, n, DN_CHUNK, dk)
    kn = k.reshape(b, h, n, DN_CHUNK, dk)
    vn = v.reshape(b, h, n, DN_CHUNK, dv)
    bn = beta.reshape(b, h, n, DN_CHUNK).astype(jnp.float32)
    cum = jnp.cumsum(log_a.astype(jnp.float32).reshape(b, h, n, DN_CHUNK), axis=-1)
    incl = jnp.tril(jnp.ones((DN_CHUNK, DN_CHUNK), dtype=bool))
    strict = jnp.tril(jnp.ones((DN_CHUNK, DN_CHUNK), dtype=bool), -1)
    diff = cum[..., :, None] - cum[..., None, :]
    decay = jnp.where(incl, jnp.exp(jnp.where(incl, diff, 0.0)), 0.0)
    kk = jnp.einsum('bhntd,bhnsd->bhnts', kn, kn).astype(jnp.float32)
    a_mat = jnp.where(strict, bn[..., :, None] * kk * decay, 0.0) + jnp.eye(DN_CHUNK, dtype=jnp.float32)
    rhs = jnp.concatenate([kn.astype(jnp.float32) * (bn * jnp.exp(cum))[..., None],
                           vn.astype(jnp.float32) * bn[..., None]], axis=-1)
    sol = lax.linalg.triangular_solve(a_mat, rhs, left_side=True, lower=True, unit_diagonal=True)
    w = sol[..., :dk].astype(dt)
    u = sol[..., dk:].astype(dt)
    qk = jnp.einsum('bhntd,bhnsd->bhnts', qn, kn) * decay.astype(dt)
    q_dec = qn * jnp.exp(cum)[..., None].astype(dt)
    k_dec = kn * jnp.exp(cum[..., -1:] - cum)[..., None].astype(dt)
    g_tot = jnp.exp(cum[..., -1]).astype(dt)

    def step(state, inp):
        w_c, u_c, qk_c, qd_c, kd_c, gt_c = inp
        v_new = u_c - jnp.einsum('bhtd,bhde->bhte', w_c, state)
        o = jnp.einsum('bhtd,bhde->bhte', qd_c, state) + jnp.einsum('bhts,bhse->bhte', qk_c, v_new)
        state = gt_c[..., None, None] * state + jnp.einsum('bhtd,bhte->bhde', kd_c, v_new)
        return state, o

    xs = tuple(jnp.moveaxis(t, 2, 0) for t in (w, u, qk, q_dec, k_dec, g_tot))
    state, o = lax.scan(step, s0, xs)
    return jnp.moveaxis(o, 0, 2).reshape(b, h, length, dv), state


def run_bidirectional(scan_fn, ctx_fwd, lat_fwd, ctx_bwd, lat_bwd, s0):
    oc_f, sc_f = scan_fn(*ctx_fwd, s0)
    ox_f, _ = scan_fn(*lat_fwd, sc_f)
    oc_b, sc_b = scan_fn(*[jnp.flip(t, axis=2) for t in ctx_bwd], s0)
    ox_b, _ = scan_fn(*[jnp.flip(t, axis=2) for t in lat_bwd], sc_b)
    return oc_f + jnp.flip(oc_b, axis=2), ox_f + jnp.flip(ox_b, axis=2)


def hgrn2_log_forget(f_logit, lb):
    return jnp.logaddexp(jnp.log(lb), jnp.log1p(-lb) + jax.nn.log_sigmoid(f_logit))


def hgrn2_branch(cols_c, cols_x, lb, norm_w):
    def prep(cols):
        q, i, g, f_fwd, f_bwd = cols
        q = to_heads(jax.nn.silu(q), HG_HEADS)
        v = to_heads(i, HG_HEADS)
        per_dir = []
        for f_logit, lb_d in ((f_fwd, lb[0]), (f_bwd, lb[1])):
            log_f = to_heads(hgrn2_log_forget(f_logit, lb_d), HG_HEADS)
            per_dir.append((q, -jnp.expm1(log_f), v, log_f))
        return per_dir, to_heads(g, HG_HEADS)

    dirs_c, gc = prep(cols_c)
    dirs_x, gx = prep(cols_x)
    s0 = jnp.zeros((gc.shape[0], HG_HEADS, HG_HEAD_DIM, HG_HEAD_DIM), gc.dtype)
    oc, ox = run_bidirectional(gla_chunk_scan, dirs_c[0], dirs_x[0], dirs_c[1], dirs_x[1], s0)
    return gated_head_norm(oc, gc, norm_w), gated_head_norm(ox, gx, norm_w)


def short_conv_latent(t, w):
    b, length, ch = t.shape
    rows = length // GRID_W
    y = lax.conv_general_dilated(t.reshape(b, rows, GRID_W, ch), w[:, :, None, :], window_strides=(1, 1),
                                 padding='SAME', dimension_numbers=('NHWC', 'HWIO', 'NHWC'),
                                 feature_group_count=ch)
    return y.reshape(b, length, ch)


def short_conv_context(t, w):
    ch = t.shape[-1]
    return lax.conv_general_dilated(t, w[CONV_SIZE // 2][:, None, :], window_strides=(1,), padding='SAME',
                                    dimension_numbers=('NWC', 'WIO', 'NWC'), feature_group_count=ch)


def deltanet_branch(cols_c, cols_x, conv_w, a_log, dt_bias, norm_w):
    def prep(cols, conv_fn):
        q, k, v, z, b_fwd, b_bwd, a_fwd, a_bwd = cols
        qkv = jax.nn.silu(conv_fn(jnp.concatenate([q, k, v], axis=-1), conv_w))
        q, k, v = jnp.split(qkv, 3, axis=-1)
        q = l2_normalize(to_heads(q, DN_HEADS)) * (DN_HEAD_DIM ** -0.5)
        k = l2_normalize(to_heads(k, DN_HEADS))
        v = to_heads(v, DN_HEADS)
        per_dir = []
        for d, (b_logit, a_logit) in enumerate(((b_fwd, a_fwd), (b_bwd, a_bwd))):
            beta = jax.nn.sigmoid(b_logit).transpose(0, 2, 1)
            log_a = (-jnp.exp(a_log[d]) * jax.nn.softplus(a_logit + dt_bias[d])).transpose(0, 2, 1)
            per_dir.append((q, k, v, beta, log_a))
        return per_dir, to_heads(z, DN_HEADS)

    dirs_c, zc = prep(cols_c, short_conv_context)
    dirs_x, zx = prep(cols_x, short_conv_latent)
    s0 = jnp.zeros((zc.shape[0], DN_HEADS, DN_HEAD_DIM, DN_HEAD_DIM), zc.dtype)
    oc, ox = run_bidirectional(gated_delta_chunk_scan, dirs_c[0], dirs_x[0], dirs_c[1], dirs_x[1], s0)
    return gated_head_norm(oc, zc, norm_w), gated_head_norm(ox, zx, norm_w)


def hybrid_mixer(pc, px, lb, hg_norm_w, conv_w, a_log, dt_bias, dn_norm_w):
    cols_c = split_in_proj(pc)
    cols_x = split_in_proj(px)
    hg_c, hg_x = hgrn2_branch(cols_c[:5], cols_x[:5], lb, hg_norm_w)
    dn_c, dn_x = deltanet_branch(cols_c[5:], cols_x[5:], conv_w, a_log, dt_bias, dn_norm_w)
    return jnp.concatenate([hg_c, dn_c], axis=-1), jnp.concatenate([hg_x, dn_x], axis=-1)


def hierarchical_moe(h, w_group, b_group, w_expert, b_expert, w_gate, w_up, w_down):
    t = h.reshape(-1, h.shape[-1])
    g_logits = (t @ w_group + b_group).astype(jnp.float32)
    g_prob = jax.nn.softmax(g_logits, axis=-1)
    g_idx = jnp.argmax(g_logits, axis=-1)
    g_w = jnp.take_along_axis(g_prob, g_idx[:, None], axis=-1)
    e_logits = (t @ w_expert + b_expert).astype(jnp.float32).reshape(-1, N_GROUPS, EXPERTS_PER_GROUP)
    e_in_group = jnp.take_along_axis(e_logits, g_idx[:, None, None], axis=1)[:, 0]
    top_v, top_i = lax.top_k(e_in_group, TOP_K_IN_GROUP)
    top_w = jax.nn.softmax(top_v, axis=-1) * g_w
    expert_id = g_idx[:, None] * EXPERTS_PER_GROUP + top_i
    gates = jnp.sum(jax.nn.one_hot(expert_id, N_EXPERTS, dtype=jnp.float32) * top_w[..., None],
                    axis=1).astype(t.dtype)
    y = jnp.zeros_like(t)
    for e in range(N_EXPERTS):
        a = jax.nn.silu(t @ w_gate[e]) * (t @ w_up[e])
        y = y + gates[:, e:e + 1] * (a @ w_down[e])
    return y.reshape(h.shape)


def setup_inputs(seed: int = 0) -> dict:
    key = jax.random.key(seed)
    ks = jax.random.split(key, 24)
    f32 = jnp.float32

    def nrm(k, shape, scale):
        return jax.random.normal(k, shape, f32) * scale

    a_init = jax.random.uniform(ks[13], (DEPTH, 2, DN_HEADS), f32, minval=1.0, maxval=16.0)
    dt = jnp.exp(jax.random.uniform(ks[14], (DEPTH, 2, DN_HEADS), f32,
                                    minval=float(np.log(1e-3)), maxval=float(np.log(1e-1))))
    return {
        'x': nrm(ks[0], (BATCH, SEQ, D_MODEL), 1.0),
        'c': nrm(ks[1], (BATCH, D_MODEL), 1.0),
        'ctx': nrm(ks[2], (BATCH, CTX_LEN, D_MODEL), 1.0),
        'c_ctx': nrm(ks[3], (D_MODEL,), 1.0),
        'w_mod': nrm(ks[4], (DEPTH, D_MODEL, 6 * D_MODEL), 0.5 * D_MODEL ** -0.5),
        'b_mod': nrm(ks[5], (DEPTH, 6 * D_MODEL), 0.02),
        'norm1_w': 1.0 + nrm(ks[6], (DEPTH, D_MODEL), 0.02),
        'norm2_w': 1.0 + nrm(ks[7], (DEPTH, D_MODEL), 0.02),
        'w_in': nrm(ks[8], (DEPTH, D_MODEL, IN_COLS), D_MODEL ** -0.5),
        'w_out': nrm(ks[9], (DEPTH, MIX_WIDTH, D_MODEL), MIX_WIDTH ** -0.5),
        'hg_lb': nrm(ks[10], (DEPTH, 2, HG_WIDTH), 0.1),
        'hg_norm_w': 1.0 + nrm(ks[11], (DEPTH, HG_HEAD_DIM), 0.02),
        'dn_conv_w': nrm(ks[12], (DEPTH, CONV_SIZE, CONV_SIZE, 3 * DN_WIDTH), 1.0 / CONV_SIZE),
        'dn_a_log': jnp.log(a_init),
        'dn_dt_bias': dt + jnp.log(-jnp.expm1(-dt)),
        'dn_norm_w': 1.0 + nrm(ks[15], (DEPTH, DN_HEAD_DIM), 0.02),
        'w_group': nrm(ks[16], (DEPTH, D_MODEL, N_GROUPS), D_MODEL ** -0.5),
        'b_group': nrm(ks[17], (DEPTH, N_GROUPS), 0.01),
        'w_expert': nrm(ks[18], (DEPTH, D_MODEL, N_EXPERTS), D_MODEL ** -0.5),
        'b_expert': nrm(ks[19], (DEPTH, N_EXPERTS), 0.01),
        'w_gate': nrm(ks[20], (DEPTH, N_EXPERTS, D_MODEL, EXPERT_FF), D_MODEL ** -0.5),
        'w_up': nrm(ks[21], (DEPTH, N_EXPERTS, D_MODEL, EXPERT_FF), D_MODEL ** -0.5),
        'w_down': nrm(ks[22], (DEPTH, N_EXPERTS, EXPERT_FF, D_MODEL), EXPERT_FF ** -0.5),
        'final_norm_w': 1.0 + nrm(ks[23], (D_MODEL,), 0.02),
    }


def reference(x, c, ctx, c_ctx, w_mod, b_mod, norm1_w, norm2_w, w_in, w_out, hg_lb, hg_norm_w,
              dn_conv_w, dn_a_log, dn_dt_bias, dn_norm_w, w_group, b_group, w_expert, b_expert,
              w_gate, w_up, w_down, final_norm_w):
    lb_all = jnp.cumsum(jax.nn.softmax(hg_lb.astype(jnp.float32), axis=0), axis=0)
    lb_all = (lb_all - lb_all[0]).astype(x.dtype)
    silu_c = jax.nn.silu(c)
    silu_c_ctx = jax.nn.silu(c_ctx)
    xs, cs = x, ctx
    for l in range(DEPTH):
        last = l == DEPTH - 1
        sh1x, sc1x, g1x, sh2x, sc2x, g2x = jnp.split((silu_c @ w_mod[l] + b_mod[l])[:, None, :], 6, axis=-1)
        sh1c, sc1c, g1c, sh2c, sc2c, g2c = jnp.split((silu_c_ctx @ w_mod[l] + b_mod[l])[None, None, :], 6, axis=-1)
        hx = modulate(rms_norm(xs, norm1_w[l]), sh1x, sc1x)
        hc = modulate(rms_norm(cs, norm1_w[l]), sh1c, sc1c)
        oc, ox = hybrid_mixer(hc @ w_in[l], hx @ w_in[l], lb_all[l], hg_norm_w[l], dn_conv_w[l],
                              dn_a_log[l], dn_dt_bias[l], dn_norm_w[l])
        moe_args = (w_group[l], b_group[l], w_expert[l], b_expert[l], w_gate[l], w_up[l], w_down[l])
        xs = xs + g1x * (ox @ w_out[l])
        xs = xs + g2x * hierarchical_moe(modulate(rms_norm(xs, norm2_w[l]), sh2x, sc2x), *moe_args)
        if not last:
            cs = cs + g1c * (oc @ w_out[l])
            cs = cs + g2c * hierarchical_moe(modulate(rms_norm(cs, norm2_w[l]), sh2c, sc2c), *moe_args)
    return rms_norm(xs, final_norm_w)
```

```python
import numpy as np
from contextlib import ExitStack
import concourse.bass as bass
import concourse.mybir as mybir
from concourse.bass_utils import run_bass_kernel_spmd

F32 = mybir.dt.float32
BF16 = mybir.dt.bfloat16
ALU = mybir.AluOpType
AF = mybir.ActivationFunctionType
AX = mybir.AxisListType

D = 1024
KD = 8
DEPTH = 4
NB = 2
TC = 256
TL = 2048
T = TC + TL
NT = T // 128
GRID_W = 64
IN_COLS = 4624
NGRP = 37
FF = 512
NE = 16
EPS = 1e-6
HG_Q, HG_I, HG_G, HG_FF, HG_FB = 0, 512, 1024, 1536, 2048
DN_Q, DN_K, DN_V, DN_Z = 2560, 3072, 3584, 4096
SMALL0 = 4608
C_ID, C_ONES, C_INCF, C_STRF, C_INCB, C_STRB, C_NEGF, C_NEGB, C_GTF, C_GTB, C_INCF32, C_INCB32 = range(12)
NCONST = 12 * 128
TTILES = [(0, 256), (256, 512), (768, 512), (1280, 512), (1792, 512)]


class Dep:
    __slots__ = ("name", "lw", "rd", "sem", "semv", "excl")

    def __init__(self, name, excl=False):
        self.name = name
        self.excl = excl
        self.lw = None
        self.rd = {}
        self.sem = None
        self.semv = 0


class Sched:
    ENGS = ("pe", "dve", "act", "pool", "sp")

    def __init__(self, nc, stack):
        self.nc = nc
        self.stack = stack
        self.q = {e: [] for e in self.ENGS}
        self.cnt = {e: 0 for e in self.ENGS}
        self.seen = {}
        for e in self.ENGS:
            self.seen[e] = {}
            self.seen["dmaq_" + e] = {}
        self.esem = {e: stack.enter_context(nc.semaphore("prog_" + e)) for e in self.ENGS}
        self.dsems = []
        self.free_sems = []
        self.nsem_alloc = 0
        self.ninst = 0
        self.uid = 0

    def sb(self, name, shape, dt, stack=None):
        self.uid += 1
        nm = f"{name}_{self.uid}"
        t = (stack or self.stack).enter_context(self.nc.sbuf_tensor(nm, list(shape), dt))
        return t, Dep(nm)

    def ps(self, name, shape, dt, stack=None):
        self.uid += 1
        nm = f"{name}_{self.uid}"
        t = (stack or self.stack).enter_context(self.nc.psum_tensor(nm, list(shape), dt))
        return t, Dep(nm)

    def _dsem(self, d):
        if d.sem is None:
            if self.free_sems:
                d.sem, d.semv = self.free_sems.pop()
            else:
                self.nsem_alloc += 1
                d.sem = self.stack.enter_context(self.nc.semaphore("dsem%d" % self.nsem_alloc))
                d.semv = 0
            self.dsems.append(d)
        return d.sem

    def _need(self, eng, ev, waits, is_dma, raw=False):
        if ev is None:
            return
        key, val, sem = ev
        if key == eng and not is_dma and not (raw and eng != "pe"):
            return
        k2 = ("dmaq_" + eng) if is_dma else eng
        if self.seen[k2].get(key, 0) >= val:
            return
        self.seen[k2][key] = val
        if not is_dma:
            pass
        waits.append((sem, val))

    def _collect(self, eng, reads, writes, is_dma=False):
        waits = []
        for d in reads:
            self._need(eng, d.lw, waits, is_dma, raw=True)
        for d in writes:
            self._need(eng, d.lw, waits, is_dma)
            for ev in d.rd.values():
                self._need(eng, ev, waits, is_dma)
        return waits

    def _mark(self, ev, reads, writes):
        for d in reads:
            old = d.rd.get(ev[0])
            if old is None or old[1] < ev[1]:
                d.rd[ev[0]] = ev
        for d in writes:
            d.lw = ev
            d.rd = {}

    def op(self, eng, fn, reads=(), writes=()):
        if any(d.excl for d in reads):
            writes = list(writes) + [d for d in reads if d.excl]
            reads = [d for d in reads if not d.excl]
        waits = self._collect(eng, reads, writes)
        self.cnt[eng] += 1
        ev = (eng, self.cnt[eng], self.esem[eng])
        self._mark(ev, reads, writes)
        self.q[eng].append((waits, fn, self.esem[eng], 1))
        self.ninst += 1

    def dma(self, eng, out, in_, reads=(), writes=(), **kw):
        waits = self._collect(eng, reads, writes, True)
        anchor = (list(writes) + list(reads))[0]
        sem = self._dsem(anchor)
        anchor.semv += 16
        ev = ("dma_%d" % id(sem), anchor.semv, sem)
        self._mark(ev, reads, writes)
        self.q[eng].append((waits, lambda e: e.dma_start(out=out, in_=in_, **kw), sem, 16))
        self.ninst += 1

    def barrier(self):
        for e in self.ENGS:
            waits = []
            for o in self.ENGS:
                if o != e and self.cnt[o] > self.seen[e].get(o, 0):
                    self.seen[e][o] = self.cnt[o]
                    waits.append((self.esem[o], self.cnt[o]))
            for d in self.dsems:
                key = "dma_%d" % id(d.sem)
                if d.semv > self.seen[e].get(key, 0):
                    self.seen[e][key] = d.semv
                    waits.append((d.sem, d.semv))
            for k, v in self.seen[e].items():
                if self.seen["dmaq_" + e].get(k, 0) < v:
                    self.seen["dmaq_" + e][k] = v
            self.q[e].append((waits, None, None, 0))
        for d in self.dsems:
            self.free_sems.append((d.sem, d.semv))
            d.sem = None
        self.dsems = []

    def build(self):
        nc = self.nc
        with nc.Block() as block:
            def run(eng_name):
                def body(e):
                    for waits, fn, sem, inc in self.q[eng_name]:
                        for (s, v) in waits:
                            e.wait_ge(s, v)
                        if fn is not None:
                            fn(e).then_inc(sem, inc)
                return body
            block.tensor(run("pe"))
            block.vector(run("dve"))
            block.scalar(run("act"))
            block.gpsimd(run("pool"))
            block.sync(run("sp"))
        self.q = {e: [] for e in self.ENGS}


class Ring:
    def __init__(self, S, kind, name, shape, dt, n, stack):
        mk = S.sb if kind == "sb" else S.ps
        self.items = [mk(f"{name}{i}", shape, dt, stack) for i in range(n)]
        self.i = 0

    def next(self):
        it = self.items[self.i % len(self.items)]
        self.i += 1
        return it


class PsRing:
    def __init__(self, S, name, nbanks, width, dt, stack):
        per = (2048 // (4 if dt == F32 else 2))
        banks = []
        for b in range(nbanks):
            t, _ = S.ps(f"{name}{b}", [128, per], dt, stack)
            banks.append((t, Dep(f"{name}{b}", excl=True)))
        self.items = []
        for j in range(per // width):
            for (t, dep) in banks:
                self.items.append((t[:, j * width:(j + 1) * width], dep))
        self.i = 0

    def next(self):
        it = self.items[self.i % len(self.items)]
        self.i += 1
        return it


def roundrobin(gens):
    gens = list(gens)
    while gens:
        for g in list(gens):
            try:
                next(g)
            except StopIteration:
                gens.remove(g)


def build_program(nlayers=DEPTH, dbg=(), stop=None, nb_run=NB):
    nc = bass.Bass("TRN2", target_bir_lowering=False)

    def din(name, shape):
        return nc.dram_tensor(name, list(shape), F32, kind="ExternalInput").ap()

    xin = din("xin", [NB, T, D])
    cvec_d = din("cvec", [128, KD, 3])
    consts_d = din("consts", [128, NCONST])
    wmod_d = din("w_mod", [DEPTH, D, 6 * D])
    bmod_d = din("b_mod", [DEPTH, 6 * D])
    normw_d = din("normw", [128, DEPTH * 2 * KD])
    lb_d = din("lbh", [128, DEPTH * 8])
    headnw_d = din("headnw", [128, DEPTH * 2])
    convw_d = din("convw", [128, DEPTH * 12 * 9])
    alog_d = din("alog", [128, DEPTH * 16])
    wr_d = din("wr", [DEPTH, 128, KD, 20])
    br_d = din("br", [DEPTH, 20])
    fnw_d = din("fnw", [128, D])
    win_d = din("win", [DEPTH, NGRP, 128, KD, 128])
    wout_d = din("w_out", [DEPTH, D, D])
    wg_d = din("w_gate", [DEPTH, NE, D, FF])
    wu_d = din("w_up", [DEPTH, NE, D, FF])
    wd_d = din("w_down", [DEPTH, NE, FF, D])
    out_d = nc.dram_tensor("out", [NB, TL, D], F32, kind="ExternalOutput").ap()
    resA = nc.dram_tensor("resA", [NB, T, D], F32, kind="Internal").ap()
    resB = nc.dram_tensor("resB", [NB, T, D], F32, kind="Internal").ap()
    dbg_out = {}
    for name, shape in dbg:
        dbg_out[name] = nc.dram_tensor(name, list(shape), F32, kind="ExternalOutput").ap()

    with ExitStack() as top:
        S = Sched(nc, top)

        def mm(out, lhsT, rhs, st, sp, R, W):
            S.op("pe", lambda e: e.matmul(out, lhsT=lhsT, rhs=rhs, start=st, stop=sp), R, W)

        def tr(out, in_, ident, R, W):
            S.op("pe", lambda e: e.transpose(out, in_, ident), R, W)

        def act(out, in_, func, R, W, **kw):
            S.op("act", lambda e: e.activation(out, in_, func, **kw), R, W)

        def tt(eng, out, a, b, op, R, W):
            S.op(eng, lambda e: e.tensor_tensor(out, a, b, op), R, W)

        def ts1(eng, out, a, s, op, R, W):
            S.op(eng, lambda e: e.tensor_single_scalar(out, a, s, op), R, W)

        def ts2(eng, out, a, s1, s2, op0, op1, R, W):
            S.op(eng, lambda e: e.tensor_scalar(out, a, s1, s2, op0, op1), R, W)

        def stt(eng, out, a, s, b, op0, op1, R, W):
            S.op(eng, lambda e: e.scalar_tensor_tensor(out, a, s, b, op0, op1), R, W)

        def cp(eng, out, a, R, W):
            if eng == "act":
                S.op("act", lambda e: e.copy(out, a), R, W)
            else:
                S.op(eng, lambda e: e.tensor_copy(out, a), R, W)

        def dump(name, ap_sb, DEP, dram_ap=None):
            if name in dbg_out:
                dd = Dep("dbg_" + name)
                S.dma("sp", dram_ap if dram_ap is not None else dbg_out[name], ap_sb, reads=[DEP], writes=[dd])

        cst, CST = S.sb("cst", [128, NCONST], F32)
        S.dma("sp", cst[:], consts_d, writes=[CST])

        def C(i):
            return cst[:, i * 128:(i + 1) * 128]

        idb, IDB = S.sb("idb", [128, 128], BF16)
        onb, ONB = S.sb("onb", [128, 128], BF16)
        cp("dve", idb[:], C(C_ID), [CST], [IDB])
        cp("dve", onb[:], C(C_ONES), [CST], [ONB])
        epsc, EPSC = S.sb("epsc", [128, 1], F32)
        S.op("dve", lambda e: e.memset(epsc[:], EPS), (), [EPSC])
        normw, NORMW = S.sb("normw", [128, DEPTH, 2, KD], F32)
        S.dma("sp", normw[:].rearrange("p a b c -> p (a b c)"), normw_d, writes=[NORMW])
        headnw, HEADNW = S.sb("headnw", [128, DEPTH, 2], F32)
        S.dma("sp", headnw[:].rearrange("p a b -> p (a b)"), headnw_d, writes=[HEADNW])
        convw, CONVW = S.sb("convw", [128, DEPTH, 12, 9], F32)
        S.dma("sp", convw[:].rearrange("p a b c -> p (a b c)"), convw_d, writes=[CONVW])
        alog, ALOG = S.sb("alog", [128, DEPTH, 16], F32)
        S.dma("sp", alog[:].rearrange("p a b -> p (a b)"), alog_d, writes=[ALOG])
        act(alog[:, :, 0:8], alog[:, :, 0:8], AF.Exp, [ALOG], [ALOG])
        ts1("dve", alog[:, :, 0:8], alog[:, :, 0:8], -1.0, ALU.mult, [ALOG], [ALOG])
        scT, SCT = S.sb("scT", [128, KD, 3], F32)
        S.dma("sp", scT[:].rearrange("p a b -> p (a b)"), cvec_d.rearrange("p a b -> p (a b)"), writes=[SCT])
        act(scT[:], scT[:], AF.Silu, [SCT], [SCT])
        lbt, LBT = S.sb("lbt", [128, DEPTH, 8], F32)
        oml, OML = S.sb("oml", [128, DEPTH, 8], F32)
        with ExitStack() as ph:
            raw, RAWL = S.sb("lbraw", [128, DEPTH, 8], F32, ph)
            m8, M8 = S.sb("lbm", [128, 8], F32, ph)
            S.dma("sp", raw[:].rearrange("p a b -> p (a b)"), lb_d, writes=[RAWL])
            tt("dve", m8[:], raw[:, 0, :], raw[:, 1, :], ALU.max, [RAWL], [M8])
            tt("dve", m8[:], m8[:], raw[:, 2, :], ALU.max, [RAWL, M8], [M8])
            tt("dve", m8[:], m8[:], raw[:, 3, :], ALU.max, [RAWL, M8], [M8])
            for l in range(DEPTH):
                tt("dve", raw[:, l, :], raw[:, l, :], m8[:], ALU.subtract, [RAWL, M8], [RAWL])
            act(raw[:], raw[:], AF.Exp, [RAWL], [RAWL])
            tt("dve", m8[:], raw[:, 0, :], raw[:, 1, :], ALU.add, [RAWL], [M8])
            tt("dve", m8[:], m8[:], raw[:, 2, :], ALU.add, [RAWL, M8], [M8])
            tt("dve", m8[:], m8[:], raw[:, 3, :], ALU.add, [RAWL, M8], [M8])
            S.op("dve", lambda e: e.reciprocal(m8[:], m8[:]), [M8], [M8])
            for l in range(DEPTH):
                tt("dve", raw[:, l, :], raw[:, l, :], m8[:], ALU.mult, [RAWL, M8], [RAWL])
            S.op("dve", lambda e: e.memset(lbt[:, 0, :], 0.0), (), [LBT])
            cp("dve", lbt[:, 1, :], raw[:, 1, :], [RAWL], [LBT])
            tt("dve", lbt[:, 2, :], lbt[:, 1, :], raw[:, 2, :], ALU.add, [RAWL, LBT], [LBT])
            tt("dve", lbt[:, 3, :], lbt[:, 2, :], raw[:, 3, :], ALU.add, [RAWL, LBT], [LBT])
            ts2("dve", oml[:], lbt[:], -1.0, 1.0, ALU.mult, ALU.add, [LBT], [OML])
            S.barrier()
            S.build()

        class NS:
            pass

        class _Stop(Exception):
            pass

        STOPPED = [False]

        def chk(tag):
            if stop == tag and not STOPPED[0]:
                S.barrier()
                S.build()
                STOPPED[0] = True
            return STOPPED[0]

        ORDER = {0: list(range(NT)), 1: [1, 0] + list(range(NT - 1, 1, -1))}

        def dmask(d):
            return (C(C_INCF), C(C_STRF), C(C_NEGF), C(C_GTF)) if d == 0 else \
                   (C(C_INCB), C(C_STRB), C(C_NEGB), C(C_GTB))

        def gbcast(ph, L, r, w, psr):
            G, GD = S.sb("G", [128, D], F32, ph)
            lr = Ring(S, "sb", "gl", [128, 128], F32, 2, ph)
            base = (2 + 3 * w) * 8
            for hf in range(2):
                ps, PS = psr.next()
                for k4 in range(4):
                    k = hf * 4 + k4
                    lh, LH = lr.next()
                    cp("dve", lh[:], L.modT[:, base + k, r:r + 1].to_broadcast([128, 128]), [L.MODT], [LH])
                    mm(ps[:, k4 * 128:(k4 + 1) * 128], lh[:], C(C_ID), True, True, [LH, CST], [PS])
                cp("act", G[:, hf * 512:(hf + 1) * 512], ps[:, :], [PS], [GD])
            return G, GD

        def norm_phase(L, b, src, SRC, wi, hT, HT, tstart):
            with ExitStack() as ph:
                xr = Ring(S, "sb", "xt", [128, D], F32, 3, ph)
                xnr = Ring(S, "sb", "xn", [128, D], F32, 2, ph)
                junk, JUNK = S.sb("junk", [128, D], BF16, ph)
                stt_r = Ring(S, "sb", "st", [128, 4], F32, 4, ph)
                ptr = PsRing(S, "ptr", 4, 512, F32, ph)
                tmpr = Ring(S, "sb", "mt", [128, 4, 128], F32, 3, ph)
                for i in range(tstart, NT):
                    x, X = xr.next()
                    S.dma("sp", x[:], src[b, i * 128:(i + 1) * 128, :], reads=[SRC], writes=[X])
                    st, ST = stt_r.next()
                    act(junk[:], x[:], AF.Square, [X], [JUNK, ST], accum_out=st[:, 0:1])
                    act(st[:, 1:2], st[:, 0:1], AF.Sqrt, [ST, EPSC], [ST], scale=1.0 / D, bias=epsc[:, 0:1])
                    S.op("dve", lambda e, st=st: e.reciprocal(st[:, 2:3], st[:, 1:2]), [ST], [ST])
                    xn, XN = xnr.next()
                    ts1("pool", xn[:], x[:], st[:, 2:3], ALU.mult, [X, ST], [XN])
                    r = 2 if i < 2 else b
                    for half in range(2):
                        pp, PP = ptr.next()
                        for k4 in range(4):
                            k = half * 4 + k4
                            tr(pp[:, k4 * 128:(k4 + 1) * 128], xn[:, k * 128:(k + 1) * 128], C(C_ID), [XN, CST], [PP])
                        m, M = tmpr.next()
                        A = L.abc[:, 2 * wi, half * 4:half * 4 + 4, r:r + 1].to_broadcast([128, 4, 128])
                        B = L.abc[:, 2 * wi + 1, half * 4:half * 4 + 4, r:r + 1].to_broadcast([128, 4, 128])
                        tt("dve", m[:], pp.rearrange("p (a b) -> p a b", b=128), A, ALU.mult, [PP, L.ABC], [M])
                        tt("pool", hT[:, half * 4:half * 4 + 4, i * 128:(i + 1) * 128], m[:], B, ALU.add,
                           [M, L.ABC], [HT])
                S.barrier()
                S.build()

        def inproj(L, hT, HT, wring, pin, g, evac):
            wb, WB = wring.next()
            S.dma("pool", wb[:], win_d[L.l, g], writes=[WB])
            for (s, n) in TTILES:
                ps, PS = pin.next()
                for k in range(KD):
                    mm(ps[:, 0:n], wb[:, k, :], hT[:, k, s:s + n], k == 0, k == KD - 1, [WB, HT], [PS])
                evac(ps, PS, s, n)

        def conv(eng, raw, RAW, acc, ACC, wcol):
            rl = raw[:, TC:T].rearrange("p (r c) -> p r c", c=GRID_W)
            al = acc[:, TC:T].rearrange("p (r c) -> p r c", c=GRID_W)
            ts1(eng, acc[:, TC:T], raw[:, TC:T], wcol(4), ALU.mult, [RAW, CONVW], [ACC])
            for a in range(3):
                for b3 in range(3):
                    if a == 1 and b3 == 1:
                        continue
                    dr, dc = a - 1, b3 - 1
                    r0, r1 = max(0, -dr), 32 - max(0, dr)
                    c0, c1 = max(0, -dc), GRID_W - max(0, dc)
                    stt(eng, al[:, r0:r1, c0:c1], rl[:, r0 + dr:r1 + dr, c0 + dc:c1 + dc], wcol(a * 3 + b3),
                        al[:, r0:r1, c0:c1], ALU.mult, ALU.add, [RAW, ACC, CONVW], [ACC])
            ts1(eng, acc[:, 0:TC], raw[:, 0:TC], wcol(4), ALU.mult, [RAW, CONVW], [ACC])
            stt(eng, acc[:, 1:TC], raw[:, 0:TC - 1], wcol(3), acc[:, 1:TC], ALU.mult, ALU.add, [RAW, ACC, CONVW], [ACC])
            stt(eng, acc[:, 0:TC - 1], raw[:, 1:TC], wcol(5), acc[:, 0:TC - 1], ALU.mult, ALU.add,
                [RAW, ACC, CONVW], [ACC])

        def headnorm(ph, L, oacc, OACC, ZS, ZSD, which, hp, yT, YT, pss):
            sqr = Ring(S, "sb", "hsq", [128, 512], BF16, 2, ph)
            rtr = Ring(S, "sb", "hrt", [128, 512], F32, 2, ph)
            tfr = Ring(S, "sb", "htf", [128, 512], F32, 2, ph)
            for hh in range(2):
                for (s, n) in TTILES:
                    sq, SQ = sqr.next()
                    act(sq[:, 0:n], oacc[:, hh, s:s + n], AF.Square, [OACC], [SQ])
                    ps, PS = pss.next()
                    mm(ps[:, 0:n], onb[:], sq[:, 0:n], True, True, [ONB, SQ], [PS])
                    rt, RT = rtr.next()
                    act(rt[:, 0:n], ps[:, 0:n], AF.Sqrt, [PS, EPSC], [RT], scale=1.0 / 128, bias=epsc[:, 0:1])
                    S.op("dve", lambda e, rt=rt, n=n: e.reciprocal(rt[:, 0:n], rt[:, 0:n]), [RT], [RT])
                    tf, TF = tfr.next()
                    stt("dve", tf[:, 0:n], oacc[:, hh, s:s + n], headnw[:, L.l, which:which + 1], rt[:, 0:n],
                        ALU.mult, ALU.mult, [OACC, HEADNW, RT], [TF])
                    tt("pool", yT[:, which * 4 + 2 * hp + hh, s:s + n], tf[:, 0:n], ZS[:, hh, s:s + n], ALU.mult,
                       [TF, ZSD], [YT])

        def small_cols(L, b, hT, HT, bp):
            P_ = NS()
            P_.beta, P_.BETA = S.sb("beta", [128, NT, 8], F32, bp)
            P_.la, P_.LA = S.sb("la", [128, NT, 8], F32, bp)
            P_.ecols, P_.ECOLS = S.sb("ecols", [128, NT, 16], F32, bp)
            P_.er, P_.ER = S.sb("er", [128, NT, 2, 8], F32, bp)
            with ExitStack() as ph:
                wb, WB = S.sb("wbs", [128, KD, 128], BF16, ph)
                S.dma("pool", wb[:], win_d[L.l, NGRP - 1], writes=[WB])
                ba, BA = S.sb("ba", [128, NT, 16], F32, ph)
                pr = PsRing(S, "pba", 2, 16, F32, ph)
                for i in range(NT):
                    ps, PS = pr.next()
                    for k in range(KD):
                        mm(ps, hT[:, k, i * 128:(i + 1) * 128], wb[:, k, 0:16], k == 0, k == KD - 1, [HT, WB], [PS])
                    cp("dve", ba[:, i, :], ps, [PS], [BA])
                act(P_.beta[:], ba[:, :, 0:8], AF.Sigmoid, [BA], [P_.BETA])
                tt("dve", P_.la[:], ba[:, :, 8:16], alog[:, L.l, 8:16].unsqueeze(1).to_broadcast([128, NT, 8]), ALU.add,
                   [BA, ALOG], [P_.LA])
                act(P_.la[:], P_.la[:], AF.Exp, [P_.LA], [P_.LA])
                act(P_.la[:], P_.la[:], AF.Ln, [P_.LA], [P_.LA], bias=1.0)
                tt("dve", P_.la[:], P_.la[:], alog[:, L.l, 0:8].unsqueeze(1).to_broadcast([128, NT, 8]), ALU.mult,
                   [P_.LA, ALOG], [P_.LA])
                pc = PsRing(S, "pcol", 2, 16, F32, ph)
                for i in range(NT):
                    ps, PS = pc.next()
                    for d in range(2):
                        INC, STR, NEG, GT = dmask(d)
                        STRO = dmask(1 - d)[1]
                        mm(ps[:, d * 4:d * 4 + 4], INC, P_.la[:, i, d * 4:d * 4 + 4], True, True, [CST, P_.LA], [PS])
                        mm(ps[:, 8 + d * 4:8 + d * 4 + 4], STRO, P_.la[:, i, d * 4:d * 4 + 4], True, True,
                           [CST, P_.LA], [PS])
                    act(P_.ecols[:, i, :], ps, AF.Exp, [PS], [P_.ECOLS])
                ts1("dve", P_.ecols[:, :, 0:8], P_.ecols[:, :, 0:8], -1.0, ALU.mult, [P_.ECOLS], [P_.ECOLS])
                ts1("dve", P_.er[:, :, 0, :], P_.ecols[:, :, 8:16], cst[:, C_INCF * 128 + 63:C_INCF * 128 + 64], ALU.mult,
                    [P_.ECOLS, CST], [P_.ER])
                ts1("dve", P_.er[:, :, 1, :], P_.ecols[:, :, 8:16], cst[:, C_INCB * 128 + 64:C_INCB * 128 + 65], ALU.mult,
                    [P_.ECOLS, CST], [P_.ER])
                S.barrier()
                S.build()
            return P_

        def gdn_headpair(L, b, hp, hT, HT, yT, YT, SC):
            l = L.l
            with ExitStack() as ph:
                QT, QTD = S.sb("QT", [128, 2, T], BF16, ph)
                KT, KTD = S.sb("KT", [128, 2, T], BF16, ph)
                VT, VTD = S.sb("VT", [128, 2, T], BF16, ph)
                ZS, ZSD = S.sb("ZS", [128, 2, T], BF16, ph)
                oacc, OACC = S.sb("oacc", [128, 2, T], F32, ph)
                S.op("pool", lambda e: e.memset(oacc[:], 0.0), (), [OACC])
                with ExitStack() as ph2:
                    rawr = Ring(S, "sb", "raw", [128, T], F32, 1, ph2)
                    caccr = Ring(S, "sb", "cacc", [128, T], F32, 1, ph2)
                    sq32, SQ32 = S.sb("sq32", [128, T], F32, ph2)
                    sqb, SQB = S.sb("sqb", [128, T], BF16, ph2)
                    rt, RT = S.sb("rt", [128, T], F32, ph2)
                    wring = Ring(S, "sb", "wb", [128, KD, 128], BF16, 3, ph2)
                    pin = PsRing(S, "pin", 3, 512, F32, ph2)
                    pss = PsRing(S, "pss", 2, 512, F32, ph2)
                    gi = 0
                    for kind, colbase in (("z", DN_Z), ("v", DN_V), ("q", DN_Q), ("k", DN_K)):
                        for hh in range(2):
                            h = 2 * hp + hh
                            g = colbase // 128 + h
                            if kind == "z":
                                inproj(L, hT, HT, wring, pin, g,
                                       lambda ps, PS, s, n, hh=hh: act(ZS[:, hh, s:s + n], ps[:, 0:n], AF.Silu, [PS], [ZSD]))
                                continue
                            raw, RAW = rawr.next()
                            cacc, CACC = caccr.next()
                            inproj(L, hT, HT, wring, pin, g,
                                   lambda ps, PS, s, n, raw=raw, RAW=RAW: cp("act", raw[:, s:s + n], ps[:, 0:n], [PS], [RAW]))
                            cg = {"q": 0, "k": 1, "v": 2}[kind] * 4 + h
                            conv("dve", raw, RAW, cacc, CACC,
                                 lambda tap, cg=cg: convw[:, l, cg, tap:tap + 1])
                            gi += 1
                            if kind == "v":
                                act(VT[:, hh, :], cacc[:], AF.Silu, [CACC], [VTD])
                                continue
                            act(sq32[:], cacc[:], AF.Silu, [CACC], [SQ32])
                            tt("pool", sqb[:], sq32[:], sq32[:], ALU.mult, [SQ32], [SQB])
                            for (s, n) in TTILES:
                                ps, PS = pss.next()
                                mm(ps[:, 0:n], onb[:], sqb[:, s:s + n], True, True, [ONB, SQB], [PS])
                                act(rt[:, s:s + n], ps[:, 0:n], AF.Sqrt, [PS, EPSC], [RT], bias=epsc[:, 0:1])
                            S.op("dve", lambda e: e.reciprocal(rt[:], rt[:]), [RT], [RT])
                            if kind == "q":
                                stt("dve", QT[:, hh, :], sq32[:], float(128 ** -0.5), rt[:], ALU.mult, ALU.mult,
                                    [SQ32, RT], [QTD])
                            else:
                                tt("dve", KT[:, hh, :], sq32[:], rt[:], ALU.mult, [SQ32, RT], [KTD])
                    if stop == "G1" and hp == 0 and b == 0 and l == 0:
                        for j, (tt_, TD_) in enumerate(((QT, QTD), (KT, KTD), (VT, VTD), (ZS, ZSD))):
                            for hh_ in range(2):
                                cp("act", yT[:, j * 2 + hh_, :], tt_[:, hh_, :], [TD_], [YT])
                        S.dma("pool", dbg_out["yT"].rearrange("p (k t) -> p k t", k=KD), yT[:], reads=[YT], writes=[Dep("dbg_yT1")])
                    S.barrier()
                    S.build()
                    if chk("G1"):
                        return
                with ExitStack() as ph3:
                    chains = [(hh, d) for hh in range(2) for d in range(2)]
                    pab = [S.ps(f"pa{j}", [128, 512], F32, ph3)[0] for j in range(4)]
                    pabd = [Dep(f"pa{j}", excl=True) for j in range(4)]
                    pbb = S.ps("pb16", [128, 1024], BF16, ph3)[0]
                    pbbd = Dep("pb16", excl=True)
                    psb = [S.ps(f"pst{j}", [128, 512], F32, ph3)[0] for j in range(2)]
                    psbd = [Dep(f"pst{j}", excl=True) for j in range(2)]

                    class Cyc:
                        def __init__(self, items):
                            self.items = items
                            self.i = 0

                        def next(self):
                            it = self.items[self.i % len(self.items)]
                            self.i += 1
                            return it
                    CH = []
                    for c in range(4):
                        o = NS()
                        o.pa = Cyc([(pab[j][:, c * 128:(c + 1) * 128], pabd[j]) for j in range(4)])
                        o.pb16 = Cyc([(pbb[:, (2 * c + j) * 128:(2 * c + j + 1) * 128], pbbd) for j in range(2)])
                        o.pstep = Cyc([(psb[0][:, c * 128:(c + 1) * 128], psbd[0])])
                        o.pout = Cyc([(psb[1][:, c * 128:(c + 1) * 128], psbd[1])])
                        o.Sf, o.SF = S.sb("Sf", [128, 128], F32, ph3)
                        o.Sb, o.SB = S.sb("Sb", [128, 128], BF16, ph3)
                        S.op("dve", lambda e, o=o: e.memset(o.Sf[:], 0.0), (), [o.SF])
                        S.op("dve", lambda e, o=o: e.memset(o.Sb[:], 0.0), (), [o.SB])
                        for nm, dt, n in (("lam", F32, 2), ("ecb", F32, 2), ("dec", F32, 2), ("decs", F32, 2),
                                          ("Qd", BF16, 2), ("qkm", BF16, 2), ("A", F32, 2), ("Bm", F32, 2),
                                          ("Pt", F32, 2), ("Pf", BF16, 2), ("kdec0", BF16, 2), ("kdec1", BF16, 2),
                                          ("vtok", BF16, 2), ("Y0", BF16, 1), ("vnew", BF16, 1)):
                            setattr(o, nm, Ring(S, "sb", nm, [128, 128], dt, n, ph3))
                        for rg in (o.Y0, o.vnew):
                            for (t_, TD_) in rg.items:
                                S.op("pool", lambda e, t_=t_: e.memset(t_[:], 0.0), (), [TD_])
                        CH.append(o)

                    def prep(c, i):
                        o = CH[c]
                        hh, d = chains[c]
                        h = 2 * hp + hh
                        dh = d * 4 + h
                        blk = slice(i * 128, (i + 1) * 128)
                        INC, STR, NEG, GT = dmask(d)
                        lam, LAM = o.lam.next()
                        ts1("pool", lam[:], INC, SC.la[:, i, dh:dh + 1], ALU.mult, [CST, SC.LA], [LAM])
                        pc, PC = o.pa.next()
                        mm(pc, C(C_ONES), lam[:], True, True, [CST, LAM], [PC])
                        pd, PD = o.pa.next()
                        mm(pd, GT, lam[:], True, False, [CST, LAM], [PD])
                        mm(pd, C(C_ID), NEG, False, True, [CST], [PD])
                        pkk, PKK = o.pa.next()
                        mm(pkk, KT[:, hh, blk], KT[:, hh, blk], True, True, [KTD], [PKK])
                        pqk, PQK = o.pa.next()
                        mm(pqk, KT[:, hh, blk], QT[:, hh, blk], True, True, [KTD, QTD], [PQK])
                        pt2, PT2 = o.pb16.next()
                        tr(pt2, KT[:, hh, blk], idb[:], [KTD, IDB], [PT2])
                        pt3, PT3 = o.pb16.next()
                        tr(pt3, VT[:, hh, blk], idb[:], [VTD, IDB], [PT3])
                        yield
                        ecb, ECB = o.ecb.next()
                        act(ecb[:], pc, AF.Exp, [PC], [ECB])
                        dec, DEC = o.dec.next()
                        act(dec[:], pd, AF.Exp, [PD], [DEC])
                        kdec0, KDEC0 = o.kdec0.next()
                        ts1("dve", kdec0[:], pt2, SC.er[:, i, 0, dh:dh + 1], ALU.mult, [PT2, SC.ER], [KDEC0])
                        kdec1, KDEC1 = o.kdec1.next()
                        ts1("dve", kdec1[:], pt2, SC.er[:, i, 1, dh:dh + 1], ALU.mult, [PT2, SC.ER], [KDEC1])
                        vtok, VTOK = o.vtok.next()
                        cp("act", vtok[:], pt3, [PT3], [VTOK])
                        yield
                        Qd, QD = o.Qd.next()
                        tt("pool", Qd[:], QT[:, hh, blk], ecb[:], ALU.mult, [QTD, ECB], [QD])
                        qkm, QKM = o.qkm.next()
                        tt("dve", qkm[:], pqk, dec[:], ALU.mult, [PQK, DEC], [QKM])
                        decs, DECS = o.decs.next()
                        tt("pool", decs[:], dec[:], STR, ALU.mult, [DEC, CST], [DECS])
                        A0, A0D = o.A.next()
                        stt("dve", A0[:], pkk, SC.beta[:, i, dh:dh + 1], decs[:], ALU.mult, ALU.mult,
                            [PKK, SC.BETA, DECS], [A0D])
                        yield
                        pt1, PT1 = o.pa.next()
                        tr(pt1, A0[:], C(C_ID), [A0D, CST], [PT1])
                        P0, P0D = o.Pt.next()
                        tt("pool", P0[:], C(C_ID), A0[:], ALU.subtract, [CST, A0D], [P0D])
                        yield
                        B0, B0D = o.Bm.next()
                        cp("act", B0[:], pt1, [PT1], [B0D])
                        yield
                        Ap, APD, Bp, BPD, Pp, PPD = A0, A0D, B0, B0D, P0, P0D
                        for lev in range(1, 6):
                            if lev < 5:
                                pA, PA_ = o.pa.next()
                                mm(pA, Bp[:], Ap[:], True, True, [BPD, APD], [PA_])
                            pB, PB_ = o.pa.next()
                            mm(pB, Ap[:], Bp[:], True, True, [APD, BPD], [PB_])
                            yield
                            if lev < 5:
                                An, AND_ = o.A.next()
                                cp("act", An[:], pA, [PA_], [AND_])
                            Bn, BND = o.Bm.next()
                            cp("dve", Bn[:], pB, [PB_], [BND])
                            yield
                            pP, PP_ = o.pa.next()
                            mm(pP, Bn[:], Pp[:], True, True, [BND, PPD], [PP_])
                            yield
                            Pn, PND = (o.Pf if lev == 5 else o.Pt).next()
                            tt("dve", Pn[:], pP, Pp[:], ALU.add, [PP_, PPD], [PND])
                            yield
                            if lev < 5:
                                Ap, APD = An, AND_
                            Bp, BPD, Pp, PPD = Bn, BND, Pn, PND
                        o.cur = dict(ecb=(ecb, ECB), Qd=(Qd, QD), qkm=(qkm, QKM), Pf=(Pp, PPD), kdec=((kdec0, KDEC0), (kdec1, KDEC1)),
                                     vtok=(vtok, VTOK))

                    def steps(c, i, cur):
                        o = CH[c]
                        hh, d = chains[c]
                        h = 2 * hp + hh
                        dh = d * 4 + h
                        blk = slice(i * 128, (i + 1) * 128)
                        ecb, ECB = cur["ecb"]
                        Qd, QD = cur["Qd"]
                        qkm, QKM = cur["qkm"]
                        Pf, PFD = cur["Pf"]
                        vtok, VTOK = cur["vtok"]
                        po, PO = o.pout.next()
                        Y0, Y0D = o.Y0.next()
                        vnew, VNEW = o.vnew.next()
                        for ch in ((0, 1) if d == 0 else (1, 0)):
                            rows = slice(ch * 64, ch * 64 + 64)
                            pks, PKS = o.pstep.next()
                            mm(pks, KT[:, hh, blk], o.Sb[:], True, True, [KTD, o.SB], [PKS])
                            yield
                            stt("dve", Y0[rows, :], pks[rows, :], SC.ecols[rows, i, dh:dh + 1], vtok[rows, :],
                                ALU.mult, ALU.add, [PKS, SC.ECOLS, VTOK], [Y0D])
                            yield
                            pz, PZ = o.pstep.next()
                            mm(pz, Pf[:, :], Y0[:, :], True, True, [PFD, Y0D], [PZ])
                            yield
                            act(vnew[rows, :], pz[rows, :], AF.Copy, [PZ, SC.BETA], [VNEW], scale=SC.beta[rows, i, dh:dh + 1])
                            yield
                            mm(po[:, rows], o.Sb[:], Qd[:, rows], True, False, [o.SB, QD], [PO])
                            mm(po[:, rows], vnew[:, :], qkm[:, rows], False, True, [VNEW, QKM], [PO])
                            pds, PDS = o.pstep.next()
                            kdec, KDEC = cur["kdec"][ch]
                            mm(pds, kdec[:, :], vnew[:, :], True, True, [KDEC, VNEW], [PDS])
                            yield
                            gc = (ch * 64 + 63) if d == 0 else ch * 64
                            stt("dve", o.Sf[:], o.Sf[:], ecb[:, gc:gc + 1], pds, ALU.mult, ALU.add, [o.SF, ECB, PDS], [o.SF])
                            yield
                            cp("pool", o.Sb[:], o.Sf[:], [o.SF], [o.SB])
                            yield
                        tt("dve", oacc[:, hh, blk], oacc[:, hh, blk], po, ALU.add, [OACC, PO], [OACC])

                    roundrobin([prep(c, ORDER[chains[c][1]][0]) for c in range(4)])
                    if chk("G2"):
                        return
                    for n in range(NT):
                        curs = [CH[c].cur for c in range(4)]
                        gens = [steps(c, ORDER[chains[c][1]][n], curs[c]) for c in range(4)]
                        if n + 1 < NT:
                            gens += [prep(c, ORDER[chains[c][1]][n + 1]) for c in range(4)]
                        roundrobin(gens)
                        if n == 0 and chk("G3"):
                            return
                    S.barrier()
                    S.build()
                    if chk("G4"):
                        return
                with ExitStack() as ph4:
                    pss = PsRing(S, "pss", 2, 512, F32, ph4)
                    headnorm(ph4, L, oacc, OACC, ZS, ZSD, 1, hp, yT, YT, pss)
                    S.barrier()
                    S.build()

        def hgrn_headpair(L, b, hp, hT, HT, yT, YT):
            l = L.l
            with ExitStack() as ph:
                qs, QS = S.sb("qs", [128, 2, T], BF16, ph)
                VT, VTD = S.sb("VTh", [128, 2, T], BF16, ph)
                ZS, ZSD = S.sb("ZSh", [128, 2, T], BF16, ph)
                oacc, OACC = S.sb("oacch", [128, 2, T], F32, ph)
                S.op("pool", lambda e: e.memset(oacc[:], 0.0), (), [OACC])
                chains = [(hh, d) for hh in range(2) for d in range(2)]
                CH = []
                for c in range(4):
                    o = NS()
                    o.qd, o.QD = S.sb("qd", [128, T], BF16, ph)
                    o.kd, o.KD = S.sb("kd", [128, T], BF16, ph)
                    o.gch, o.GCH = S.sb("gch", [128, T // 32], F32, ph)
                    CH.append(o)
                with ExitStack() as ph2:
                    tA, TA = S.sb("tA", [128, T], F32, ph2)
                    tB, TB = S.sb("tB", [128, T], F32, ph2)
                    tC, TCD = S.sb("tC", [128, T], F32, ph2)
                    rst, RST = S.sb("rst", [128, T], BF16, ph2)
                    S.op("pool", lambda e: e.memset(rst[:], 1.0), (), [RST])
                    S.op("pool", lambda e: e.memset(rst[:].rearrange("p (c k) -> p c k", k=32)[:, :, 0:1], 0.0), (), [RST])
                    wring = Ring(S, "sb", "wbh", [128, KD, 128], BF16, 2, ph2)
                    pin = PsRing(S, "pinh", 4, 512, F32, ph2)
                    for hh in range(2):
                        h = 2 * hp + hh
                        inproj(L, hT, HT, wring, pin, HG_Q // 128 + h,
                               lambda ps, PS, s, n, hh=hh: act(qs[:, hh, s:s + n], ps[:, 0:n], AF.Silu, [PS], [QS]))
                        inproj(L, hT, HT, wring, pin, HG_I // 128 + h,
                               lambda ps, PS, s, n, hh=hh: cp("dve", VT[:, hh, s:s + n], ps[:, 0:n], [PS], [VTD]))
                        inproj(L, hT, HT, wring, pin, HG_G // 128 + h,
                               lambda ps, PS, s, n, hh=hh: act(ZS[:, hh, s:s + n], ps[:, 0:n], AF.Silu, [PS], [ZSD]))
                    for c in range(4):
                        o = CH[c]
                        hh, d = chains[c]
                        h = 2 * hp + hh
                        dh = d * 4 + h
                        inproj(L, hT, HT, wring, pin, (HG_FF if d == 0 else HG_FB) // 128 + h,
                               lambda ps, PS, s, n: act(tA[:, s:s + n], ps[:, 0:n], AF.Sigmoid, [PS], [TA]))
                        ts2("dve", tA[:], tA[:], oml[:, l, dh:dh + 1], lbt[:, l, dh:dh + 1], ALU.mult, ALU.add,
                            [TA, OML, LBT], [TA])
                        act(tB[:], tA[:], AF.Ln, [TA], [TB])
                        ts2("pool", tA[:], tA[:], -1.0, 1.0, ALU.mult, ALU.add, [TA], [TA])
                        S.op("dve", lambda e: e.tensor_tensor_scan(tC[:], rst[:], tB[:], 0.0, ALU.mult, ALU.add),
                             [RST, TB], [TCD])
                        if d == 0:
                            cum, CUM, oth, OTH = tC, TCD, tB, TB
                            gcol = 31
                        else:
                            tt("pool", tB[:], tB[:], tC[:], ALU.subtract, [TB, TCD], [TB])
                            tB3 = tB[:].rearrange("p (c k) -> p c k", k=32)
                            tC3 = tC[:].rearrange("p (c k) -> p c k", k=32)
                            tt("dve", tB3, tB3, tC3[:, :, 31:32].to_broadcast([128, T // 32, 32]), ALU.add,
                               [TB, TCD], [TB])
                            cum, CUM, oth, OTH = tB, TB, tC, TCD
                            gcol = 0
                        act(oth[:], cum[:], AF.Exp, [CUM], [OTH])
                        tt("pool", o.qd[:], qs[:, hh, :], oth[:], ALU.mult, [QS, OTH], [o.QD])
                        cp("dve", o.gch[:], oth[:].rearrange("p (c k) -> p c k", k=32)[:, :, gcol], [OTH], [o.GCH])
                        act(oth[:], cum[:], AF.Exp, [CUM, o.QD, o.GCH], [OTH], scale=-1.0)
                        tt("dve", o.kd[:], tA[:], oth[:], ALU.mult, [TA, OTH], [o.KD])
                    S.barrier()
                    S.build()
                with ExitStack() as ph3:
                    pa = PsRing(S, "pah", 2, 128, F32, ph3)
                    pb16 = PsRing(S, "pb16h", 1, 128, BF16, ph3)
                    pstep = PsRing(S, "psteph", 2, 128, F32, ph3)
                    pout = PsRing(S, "pouth", 2, 128, F32, ph3)
                    for c in range(4):
                        o = CH[c]
                        o.Sf, o.SF = S.sb("Sfh", [128, 128], F32, ph3)
                        o.Sb, o.SB = S.sb("Sbh", [128, 128], BF16, ph3)
                        o.tS, o.TS = S.sb("tSh", [128, 128], F32, ph3)
                        S.op("dve", lambda e, o=o: e.memset(o.Sf[:], 0.0), (), [o.SF])
                        S.op("dve", lambda e, o=o: e.memset(o.Sb[:], 0.0), (), [o.SB])
                        for nm, dt, n in (("kdtok0", BF16, 2), ("kdtok1", BF16, 2), ("vtok", BF16, 2), ("attm", BF16, 2)):
                            setattr(o, nm, Ring(S, "sb", nm + "h", [128, 128], dt, n, ph3))

                    NBH = T // 64
                    ORDH = {0: list(range(NBH)), 1: [3, 2, 1, 0] + list(range(NBH - 1, 3, -1))}

                    def prep(c, i):
                        o = CH[c]
                        hh, d = chains[c]
                        blk = slice(i * 64, (i + 1) * 64)
                        INC = (cst[0:64, C_INCF32 * 128:C_INCF32 * 128 + 64] if d == 0 else
                               cst[0:64, C_INCB32 * 128:C_INCB32 * 128 + 64])
                        pt1, PT1 = pb16.next()
                        tr(pt1[0:64, :], o.kd[:, blk], idb[:], [o.KD, IDB], [PT1])
                        pt2, PT2 = pb16.next()
                        tr(pt2[0:64, :], VT[:, hh, blk], idb[:], [VTD, IDB], [PT2])
                        pat, PAT = pa.next()
                        mm(pat[0:64, 0:64], o.kd[:, blk], o.qd[:, blk], True, True, [o.KD, o.QD], [PAT])
                        yield
                        kdtok0, KDTOK0 = o.kdtok0.next()
                        act(kdtok0[0:64, :], pt1[0:64, :], AF.Copy, [PT1, CST], [KDTOK0],
                            scale=cst[0:64, C_INCF32 * 128 + 31:C_INCF32 * 128 + 32])
                        kdtok1, KDTOK1 = o.kdtok1.next()
                        act(kdtok1[0:64, :], pt1[0:64, :], AF.Copy, [PT1, CST], [KDTOK1],
                            scale=cst[0:64, C_INCB32 * 128 + 32:C_INCB32 * 128 + 33])
                        vtok, VTOK = o.vtok.next()
                        cp("act", vtok[0:64, :], pt2[0:64, :], [PT2], [VTOK])
                        attm, ATTM = o.attm.next()
                        tt("dve", attm[0:64, 0:64], pat[0:64, 0:64], INC, ALU.mult, [PAT, CST], [ATTM])
                        yield
                        o.cur = dict(kdtok=((kdtok0, KDTOK0), (kdtok1, KDTOK1)), vtok=(vtok, VTOK), attm=(attm, ATTM))

                    def steps(c, i, cur):
                        o = CH[c]
                        hh, d = chains[c]
                        blk0 = i * 64
                        vtok, VTOK = cur["vtok"]
                        attm, ATTM = cur["attm"]
                        po, PO = pout.next()
                        for ch in ((0, 1) if d == 0 else (1, 0)):
                            rows = slice(ch * 32, ch * 32 + 32)
                            cols = slice(blk0 + ch * 32, blk0 + ch * 32 + 32)
                            mm(po[:, rows], o.Sb[:], o.qd[:, cols], True, False, [o.SB, o.QD], [PO])
                            mm(po[:, rows], vtok[0:64, :], attm[0:64, rows], False, True, [VTOK, ATTM], [PO])
                            pds, PDS = pstep.next()
                            kdtok, KDTOK = cur["kdtok"][ch]
                            mm(pds, kdtok[0:64, :], vtok[0:64, :], True, True, [KDTOK, VTOK], [PDS])
                            yield
                            tt("dve", o.tS[:], o.Sf[:], pds, ALU.add, [o.SF, PDS], [o.TS])
                            yield
                            ci = i * 2 + ch
                            ts1("pool", o.Sf[:], o.tS[:], o.gch[:, ci:ci + 1], ALU.mult, [o.TS, o.GCH], [o.SF])
                            act(o.Sb[:], o.tS[:], AF.Copy, [o.TS, o.GCH], [o.SB], scale=o.gch[:, ci:ci + 1])
                            yield
                        tt("dve", oacc[:, hh, blk0:blk0 + 64], oacc[:, hh, blk0:blk0 + 64], po[:, 0:64], ALU.add,
                           [OACC, PO], [OACC])

                    roundrobin([prep(c, ORDH[chains[c][1]][0]) for c in range(4)])
                    for n in range(NBH):
                        curs = [CH[c].cur for c in range(4)]
                        gens = [steps(c, ORDH[chains[c][1]][n], curs[c]) for c in range(4)]
                        if n + 1 < NBH:
                            gens += [prep(c, ORDH[chains[c][1]][n + 1]) for c in range(4)]
                        roundrobin(gens)
                    S.barrier()
                    S.build()
                with ExitStack() as ph4:
                    pss = PsRing(S, "pssh", 2, 512, F32, ph4)
                    headnorm(ph4, L, oacc, OACC, ZS, ZSD, 0, hp, yT, YT, pss)
                    S.barrier()
                    S.build()

        def outproj_phase(L, b, src, SRC, mid, MID, yT, YT, tstart):
            with ExitStack() as ph:
                wo, WO = S.sb("wo", [128, KD, D], BF16, ph)
                S.dma("pool", wo[:], wout_d[L.l].rearrange("(k p) n -> p k n", p=128), writes=[WO])
                pin = PsRing(S, "pino", 4, 512, F32, ph)
                Gx, GX = gbcast(ph, L, b, 0, pin)
                Gc, GC = (None, None) if tstart > 0 else gbcast(ph, L, 2, 0, pin)
                xr = Ring(S, "sb", "xo", [128, D], F32, 3, ph)
                tr_ = Ring(S, "sb", "to", [128, D], F32, 2, ph)
                for i in range(tstart, NT):
                    x, X = xr.next()
                    S.dma("sp", x[:], src[b, i * 128:(i + 1) * 128, :], reads=[SRC], writes=[X])
                    G, GD = (Gc, GC) if i < 2 else (Gx, GX)
                    t_, TD_ = tr_.next()
                    for hf in range(2):
                        ps, PS = pin.next()
                        for k in range(KD):
                            mm(ps[:, :], yT[:, k, i * 128:(i + 1) * 128], wo[:, k, hf * 512:(hf + 1) * 512],
                               k == 0, k == KD - 1, [YT, WO], [PS])
                        tt("dve", t_[:, hf * 512:(hf + 1) * 512], ps[:, :], G[:, hf * 512:(hf + 1) * 512], ALU.mult,
                           [PS, GD], [TD_])
                    tt("pool", x[:], x[:], t_[:], ALU.add, [X, TD_], [X])
                    S.dma("sp", mid[b, i * 128:(i + 1) * 128, :], x[:], reads=[X], writes=[MID])
                S.barrier()
                S.build()

        def moe_phase(L, b, mid, MID, dst, DST, last, tstart):
            l = L.l
            ntm = NT - tstart
            with ExitStack() as bp:
                hT, HT = S.sb("hT2", [128, KD, T], BF16, bp)
                gT, GT_ = S.sb("gT", [16, T], F32, bp)
                norm_phase(L, b, mid, MID, 1, hT, HT, tstart)
                with ExitStack() as ph:
                    lg, LG = S.sb("lg", [128, ntm, 20], F32, ph)
                    pr = PsRing(S, "plg", 2, 32, F32, ph)
                    for ii in range(ntm):
                        i = tstart + ii
                        ps, PS = pr.next()
                        for k in range(KD):
                            mm(ps[:, 0:20], hT[:, k, i * 128:(i + 1) * 128], L.wr[:, k, :], k == 0, False, [HT, L.WR], [PS])
                        mm(ps[:, 0:20], cst[0:1, C_ONES * 128:C_ONES * 128 + 128], L.brow[0:1, :], False, True,
                           [CST, L.BROW], [PS])
                        cp("dve", lg[:, ii, :], ps[:, 0:20], [PS], [LG])

                    def sbt(name, shape):
                        return S.sb(name, shape, F32, ph)
                    gl = lg[:, :, 0:4]
                    el = lg[:, :, 4:20]
                    gmax, GMAX = sbt("gmax", [128, ntm, 1])
                    gmask, GMASK = sbt("gmask", [128, ntm, 4])
                    gex, GEX = sbt("gex", [128, ntm, 4])
                    gw, GW = sbt("gw", [128, ntm, 1])
                    elm, ELM = sbt("elm", [128, ntm, 16])
                    m1, M1 = sbt("m1", [128, ntm, 1])
                    m2, M2 = sbt("m2", [128, ntm, 1])
                    mk1, MK1 = sbt("mk1", [128, ntm, 16])
                    mk2, MK2 = sbt("mk2", [128, ntm, 16])
                    w1, W1 = sbt("w1", [128, ntm, 1])
                    w2, W2 = sbt("w2", [128, ntm, 1])
                    gates, GATES = sbt("gates", [128, ntm, 16])
                    BIG = 1.0e4
                    S.op("dve", lambda e: e.tensor_reduce(gmax[:], gl, AX.X, ALU.max), [LG], [GMAX])
                    tt("dve", gmask[:], gl, gmax[:].to_broadcast([128, ntm, 4]), ALU.is_ge, [LG, GMAX], [GMASK])
                    tt("dve", gex[:], gl, gmax[:].to_broadcast([128, ntm, 4]), ALU.subtract, [LG, GMAX], [GEX])
                    act(gex[:], gex[:], AF.Exp, [GEX], [GEX])
                    S.op("dve", lambda e: e.tensor_reduce(gw[:], gex[:], AX.X, ALU.add), [GEX], [GW])
                    S.op("dve", lambda e: e.reciprocal(gw[:], gw[:]), [GW], [GW])
                    ts2("dve", gmask[:], gmask[:], BIG, -BIG, ALU.mult, ALU.add, [GMASK], [GMASK])
                    tt("dve", elm[:].rearrange("p n (g e) -> p n g e", e=4), el.rearrange("p n (g e) -> p n g e", e=4),
                       gmask[:].unsqueeze(3).to_broadcast([128, ntm, 4, 4]), ALU.add, [LG, GMASK], [ELM])
                    S.op("dve", lambda e: e.tensor_reduce(m1[:], elm[:], AX.X, ALU.max), [ELM], [M1])
                    tt("dve", mk1[:], elm[:], m1[:].to_broadcast([128, ntm, 16]), ALU.is_ge, [ELM, M1], [MK1])
                    stt("dve", elm[:], mk1[:], -BIG, elm[:], ALU.mult, ALU.add, [MK1, ELM], [ELM])
                    S.op("dve", lambda e: e.tensor_reduce(m2[:], elm[:], AX.X, ALU.max), [ELM], [M2])
                    tt("dve", mk2[:], elm[:], m2[:].to_broadcast([128, ntm, 16]), ALU.is_ge, [ELM, M2], [MK2])
                    tt("dve", w2[:], m2[:], m1[:], ALU.subtract, [M1, M2], [W2])
                    act(w2[:], w2[:], AF.Exp, [W2], [W2])
                    ts1("dve", w1[:], w2[:], 1.0, ALU.add, [W2], [W1])
                    S.op("dve", lambda e: e.reciprocal(w1[:], w1[:]), [W1], [W1])
                    tt("dve", w2[:], w2[:], w1[:], ALU.mult, [W1, W2], [W2])
                    tt("dve", w1[:], w1[:], gw[:], ALU.mult, [W1, GW], [W1])
                    tt("dve", w2[:], w2[:], gw[:], ALU.mult, [W2, GW], [W2])
                    tt("dve", mk1[:], mk1[:], w1[:].to_broadcast([128, ntm, 16]), ALU.mult, [MK1, W1], [MK1])
                    tt("dve", mk2[:], mk2[:], w2[:].to_broadcast([128, ntm, 16]), ALU.mult, [MK2, W2], [MK2])
                    tt("dve", gates[:], mk1[:], mk2[:], ALU.add, [MK1, MK2], [GATES])
                    pg = PsRing(S, "pgt", 2, 128, F32, ph)
                    for ii in range(ntm):
                        i = tstart + ii
                        ps, PS = pg.next()
                        tr(ps[0:16, :], gates[:, ii, :], C(C_ID), [GATES, CST], [PS])
                        cp("act", gT[0:16, i * 128:(i + 1) * 128], ps[0:16, :], [PS], [GT_])
                    S.barrier()
                    S.build()
                acc, ACC = S.sb("acc", [128, NT, D], F32, bp)
                tiles = [tt_ for tt_ in TTILES if tt_[0] >= tstart * 128]
                with ExitStack() as ph:
                    wgr = Ring(S, "sb", "wg", [128, KD, FF], BF16, 2, ph)
                    wur = Ring(S, "sb", "wu", [128, KD, FF], BF16, 2, ph)
                    wdr = Ring(S, "sb", "wd", [128, 4, D], BF16, 2, ph)
                    gselr = Ring(S, "sb", "gsel", [16, 512], F32, 2, ph)
                    gbr = Ring(S, "sb", "gbs", [128, 512], F32, 2, ph)
                    sgr = Ring(S, "sb", "sg", [128, 512], F32, 2, ph)
                    t1r = Ring(S, "sb", "t1", [128, 512], F32, 2, ph)
                    atr = Ring(S, "sb", "aT", [128, 4, 512], BF16, 2, ph)
                    pin = PsRing(S, "pinm", 4, 512, F32, ph)
                    pyr = PsRing(S, "pym", 2, 512, F32, ph)
                    pgb = PsRing(S, "pgb", 1, 512, F32, ph)
                    for e_ in range(NE):
                        wg, WG = wgr.next()
                        wu, WU = wur.next()
                        wd, WD = wdr.next()
                        S.dma("pool", wg[:], wg_d[l, e_].rearrange("(k p) f -> p k f", p=128), writes=[WG])
                        S.dma("pool", wu[:], wu_d[l, e_].rearrange("(k p) f -> p k f", p=128), writes=[WU])
                        S.dma("pool", wd[:], wd_d[l, e_].rearrange("(k p) n -> p k n", p=128), writes=[WD])
                        for (s, n) in tiles:
                            gsel, GSEL = gselr.next()
                            ts1("pool", gsel[0:16, 0:n], gT[0:16, s:s + n], cst[0:16, C_ID * 128 + e_:C_ID * 128 + e_ + 1],
                                ALU.mult, [GT_, CST], [GSEL])
                            pb_, PB_ = pgb.next()
                            mm(pb_[:, 0:n], cst[0:16, C_ONES * 128:C_ONES * 128 + 128], gsel[0:16, 0:n], True, True,
                               [CST, GSEL], [PB_])
                            gbs, GBS = gbr.next()
                            cp("act", gbs[:, 0:n], pb_[:, 0:n], [PB_], [GBS])
                            aT, AT = atr.next()
                            for f in range(4):
                                pg_, PG_ = pin.next()
                                for k in range(KD):
                                    mm(pg_[:, 0:n], wg[:, k, f * 128:(f + 1) * 128], hT[:, k, s:s + n], k == 0, k == KD - 1,
                                       [WG, HT], [PG_])
                                pu_, PU_ = pin.next()
                                for k in range(KD):
                                    mm(pu_[:, 0:n], wu[:, k, f * 128:(f + 1) * 128], hT[:, k, s:s + n], k == 0, k == KD - 1,
                                       [WU, HT], [PU_])
                                sg, SG = sgr.next()
                                act(sg[:, 0:n], pg_[:, 0:n], AF.Silu, [PG_], [SG])
                                t1, T1 = t1r.next()
                                tt("dve", t1[:, 0:n], pu_[:, 0:n], sg[:, 0:n], ALU.mult, [PU_, SG], [T1])
                                tt("pool", aT[:, f, 0:n], t1[:, 0:n], gbs[:, 0:n], ALU.mult, [T1, GBS], [AT])
                            for j in range(n // 128):
                                i = s // 128 + j
                                for hf in range(2):
                                    py, PY = pyr.next()
                                    for f in range(4):
                                        mm(py[:, :], aT[:, f, j * 128:(j + 1) * 128], wd[:, f, hf * 512:(hf + 1) * 512],
                                           f == 0, f == 3, [AT, WD], [PY])
                                    a_ = acc[:, i, hf * 512:(hf + 1) * 512]
                                    if e_ == 0:
                                        cp("act", a_, py[:, :], [PY], [ACC])
                                    else:
                                        tt("dve", a_, a_, py[:, :], ALU.add, [ACC, PY], [ACC])
                    S.barrier()
                    S.build()
                with ExitStack() as ph:
                    pin = PsRing(S, "pinr", 2, 512, F32, ph)
                    Gx, GX = gbcast(ph, L, b, 1, pin)
                    Gc, GC = (None, None) if tstart > 0 else gbcast(ph, L, 2, 1, pin)
                    xr = Ring(S, "sb", "xm", [128, D], F32, 3, ph)
                    tr_ = Ring(S, "sb", "tm", [128, D], F32, 2, ph)
                    if last:
                        fnw, FNW = S.sb("fnw", [128, D], F32, ph)
                        S.dma("sp", fnw[:], fnw_d, writes=[FNW])
                        junk, JUNK = S.sb("junkf", [128, D], BF16, ph)
                        str_ = Ring(S, "sb", "stf", [128, 4], F32, 3, ph)
                    for i in range(tstart, NT):
                        x, X = xr.next()
                        S.dma("sp", x[:], mid[b, i * 128:(i + 1) * 128, :], reads=[MID], writes=[X])
                        G, GD = (Gc, GC) if i < 2 else (Gx, GX)
                        t_, TD_ = tr_.next()
                        tt("dve", t_[:], acc[:, i, :], G[:], ALU.mult, [ACC, GD], [TD_])
                        tt("pool", x[:], x[:], t_[:], ALU.add, [X, TD_], [X])
                        if not last:
                            S.dma("sp", dst[b, i * 128:(i + 1) * 128, :], x[:], reads=[X], writes=[DST])
                        else:
                            st, ST = str_.next()
                            act(junk[:], x[:], AF.Square, [X], [JUNK, ST], accum_out=st[:, 0:1])
                            act(st[:, 1:2], st[:, 0:1], AF.Sqrt, [ST, EPSC], [ST], scale=1.0 / D, bias=epsc[:, 0:1])
                            S.op("dve", lambda e, st=st: e.reciprocal(st[:, 2:3], st[:, 1:2]), [ST], [ST])
                            stt("dve", t_[:], x[:], st[:, 2:3], fnw[:], ALU.mult, ALU.mult, [X, ST, FNW], [TD_])
                            S.dma("sp", out_d[b, (i - 2) * 128:(i - 1) * 128, :], t_[:], reads=[TD_], writes=[ROUT[b]])
                    S.barrier()
                    S.build()

        def run_pass(L, b, last, tstart, src, SRC, mid, MID, dst, DST):
            with ExitStack() as bp:
                hT, HT = S.sb("hT", [128, KD, T], BF16, bp)
                yT, YT = S.sb("yT", [128, KD, T], BF16, bp)
                norm_phase(L, b, src, SRC, 0, hT, HT, 0)
                if "hT" in dbg_out and L.l == 0 and b == 0:
                    dd = Dep("dbg_hT")
                    S.dma("pool", dbg_out["hT"].rearrange("p (k t) -> p k t", k=KD), hT[:], reads=[HT], writes=[dd])
                if stop == "A":
                    S.barrier()
                    S.build()
                    return
                with ExitStack() as scs:
                    SC = small_cols(L, b, hT, HT, scs)
                    if chk("SC"):
                        return
                    for hp in range(2):
                        gdn_headpair(L, b, hp, hT, HT, yT, YT, SC)
                        if STOPPED[0]:
                            return
                    S.barrier()
                    S.build()
                for hp in range(2):
                    hgrn_headpair(L, b, hp, hT, HT, yT, YT)
                    if STOPPED[0]:
                        return
                if "yT" in dbg_out and L.l == 0 and b == 0:
                    dd = Dep("dbg_yT")
                    S.dma("pool", dbg_out["yT"].rearrange("p (k t) -> p k t", k=KD), yT[:], reads=[YT], writes=[dd])
                if stop == "Y":
                    S.barrier()
                    S.build()
                    return
                outproj_phase(L, b, src, SRC, mid, MID, yT, YT, tstart)
            if stop == "M":
                return
            moe_phase(L, b, mid, MID, dst, DST, last, tstart)

        RXIN = Dep("rxin")
        RA = [Dep(f"resA{b}") for b in range(NB)]
        RB = [Dep(f"resB{b}") for b in range(NB)]
        ROUT = [Dep(f"rout{b}") for b in range(NB)]

        for l in range(nlayers):
          try:
            last = (l == DEPTH - 1)
            tstart = 2 if last else 0
            with ExitStack() as lay:
                L = NS()
                L.l = l
                L.modT, L.MODT = S.sb("modT", [128, 48, 3], F32, lay)
                L.abc, L.ABC = S.sb("abc", [128, 4, KD, 3], F32, lay)
                L.wr, L.WR = S.sb("wr", [128, KD, 20], BF16, lay)
                L.brow, L.BROW = S.sb("brow", [1, 20], F32, lay)
                S.dma("pool", L.wr[:], wr_d[l], writes=[L.WR])
                S.dma("sp", L.brow[:], br_d[l:l + 1, :], writes=[L.BROW])
                with ExitStack() as ph:
                    wmr = Ring(S, "sb", "wm", [128, KD, 512], F32, 2, ph)
                    bmr = Ring(S, "sb", "bm", [1, 512], F32, 2, ph)
                    pst = PsRing(S, "pmT", 2, 16, F32, ph)
                    for n in range(12):
                        wm, WM = wmr.next()
                        bm, BM = bmr.next()
                        S.dma("sp", wm[:], wmod_d[l].rearrange("(k p) n -> p k n", p=128)[:, :, n * 512:(n + 1) * 512],
                              writes=[WM])
                        S.dma("sp", bm[:], bmod_d[l:l + 1, n * 512:(n + 1) * 512], writes=[BM])
                        pt, PT = pst.next()
                        for sub in range(4):
                            o = pt[:, sub * 3:sub * 3 + 3]
                            for k in range(KD):
                                mm(o, wm[:, k, sub * 128:(sub + 1) * 128], scT[:, k, :], k == 0, False,
                                   [WM, SCT], [PT])
                            mm(o, bm[0:1, sub * 128:(sub + 1) * 128], cst[0:1, C_ONES * 128:C_ONES * 128 + 3],
                               False, True, [BM, CST], [PT])
                        cp("dve", L.modT[:, n * 4:(n + 1) * 4, :], pt[:, 0:12].rearrange("p (a b) -> p a b", b=3),
                           [PT], [L.MODT])
                    for wi, (csc, csh) in enumerate(((8, 0), (32, 24))):
                        ts1("dve", L.abc[:, 2 * wi, :, :], L.modT[:, csc:csc + 8, :], 1.0, ALU.add, [L.MODT], [L.ABC])
                        tt("dve", L.abc[:, 2 * wi, :, :], L.abc[:, 2 * wi, :, :],
                           normw[:, l, wi, :].unsqueeze(2).to_broadcast([128, KD, 3]), ALU.mult,
                           [L.ABC, NORMW], [L.ABC])
                        cp("dve", L.abc[:, 2 * wi + 1, :, :], L.modT[:, csh:csh + 8, :], [L.MODT], [L.ABC])
                    if "abc" in dbg_out and l == 0:
                        S.dma("sp", dbg_out["abc"], L.abc[:].rearrange("p a b c -> p (a b c)"), reads=[L.ABC], writes=[Dep("dbg_abc")])
                        S.dma("sp", dbg_out["modT"], L.modT[:].rearrange("p a b -> p (a b)"), reads=[L.MODT], writes=[Dep("dbg_modT")])
                        S.dma("sp", dbg_out["scT"], scT[:].rearrange("p a b -> p (a b)"), reads=[SCT], writes=[Dep("dbg_scT")])
                    S.barrier()
                    S.build()
                for b in range(nb_run):
                    src, SRC = (xin, RXIN) if l == 0 else (resB, RB[b])
                    run_pass(L, b, last, tstart, src, SRC, resA, RA[b], resB, RB[b])
                    if STOPPED[0]:
                        break
          except _Stop:
            break
          if STOPPED[0]:
            break
        if "res_out" in dbg_out:
            dd = Dep("dbg_res")
            for b in range(nb_run):
                if stop == "M":
                    S.dma("sp", dbg_out["res_out"][b], resA[b], reads=[RA[b]], writes=[dd])
                else:
                    S.dma("sp", dbg_out["res_out"][b], resB[b], reads=[RB[b]], writes=[dd])
        S.barrier()
        S.build()
    return nc


def make_consts():
    c = np.zeros((128, NCONST), np.float32)
    idx = np.arange(128)
    s = idx[:, None]
    t = idx[None, :]
    same = (s // 64) == (t // 64)

    def put(i, m):
        c[:, i * 128:(i + 1) * 128] = m.astype(np.float32)
    put(C_ID, s == t)
    put(C_ONES, np.ones((128, 128)))
    incf = same & (s <= t)
    incb = same & (s >= t)
    put(C_INCF, incf)
    put(C_STRF, same & (s < t))
    put(C_INCB, incb)
    put(C_STRB, same & (s > t))
    put(C_NEGF, np.where(incf, 0.0, -30000.0))
    put(C_NEGB, np.where(incb, 0.0, -30000.0))
    put(C_GTF, s > t)
    put(C_GTB, s < t)
    same32 = (s // 32) == (t // 32)
    put(C_INCF32, same32 & (s <= t))
    put(C_INCB32, same32 & (s >= t))
    return c


def prepare_shared(inp):
    f = np.float32
    sh = {}
    sh["consts"] = make_consts()
    sh["w_mod"] = np.ascontiguousarray(inp["w_mod"], dtype=f)
    sh["b_mod"] = np.ascontiguousarray(inp["b_mod"], dtype=f)
    nw = np.stack([inp["norm1_w"], inp["norm2_w"]], axis=1)
    sh["normw"] = np.ascontiguousarray(nw.reshape(DEPTH, 2, KD, 128).transpose(3, 0, 1, 2).reshape(128, -1), dtype=f)
    lb = inp["hg_lb"].reshape(DEPTH, 2, 4, 128)
    sh["lbh"] = np.ascontiguousarray(lb.transpose(3, 0, 1, 2).reshape(128, -1), dtype=f)
    hn = np.stack([inp["hg_norm_w"], inp["dn_norm_w"]], axis=1)
    sh["headnw"] = np.ascontiguousarray(hn.transpose(2, 0, 1).reshape(128, -1), dtype=f)
    cw = inp["dn_conv_w"].reshape(DEPTH, 9, 12, 128)
    sh["convw"] = np.ascontiguousarray(cw.transpose(3, 0, 2, 1).reshape(128, -1), dtype=f)
    al = np.concatenate([inp["dn_a_log"].reshape(DEPTH, 8), inp["dn_dt_bias"].reshape(DEPTH, 8)], axis=1)
    sh["alog"] = np.ascontiguousarray(np.broadcast_to(al.reshape(1, -1), (128, DEPTH * 16)), dtype=f)
    wrr = np.concatenate([inp["w_group"], inp["w_expert"]], axis=2)
    sh["wr"] = np.ascontiguousarray(wrr.reshape(DEPTH, KD, 128, 20).transpose(0, 2, 1, 3), dtype=f)
    sh["br"] = np.ascontiguousarray(np.concatenate([inp["b_group"], inp["b_expert"]], axis=1), dtype=f)
    sh["fnw"] = np.ascontiguousarray(np.broadcast_to(inp["final_norm_w"].reshape(1, D), (128, D)), dtype=f)
    wi = np.zeros((DEPTH, D, NGRP * 128), f)
    wi[:, :, :IN_COLS] = inp["w_in"]
    sh["win"] = np.ascontiguousarray(wi.reshape(DEPTH, KD, 128, NGRP, 128).transpose(0, 3, 2, 1, 4))
    sh["w_out"] = np.ascontiguousarray(inp["w_out"], dtype=f)
    sh["w_gate"] = np.ascontiguousarray(inp["w_gate"], dtype=f)
    sh["w_up"] = np.ascontiguousarray(inp["w_up"], dtype=f)
    sh["w_down"] = np.ascontiguousarray(inp["w_down"], dtype=f)
    return sh


def prepare_core(inp, core):
    f = np.float32
    b0 = core * NB
    m = {}
    m["xin"] = np.ascontiguousarray(np.concatenate([inp["ctx"][b0:b0 + NB], inp["x"][b0:b0 + NB]], axis=1), dtype=f)
    cv = np.concatenate([inp["c"][b0:b0 + NB], inp["c_ctx"].reshape(1, D)], axis=0)
    m["cvec"] = np.ascontiguousarray(cv.reshape(3, KD, 128).transpose(2, 1, 0), dtype=f)
    return m


_CACHE = {}


def kernel(**inputs):
    inp = {k: np.asarray(v) for k, v in inputs.items()}
    n_cores = 8
    if "nc" not in _CACHE:
        _CACHE["nc"] = build_program(DEPTH)
    nc = _CACHE["nc"]
    shared = prepare_shared(inp)
    in_maps = []
    for core in range(n_cores):
        m = dict(shared)
        m.update(prepare_core(inp, core))
        in_maps.append(m)
    res = run_bass_kernel_spmd(nc, in_maps, core_ids=list(range(n_cores)))
    out = np.concatenate([np.asarray(r["out"], dtype=np.float32) for r in res.results], axis=0)
    return out
```

```python
import numpy as np
from contextlib import ExitStack
import concourse.bass as bass
import concourse.mybir as mybir
from concourse.bass_utils import run_bass_kernel_spmd

F32 = mybir.dt.float32
BF16 = mybir.dt.bfloat16
ALU = mybir.AluOpType
AF = mybir.ActivationFunctionType
AX = mybir.AxisListType

D = 1024
KD = 8
DEPTH = 4
NB = 2
TC = 256
TL = 2048
T = TC + TL
NT = T // 128
GRID_W = 64
IN_COLS = 4624
NGRP = 37
FF = 512
NE = 16
EPS = 1e-6
HG_Q, HG_I, HG_G, HG_FF, HG_FB = 0, 512, 1024, 1536, 2048
DN_Q, DN_K, DN_V, DN_Z = 2560, 3072, 3584, 4096
SMALL0 = 4608
C_ID, C_ONES, C_INCF, C_STRF, C_INCB, C_STRB, C_NEGF, C_NEGB, C_GTF, C_GTB, C_INCF32, C_INCB32 = range(12)
NCONST = 12 * 128
TTILES = [(0, 256), (256, 512), (768, 512), (1280, 512), (1792, 512)]


class Dep:
    __slots__ = ("name", "lw", "rd", "sem", "semv", "excl")

    def __init__(self, name, excl=False):
        self.name = name
        self.excl = excl
        self.lw = None
        self.rd = {}
        self.sem = None
        self.semv = 0


class Sched:
    ENGS = ("pe", "dve", "act", "pool", "sp")

    def __init__(self, nc, stack):
        self.nc = nc
        self.stack = stack
        self.q = {e: [] for e in self.ENGS}
        self.cnt = {e: 0 for e in self.ENGS}
        self.seen = {}
        for e in self.ENGS:
            self.seen[e] = {}
            self.seen["dmaq_" + e] = {}
        self.esem = {e: stack.enter_context(nc.semaphore("prog_" + e)) for e in self.ENGS}
        self.dsems = []
        self.free_sems = []
        self.nsem_alloc = 0
        self.ninst = 0
        self.uid = 0

    def sb(self, name, shape, dt, stack=None):
        self.uid += 1
        nm = f"{name}_{self.uid}"
        t = (stack or self.stack).enter_context(self.nc.sbuf_tensor(nm, list(shape), dt))
        return t, Dep(nm)

    def ps(self, name, shape, dt, stack=None):
        self.uid += 1
        nm = f"{name}_{self.uid}"
        t = (stack or self.stack).enter_context(self.nc.psum_tensor(nm, list(shape), dt))
        return t, Dep(nm)

    def _dsem(self, d):
        if d.sem is None:
            if self.free_sems:
                d.sem, d.semv = self.free_sems.pop()
            else:
                self.nsem_alloc += 1
                d.sem = self.stack.enter_context(self.nc.semaphore("dsem%d" % self.nsem_alloc))
                d.semv = 0
            self.dsems.append(d)
        return d.sem

    def _need(self, eng, ev, waits, is_dma, raw=False):
        if ev is None:
            return
        key, val, sem = ev
        if key == eng and not is_dma and not (raw and eng != "pe"):
            return
        k2 = ("dmaq_" + eng) if is_dma else eng
        if self.seen[k2].get(key, 0) >= val:
            return
        self.seen[k2][key] = val
        if not is_dma:
            pass
        waits.append((sem, val))

    def _collect(self, eng, reads, writes, is_dma=False):
        waits = []
        for d in reads:
            self._need(eng, d.lw, waits, is_dma, raw=True)
        for d in writes:
            self._need(eng, d.lw, waits, is_dma)
            for ev in d.rd.values():
                self._need(eng, ev, waits, is_dma)
        return waits

    def _mark(self, ev, reads, writes):
        for d in reads:
            old = d.rd.get(ev[0])
            if old is None or old[1] < ev[1]:
                d.rd[ev[0]] = ev
        for d in writes:
            d.lw = ev
            d.rd = {}

    def op(self, eng, fn, reads=(), writes=()):
        if any(d.excl for d in reads):
            writes = list(writes) + [d for d in reads if d.excl]
            reads = [d for d in reads if not d.excl]
        waits = self._collect(eng, reads, writes)
        self.cnt[eng] += 1
        ev = (eng, self.cnt[eng], self.esem[eng])
        self._mark(ev, reads, writes)
        self.q[eng].append((waits, fn, self.esem[eng], 1))
        self.ninst += 1

    def dma(self, eng, out, in_, reads=(), writes=(), **kw):
        waits = self._collect(eng, reads, writes, True)
        anchor = (list(writes) + list(reads))[0]
        sem = self._dsem(anchor)
        anchor.semv += 16
        ev = ("dma_%d" % id(sem), anchor.semv, sem)
        self._mark(ev, reads, writes)
        self.q[eng].append((waits, lambda e: e.dma_start(out=out, in_=in_, **kw), sem, 16))
        self.ninst += 1

    def barrier(self):
        for e in self.ENGS:
            waits = []
            for o in self.ENGS:
                if o != e and self.cnt[o] > self.seen[e].get(o, 0):
                    self.seen[e][o] = self.cnt[o]
                    waits.append((self.esem[o], self.cnt[o]))
            for d in self.dsems:
                key = "dma_%d" % id(d.sem)
                if d.semv > self.seen[e].get(key, 0):
                    self.seen[e][key] = d.semv
                    waits.append((d.sem, d.semv))
            for k, v in self.seen[e].items():
                if self.seen["dmaq_" + e].get(k, 0) < v:
                    self.seen["dmaq_" + e][k] = v
            self.q[e].append((waits, None, None, 0))
        for d in self.dsems:
            self.free_sems.append((d.sem, d.semv))
            d.sem = None
        self.dsems = []

    def build(self):
        nc = self.nc
        with nc.Block() as block:
            def run(eng_name):
                def body(e):
                    for waits, fn, sem, inc in self.q[eng_name]:
                        if fn is None:
                            for (s, v) in waits:
                                e.wait_ge(s, v)
                            continue
                        for (s, v) in waits[:-1]:
                            e.wait_ge(s, v)
                        ins = fn(e)
                        if waits:
                            ins._wait_ge(waits[-1][0], waits[-1][1])
                        ins.then_inc(sem, inc)
                return body
            block.tensor(run("pe"))
            block.vector(run("dve"))
            block.scalar(run("act"))
            block.gpsimd(run("pool"))
            block.sync(run("sp"))
        self.q = {e: [] for e in self.ENGS}


class Ring:
    def __init__(self, S, kind, name, shape, dt, n, stack):
        mk = S.sb if kind == "sb" else S.ps
        self.items = [mk(f"{name}{i}", shape, dt, stack) for i in range(n)]
        self.i = 0

    def next(self):
        it = self.items[self.i % len(self.items)]
        self.i += 1
        return it


class PsRing:
    def __init__(self, S, name, nbanks, width, dt, stack):
        per = (2048 // (4 if dt == F32 else 2))
        banks = []
        for b in range(nbanks):
            t, _ = S.ps(f"{name}{b}", [128, per], dt, stack)
            banks.append((t, Dep(f"{name}{b}", excl=True)))
        self.items = []
        for j in range(per // width):
            for (t, dep) in banks:
                self.items.append((t[:, j * width:(j + 1) * width], dep))
        self.i = 0

    def next(self):
        it = self.items[self.i % len(self.items)]
        self.i += 1
        return it


def roundrobin(gens):
    gens = list(gens)
    while gens:
        for g in list(gens):
            try:
                next(g)
            except StopIteration:
                gens.remove(g)


def build_program(nlayers=DEPTH, dbg=(), stop=None, nb_run=NB):
    nc = bass.Bass("TRN2", target_bir_lowering=False)

    def din(name, shape):
        return nc.dram_tensor(name, list(shape), F32, kind="ExternalInput").ap()

    xin = din("xin", [NB, T, D])
    cvec_d = din("cvec", [128, KD, 3])
    consts_d = din("consts", [128, NCONST])
    wmod_d = din("w_mod", [DEPTH, D, 6 * D])
    bmod_d = din("b_mod", [DEPTH, 6 * D])
    normw_d = din("normw", [128, DEPTH * 2 * KD])
    lb_d = din("lbh", [128, DEPTH * 8])
    headnw_d = din("headnw", [128, DEPTH * 2])
    convw_d = din("convw", [128, DEPTH * 12 * 9])
    alog_d = din("alog", [128, DEPTH * 16])
    wr_d = din("wr", [DEPTH, 128, KD, 20])
    br_d = din("br", [DEPTH, 20])
    fnw_d = din("fnw", [128, D])
    win_d = din("win", [DEPTH, NGRP, 128, KD, 128])
    wout_d = din("w_out", [DEPTH, D, D])
    wg_d = din("w_gate", [DEPTH, NE, D, FF])
    wu_d = din("w_up", [DEPTH, NE, D, FF])
    wd_d = din("w_down", [DEPTH, NE, FF, D])
    out_d = nc.dram_tensor("out", [NB, TL, D], F32, kind="ExternalOutput").ap()
    resA = nc.dram_tensor("resA", [NB, T, D], F32, kind="Internal").ap()
    resB = nc.dram_tensor("resB", [NB, T, D], F32, kind="Internal").ap()
    dbg_out = {}
    for name, shape in dbg:
        dbg_out[name] = nc.dram_tensor(name, list(shape), F32, kind="ExternalOutput").ap()

    with ExitStack() as top:
        S = Sched(nc, top)

        def mm(out, lhsT, rhs, st, sp, R, W):
            S.op("pe", lambda e: e.matmul(out, lhsT=lhsT, rhs=rhs, start=st, stop=sp), R, W)

        def tr(out, in_, ident, R, W):
            S.op("pe", lambda e: e.transpose(out, in_, ident), R, W)

        def act(out, in_, func, R, W, **kw):
            S.op("act", lambda e: e.activation(out, in_, func, **kw), R, W)

        def tt(eng, out, a, b, op, R, W):
            S.op(eng, lambda e: e.tensor_tensor(out, a, b, op), R, W)

        def ts1(eng, out, a, s, op, R, W):
            S.op(eng, lambda e: e.tensor_single_scalar(out, a, s, op), R, W)

        def ts2(eng, out, a, s1, s2, op0, op1, R, W):
            S.op(eng, lambda e: e.tensor_scalar(out, a, s1, s2, op0, op1), R, W)

        def stt(eng, out, a, s, b, op0, op1, R, W):
            S.op(eng, lambda e: e.scalar_tensor_tensor(out, a, s, b, op0, op1), R, W)

        def cp(eng, out, a, R, W):
            if eng == "act":
                S.op("act", lambda e: e.copy(out, a), R, W)
            else:
                S.op(eng, lambda e: e.tensor_copy(out, a), R, W)

        def dump(name, ap_sb, DEP, dram_ap=None):
            if name in dbg_out:
                dd = Dep("dbg_" + name)
                S.dma("sp", dram_ap if dram_ap is not None else dbg_out[name], ap_sb, reads=[DEP], writes=[dd])

        cst, CST = S.sb("cst", [128, NCONST], F32)
        S.dma("sp", cst[:], consts_d, writes=[CST])

        def C(i):
            return cst[:, i * 128:(i + 1) * 128]

        idb, IDB = S.sb("idb", [128, 128], BF16)
        onb, ONB = S.sb("onb", [128, 128], BF16)
        cp("dve", idb[:], C(C_ID), [CST], [IDB])
        cp("dve", onb[:], C(C_ONES), [CST], [ONB])
        epsc, EPSC = S.sb("epsc", [128, 1], F32)
        S.op("dve", lambda e: e.memset(epsc[:], EPS), (), [EPSC])
        normw, NORMW = S.sb("normw", [128, DEPTH, 2, KD], F32)
        S.dma("sp", normw[:].rearrange("p a b c -> p (a b c)"), normw_d, writes=[NORMW])
        headnw, HEADNW = S.sb("headnw", [128, DEPTH, 2], F32)
        S.dma("sp", headnw[:].rearrange("p a b -> p (a b)"), headnw_d, writes=[HEADNW])
        convw, CONVW = S.sb("convw", [128, DEPTH, 12, 9], F32)
        S.dma("sp", convw[:].rearrange("p a b c -> p (a b c)"), convw_d, writes=[CONVW])
        alog, ALOG = S.sb("alog", [128, DEPTH, 16], F32)
        S.dma("sp", alog[:].rearrange("p a b -> p (a b)"), alog_d, writes=[ALOG])
        act(alog[:, :, 0:8], alog[:, :, 0:8], AF.Exp, [ALOG], [ALOG])
        ts1("dve", alog[:, :, 0:8], alog[:, :, 0:8], -1.0, ALU.mult, [ALOG], [ALOG])
        scT, SCT = S.sb("scT", [128, KD, 3], F32)
        S.dma("sp", scT[:].rearrange("p a b -> p (a b)"), cvec_d.rearrange("p a b -> p (a b)"), writes=[SCT])
        act(scT[:], scT[:], AF.Silu, [SCT], [SCT])
        lbt, LBT = S.sb("lbt", [128, DEPTH, 8], F32)
        oml, OML = S.sb("oml", [128, DEPTH, 8], F32)
        with ExitStack() as ph:
            raw, RAWL = S.sb("lbraw", [128, DEPTH, 8], F32, ph)
            m8, M8 = S.sb("lbm", [128, 8], F32, ph)
            S.dma("sp", raw[:].rearrange("p a b -> p (a b)"), lb_d, writes=[RAWL])
            tt("dve", m8[:], raw[:, 0, :], raw[:, 1, :], ALU.max, [RAWL], [M8])
            tt("dve", m8[:], m8[:], raw[:, 2, :], ALU.max, [RAWL, M8], [M8])
            tt("dve", m8[:], m8[:], raw[:, 3, :], ALU.max, [RAWL, M8], [M8])
            for l in range(DEPTH):
                tt("dve", raw[:, l, :], raw[:, l, :], m8[:], ALU.subtract, [RAWL, M8], [RAWL])
            act(raw[:], raw[:], AF.Exp, [RAWL], [RAWL])
            tt("dve", m8[:], raw[:, 0, :], raw[:, 1, :], ALU.add, [RAWL], [M8])
            tt("dve", m8[:], m8[:], raw[:, 2, :], ALU.add, [RAWL, M8], [M8])
            tt("dve", m8[:], m8[:], raw[:, 3, :], ALU.add, [RAWL, M8], [M8])
            S.op("dve", lambda e: e.reciprocal(m8[:], m8[:]), [M8], [M8])
            for l in range(DEPTH):
                tt("dve", raw[:, l, :], raw[:, l, :], m8[:], ALU.mult, [RAWL, M8], [RAWL])
            S.op("dve", lambda e: e.memset(lbt[:, 0, :], 0.0), (), [LBT])
            cp("dve", lbt[:, 1, :], raw[:, 1, :], [RAWL], [LBT])
            tt("dve", lbt[:, 2, :], lbt[:, 1, :], raw[:, 2, :], ALU.add, [RAWL, LBT], [LBT])
            tt("dve", lbt[:, 3, :], lbt[:, 2, :], raw[:, 3, :], ALU.add, [RAWL, LBT], [LBT])
            ts2("dve", oml[:], lbt[:], -1.0, 1.0, ALU.mult, ALU.add, [LBT], [OML])
            S.barrier()
            S.build()

        class NS:
            pass

        class _Stop(Exception):
            pass

        STOPPED = [False]

        def chk(tag):
            if stop == tag and not STOPPED[0]:
                S.barrier()
                S.build()
                STOPPED[0] = True
            return STOPPED[0]

        ORDER = {0: list(range(NT)), 1: [1, 0] + list(range(NT - 1, 1, -1))}

        def dmask(d):
            return (C(C_INCF), C(C_STRF), C(C_NEGF), C(C_GTF)) if d == 0 else \
                   (C(C_INCB), C(C_STRB), C(C_NEGB), C(C_GTB))

        def gbcast(ph, L, r, w, psr):
            G, GD = S.sb("G", [128, D], F32, ph)
            lr = Ring(S, "sb", "gl", [128, 128], F32, 2, ph)
            base = (2 + 3 * w) * 8
            for hf in range(2):
                ps, PS = psr.next()
                for k4 in range(4):
                    k = hf * 4 + k4
                    lh, LH = lr.next()
                    cp("dve", lh[:], L.modT[:, base + k, r:r + 1].to_broadcast([128, 128]), [L.MODT], [LH])
                    mm(ps[:, k4 * 128:(k4 + 1) * 128], lh[:], C(C_ID), True, True, [LH, CST], [PS])
                cp("act", G[:, hf * 512:(hf + 1) * 512], ps[:, :], [PS], [GD])
            return G, GD

        def norm_phase(L, b, src, SRC, wi, hT, HT, tstart):
            with ExitStack() as ph:
                xr = Ring(S, "sb", "xt", [128, D], F32, 3, ph)
                xnr = Ring(S, "sb", "xn", [128, D], F32, 2, ph)
                junk, JUNK = S.sb("junk", [128, D], BF16, ph)
                stt_r = Ring(S, "sb", "st", [128, 4], F32, 4, ph)
                ptr = PsRing(S, "ptr", 4, 512, F32, ph)
                tmpr = Ring(S, "sb", "mt", [128, 4, 128], F32, 3, ph)
                for i in range(tstart, NT):
                    x, X = xr.next()
                    S.dma("sp", x[:], src[b, i * 128:(i + 1) * 128, :], reads=[SRC], writes=[X])
                    st, ST = stt_r.next()
                    act(junk[:], x[:], AF.Square, [X], [JUNK, ST], accum_out=st[:, 0:1])
                    act(st[:, 1:2], st[:, 0:1], AF.Sqrt, [ST, EPSC], [ST], scale=1.0 / D, bias=epsc[:, 0:1])
                    S.op("dve", lambda e, st=st: e.reciprocal(st[:, 2:3], st[:, 1:2]), [ST], [ST])
                    xn, XN = xnr.next()
                    act(xn[:], x[:], AF.Copy, [X, ST], [XN], scale=st[:, 2:3])
                    r = 2 if i < 2 else b
                    for half in range(2):
                        pp, PP = ptr.next()
                        for k4 in range(4):
                            k = half * 4 + k4
                            tr(pp[:, k4 * 128:(k4 + 1) * 128], xn[:, k * 128:(k + 1) * 128], C(C_ID), [XN, CST], [PP])
                        m, M = tmpr.next()
                        A = L.abc[:, 2 * wi, half * 4:half * 4 + 4, r:r + 1].to_broadcast([128, 4, 128])
                        B = L.abc[:, 2 * wi + 1, half * 4:half * 4 + 4, r:r + 1].to_broadcast([128, 4, 128])
                        tt("dve", m[:], pp.rearrange("p (a b) -> p a b", b=128), A, ALU.mult, [PP, L.ABC], [M])
                        tt("pool", hT[:, half * 4:half * 4 + 4, i * 128:(i + 1) * 128], m[:], B, ALU.add,
                           [M, L.ABC], [HT])
                S.barrier()
                S.build()

        def inproj(L, hT, HT, wring, pin, g, evac):
            wb, WB = wring.next()
            S.dma("pool", wb[:], win_d[L.l, g], writes=[WB])
            for (s, n) in TTILES:
                ps, PS = pin.next()
                for k in range(KD):
                    mm(ps[:, 0:n], wb[:, k, :], hT[:, k, s:s + n], k == 0, k == KD - 1, [WB, HT], [PS])
                evac(ps, PS, s, n)

        def conv(eng, raw, RAW, acc, ACC, wcol):
            rl = raw[:, TC:T].rearrange("p (r c) -> p r c", c=GRID_W)
            al = acc[:, TC:T].rearrange("p (r c) -> p r c", c=GRID_W)
            ts1(eng, acc[:, TC:T], raw[:, TC:T], wcol(4), ALU.mult, [RAW, CONVW], [ACC])
            for a in range(3):
                for b3 in range(3):
                    if a == 1 and b3 == 1:
                        continue
                    dr, dc = a - 1, b3 - 1
                    r0, r1 = max(0, -dr), 32 - max(0, dr)
                    c0, c1 = max(0, -dc), GRID_W - max(0, dc)
                    stt(eng, al[:, r0:r1, c0:c1], rl[:, r0 + dr:r1 + dr, c0 + dc:c1 + dc], wcol(a * 3 + b3),
                        al[:, r0:r1, c0:c1], ALU.mult, ALU.add, [RAW, ACC, CONVW], [ACC])
            ts1(eng, acc[:, 0:TC], raw[:, 0:TC], wcol(4), ALU.mult, [RAW, CONVW], [ACC])
            stt(eng, acc[:, 1:TC], raw[:, 0:TC - 1], wcol(3), acc[:, 1:TC], ALU.mult, ALU.add, [RAW, ACC, CONVW], [ACC])
            stt(eng, acc[:, 0:TC - 1], raw[:, 1:TC], wcol(5), acc[:, 0:TC - 1], ALU.mult, ALU.add,
                [RAW, ACC, CONVW], [ACC])

        def headnorm(ph, L, oacc, OACC, ZS, ZSD, which, hp, yT, YT, pss):
            sqr = Ring(S, "sb", "hsq", [128, 512], BF16, 2, ph)
            rtr = Ring(S, "sb", "hrt", [128, 512], F32, 2, ph)
            tfr = Ring(S, "sb", "htf", [128, 512], F32, 2, ph)
            for hh in range(2):
                for (s, n) in TTILES:
                    sq, SQ = sqr.next()
                    act(sq[:, 0:n], oacc[:, hh, s:s + n], AF.Square, [OACC], [SQ])
                    ps, PS = pss.next()
                    mm(ps[:, 0:n], onb[:], sq[:, 0:n], True, True, [ONB, SQ], [PS])
                    rt, RT = rtr.next()
                    act(rt[:, 0:n], ps[:, 0:n], AF.Sqrt, [PS, EPSC], [RT], scale=1.0 / 128, bias=epsc[:, 0:1])
                    S.op("dve", lambda e, rt=rt, n=n: e.reciprocal(rt[:, 0:n], rt[:, 0:n]), [RT], [RT])
                    tf, TF = tfr.next()
                    stt("dve", tf[:, 0:n], oacc[:, hh, s:s + n], headnw[:, L.l, which:which + 1], rt[:, 0:n],
                        ALU.mult, ALU.mult, [OACC, HEADNW, RT], [TF])
                    tt("pool", yT[:, which * 4 + 2 * hp + hh, s:s + n], tf[:, 0:n], ZS[:, hh, s:s + n], ALU.mult,
                       [TF, ZSD], [YT])

        def small_cols(L, b, hT, HT, bp):
            P_ = NS()
            P_.beta, P_.BETA = S.sb("beta", [128, NT, 8], F32, bp)
            P_.la, P_.LA = S.sb("la", [128, NT, 8], F32, bp)
            P_.ecols, P_.ECOLS = S.sb("ecols", [128, NT, 16], F32, bp)
            P_.er, P_.ER = S.sb("er", [128, NT, 2, 8], F32, bp)
            with ExitStack() as ph:
                wb, WB = S.sb("wbs", [128, KD, 128], BF16, ph)
                S.dma("pool", wb[:], win_d[L.l, NGRP - 1], writes=[WB])
                ba, BA = S.sb("ba", [128, NT, 16], F32, ph)
                pr = PsRing(S, "pba", 2, 16, F32, ph)
                for i in range(NT):
                    ps, PS = pr.next()
                    for k in range(KD):
                        mm(ps, hT[:, k, i * 128:(i + 1) * 128], wb[:, k, 0:16], k == 0, k == KD - 1, [HT, WB], [PS])
                    cp("dve", ba[:, i, :], ps, [PS], [BA])
                act(P_.beta[:], ba[:, :, 0:8], AF.Sigmoid, [BA], [P_.BETA])
                tt("dve", P_.la[:], ba[:, :, 8:16], alog[:, L.l, 8:16].unsqueeze(1).to_broadcast([128, NT, 8]), ALU.add,
                   [BA, ALOG], [P_.LA])
                act(P_.la[:], P_.la[:], AF.Exp, [P_.LA], [P_.LA])
                act(P_.la[:], P_.la[:], AF.Ln, [P_.LA], [P_.LA], bias=1.0)
                tt("dve", P_.la[:], P_.la[:], alog[:, L.l, 0:8].unsqueeze(1).to_broadcast([128, NT, 8]), ALU.mult,
                   [P_.LA, ALOG], [P_.LA])
                pc = PsRing(S, "pcol", 2, 16, F32, ph)
                for i in range(NT):
                    ps, PS = pc.next()
                    for d in range(2):
                        INC, STR, NEG, GT = dmask(d)
                        STRO = dmask(1 - d)[1]
                        mm(ps[:, d * 4:d * 4 + 4], INC, P_.la[:, i, d * 4:d * 4 + 4], True, True, [CST, P_.LA], [PS])
                        mm(ps[:, 8 + d * 4:8 + d * 4 + 4], STRO, P_.la[:, i, d * 4:d * 4 + 4], True, True,
                           [CST, P_.LA], [PS])
                    act(P_.ecols[:, i, :], ps, AF.Exp, [PS], [P_.ECOLS])
                ts1("dve", P_.ecols[:, :, 0:8], P_.ecols[:, :, 0:8], -1.0, ALU.mult, [P_.ECOLS], [P_.ECOLS])
                ts1("dve", P_.er[:, :, 0, :], P_.ecols[:, :, 8:16], cst[:, C_INCF * 128 + 63:C_INCF * 128 + 64], ALU.mult,
                    [P_.ECOLS, CST], [P_.ER])
                ts1("dve", P_.er[:, :, 1, :], P_.ecols[:, :, 8:16], cst[:, C_INCB * 128 + 64:C_INCB * 128 + 65], ALU.mult,
                    [P_.ECOLS, CST], [P_.ER])
                S.barrier()
                S.build()
            return P_

        def gdn_headpair(L, b, hp, hT, HT, yT, YT, SC):
            l = L.l
            with ExitStack() as ph:
                QT, QTD = S.sb("QT", [128, 2, T], BF16, ph)
                KT, KTD = S.sb("KT", [128, 2, T], BF16, ph)
                VT, VTD = S.sb("VT", [128, 2, T], BF16, ph)
                ZS, ZSD = S.sb("ZS", [128, 2, T], BF16, ph)
                oacc, OACC = S.sb("oacc", [128, 2, T], F32, ph)
                S.op("pool", lambda e: e.memset(oacc[:], 0.0), (), [OACC])
                with ExitStack() as ph2:
                    rawr = Ring(S, "sb", "raw", [128, T], F32, 1, ph2)
                    caccr = Ring(S, "sb", "cacc", [128, T], F32, 1, ph2)
                    sq32, SQ32 = S.sb("sq32", [128, T], F32, ph2)
                    sqb, SQB = S.sb("sqb", [128, T], BF16, ph2)
                    rt, RT = S.sb("rt", [128, T], F32, ph2)
                    wring = Ring(S, "sb", "wb", [128, KD, 128], BF16, 3, ph2)
                    pin = PsRing(S, "pin", 3, 512, F32, ph2)
                    pss = PsRing(S, "pss", 2, 512, F32, ph2)
                    gi = 0
                    for kind, colbase in (("z", DN_Z), ("v", DN_V), ("q", DN_Q), ("k", DN_K)):
                        for hh in range(2):
                            h = 2 * hp + hh
                            g = colbase // 128 + h
                            if kind == "z":
                                inproj(L, hT, HT, wring, pin, g,
                                       lambda ps, PS, s, n, hh=hh: act(ZS[:, hh, s:s + n], ps[:, 0:n], AF.Silu, [PS], [ZSD]))
                                continue
                            raw, RAW = rawr.next()
                            cacc, CACC = caccr.next()
                            inproj(L, hT, HT, wring, pin, g,
                                   lambda ps, PS, s, n, raw=raw, RAW=RAW: cp("act", raw[:, s:s + n], ps[:, 0:n], [PS], [RAW]))
                            cg = {"q": 0, "k": 1, "v": 2}[kind] * 4 + h
                            conv("dve", raw, RAW, cacc, CACC,
                                 lambda tap, cg=cg: convw[:, l, cg, tap:tap + 1])
                            gi += 1
                            if kind == "v":
                                act(VT[:, hh, :], cacc[:], AF.Silu, [CACC], [VTD])
                                continue
                            act(sq32[:], cacc[:], AF.Silu, [CACC], [SQ32])
                            tt("pool", sqb[:], sq32[:], sq32[:], ALU.mult, [SQ32], [SQB])
                            for (s, n) in TTILES:
                                ps, PS = pss.next()
                                mm(ps[:, 0:n], onb[:], sqb[:, s:s + n], True, True, [ONB, SQB], [PS])
                                act(rt[:, s:s + n], ps[:, 0:n], AF.Sqrt, [PS, EPSC], [RT], bias=epsc[:, 0:1])
                            S.op("dve", lambda e: e.reciprocal(rt[:], rt[:]), [RT], [RT])
                            if kind == "q":
                                stt("dve", QT[:, hh, :], sq32[:], float(128 ** -0.5), rt[:], ALU.mult, ALU.mult,
                                    [SQ32, RT], [QTD])
                            else:
                                tt("dve", KT[:, hh, :], sq32[:], rt[:], ALU.mult, [SQ32, RT], [KTD])
                    if stop == "G1" and hp == 0 and b == 0 and l == 0:
                        for j, (tt_, TD_) in enumerate(((QT, QTD), (KT, KTD), (VT, VTD), (ZS, ZSD))):
                            for hh_ in range(2):
                                cp("act", yT[:, j * 2 + hh_, :], tt_[:, hh_, :], [TD_], [YT])
                        S.dma("pool", dbg_out["yT"].rearrange("p (k t) -> p k t", k=KD), yT[:], reads=[YT], writes=[Dep("dbg_yT1")])
                    S.barrier()
                    S.build()
                    if chk("G1"):
                        return
                with ExitStack() as ph3:
                    chains = [(hh, d) for hh in range(2) for d in range(2)]
                    pab = [S.ps(f"pa{j}", [128, 512], F32, ph3)[0] for j in range(4)]
                    pabd = [Dep(f"pa{j}", excl=True) for j in range(4)]
                    pbb = S.ps("pb16", [128, 1024], BF16, ph3)[0]
                    pbbd = Dep("pb16", excl=True)
                    psb = [S.ps(f"pst{j}", [128, 512], F32, ph3)[0] for j in range(2)]
                    psbd = [Dep(f"pst{j}", excl=True) for j in range(2)]

                    class Cyc:
                        def __init__(self, items):
                            self.items = items
                            self.i = 0

                        def next(self):
                            it = self.items[self.i % len(self.items)]
                            self.i += 1
                            return it
                    CH = []
                    for c in range(4):
                        o = NS()
                        o.pa = Cyc([(pab[j][:, c * 128:(c + 1) * 128], pabd[j]) for j in range(4)])
                        o.pb16 = Cyc([(pbb[:, (2 * c + j) * 128:(2 * c + j + 1) * 128], pbbd) for j in range(2)])
                        o.pstep = Cyc([(psb[0][:, c * 128:(c + 1) * 128], psbd[0])])
                        o.pout = Cyc([(psb[1][:, c * 128:(c + 1) * 128], psbd[1])])
                        o.Sf, o.SF = S.sb("Sf", [128, 128], F32, ph3)
                        o.Sb, o.SB = S.sb("Sb", [128, 128], BF16, ph3)
                        S.op("dve", lambda e, o=o: e.memset(o.Sf[:], 0.0), (), [o.SF])
                        S.op("dve", lambda e, o=o: e.memset(o.Sb[:], 0.0), (), [o.SB])
                        for nm, dt, n in (("lam", F32, 2), ("ecb", F32, 2), ("dec", F32, 2), ("decs", F32, 2),
                                          ("Qd", BF16, 2), ("qkm", BF16, 2), ("A", F32, 2), ("Bm", F32, 2),
                                          ("Pt", F32, 2), ("Pf", BF16, 2), ("kdec0", BF16, 2), ("kdec1", BF16, 2),
                                          ("vtok", BF16, 2), ("Y0", BF16, 1), ("vnew", BF16, 1)):
                            setattr(o, nm, Ring(S, "sb", nm, [128, 128], dt, n, ph3))
                        for rg in (o.Y0, o.vnew):
                            for (t_, TD_) in rg.items:
                                S.op("pool", lambda e, t_=t_: e.memset(t_[:], 0.0), (), [TD_])
                        CH.append(o)

                    def prep(c, i):
                        o = CH[c]
                        hh, d = chains[c]
                        h = 2 * hp + hh
                        dh = d * 4 + h
                        blk = slice(i * 128, (i + 1) * 128)
                        INC, STR, NEG, GT = dmask(d)
                        lam, LAM = o.lam.next()
                        ts1("pool", lam[:], INC, SC.la[:, i, dh:dh + 1], ALU.mult, [CST, SC.LA], [LAM])
                        pc, PC = o.pa.next()
                        mm(pc, C(C_ONES), lam[:], True, True, [CST, LAM], [PC])
                        pd, PD = o.pa.next()
                        mm(pd, GT, lam[:], True, False, [CST, LAM], [PD])
                        mm(pd, C(C_ID), NEG, False, True, [CST], [PD])
                        pkk, PKK = o.pa.next()
                        mm(pkk, KT[:, hh, blk], KT[:, hh, blk], True, True, [KTD], [PKK])
                        pqk, PQK = o.pa.next()
                        mm(pqk, KT[:, hh, blk], QT[:, hh, blk], True, True, [KTD, QTD], [PQK])
                        pt2, PT2 = o.pb16.next()
                        tr(pt2, KT[:, hh, blk], idb[:], [KTD, IDB], [PT2])
                        pt3, PT3 = o.pb16.next()
                        tr(pt3, VT[:, hh, blk], idb[:], [VTD, IDB], [PT3])
                        yield
                        ecb, ECB = o.ecb.next()
                        act(ecb[:], pc, AF.Exp, [PC], [ECB])
                        dec, DEC = o.dec.next()
                        act(dec[:], pd, AF.Exp, [PD], [DEC])
                        kdec0, KDEC0 = o.kdec0.next()
                        ts1("dve", kdec0[:], pt2, SC.er[:, i, 0, dh:dh + 1], ALU.mult, [PT2, SC.ER], [KDEC0])
                        kdec1, KDEC1 = o.kdec1.next()
                        ts1("dve", kdec1[:], pt2, SC.er[:, i, 1, dh:dh + 1], ALU.mult, [PT2, SC.ER], [KDEC1])
                        vtok, VTOK = o.vtok.next()
                        cp("act", vtok[:], pt3, [PT3], [VTOK])
                        yield
                        Qd, QD = o.Qd.next()
                        tt("pool", Qd[:], QT[:, hh, blk], ecb[:], ALU.mult, [QTD, ECB], [QD])
                        qkm, QKM = o.qkm.next()
                        tt("dve", qkm[:], pqk, dec[:], ALU.mult, [PQK, DEC], [QKM])
                        decs, DECS = o.decs.next()
                        tt("pool", decs[:], dec[:], STR, ALU.mult, [DEC, CST], [DECS])
                        A0, A0D = o.A.next()
                        stt("dve", A0[:], pkk, SC.beta[:, i, dh:dh + 1], decs[:], ALU.mult, ALU.mult,
                            [PKK, SC.BETA, DECS], [A0D])
                        yield
                        pt1, PT1 = o.pa.next()
                        tr(pt1, A0[:], C(C_ID), [A0D, CST], [PT1])
                        P0, P0D = o.Pt.next()
                        tt("pool", P0[:], C(C_ID), A0[:], ALU.subtract, [CST, A0D], [P0D])
                        yield
                        B0, B0D = o.Bm.next()
                        cp("act", B0[:], pt1, [PT1], [B0D])
                        yield
                        Ap, APD, Bp, BPD, Pp, PPD = A0, A0D, B0, B0D, P0, P0D
                        for lev in range(1, 6):
                            if lev < 5:
                                pA, PA_ = o.pa.next()
                                mm(pA, Bp[:], Ap[:], True, True, [BPD, APD], [PA_])
                            pB, PB_ = o.pa.next()
                            mm(pB, Ap[:], Bp[:], True, True, [APD, BPD], [PB_])
                            yield
                            if lev < 5:
                                An, AND_ = o.A.next()
                                cp("act", An[:], pA, [PA_], [AND_])
                            Bn, BND = o.Bm.next()
                            cp("dve", Bn[:], pB, [PB_], [BND])
                            yield
                            pP, PP_ = o.pa.next()
                            mm(pP, Bn[:], Pp[:], True, True, [BND, PPD], [PP_])
                            yield
                            Pn, PND = (o.Pf if lev == 5 else o.Pt).next()
                            tt("dve", Pn[:], pP, Pp[:], ALU.add, [PP_, PPD], [PND])
                            yield
                            if lev < 5:
                                Ap, APD = An, AND_
                            Bp, BPD, Pp, PPD = Bn, BND, Pn, PND
                        o.cur = dict(ecb=(ecb, ECB), Qd=(Qd, QD), qkm=(qkm, QKM), Pf=(Pp, PPD), kdec=((kdec0, KDEC0), (kdec1, KDEC1)),
                                     vtok=(vtok, VTOK))

                    def steps(c, i, cur):
                        o = CH[c]
                        hh, d = chains[c]
                        h = 2 * hp + hh
                        dh = d * 4 + h
                        blk = slice(i * 128, (i + 1) * 128)
                        ecb, ECB = cur["ecb"]
                        Qd, QD = cur["Qd"]
                        qkm, QKM = cur["qkm"]
                        Pf, PFD = cur["Pf"]
                        vtok, VTOK = cur["vtok"]
                        po, PO = o.pout.next()
                        Y0, Y0D = o.Y0.next()
                        vnew, VNEW = o.vnew.next()
                        for ch in ((0, 1) if d == 0 else (1, 0)):
                            rows = slice(ch * 64, ch * 64 + 64)
                            pks, PKS = o.pstep.next()
                            mm(pks, KT[:, hh, blk], o.Sb[:], True, True, [KTD, o.SB], [PKS])
                            yield
                            stt("dve", Y0[rows, :], pks[rows, :], SC.ecols[rows, i, dh:dh + 1], vtok[rows, :],
                                ALU.mult, ALU.add, [PKS, SC.ECOLS, VTOK], [Y0D])
                            yield
                            pz, PZ = o.pstep.next()
                            mm(pz, Pf[:, :], Y0[:, :], True, True, [PFD, Y0D], [PZ])
                            yield
                            act(vnew[rows, :], pz[rows, :], AF.Copy, [PZ, SC.BETA], [VNEW], scale=SC.beta[rows, i, dh:dh + 1])
                            yield
                            mm(po[:, rows], o.Sb[:], Qd[:, rows], True, False, [o.SB, QD], [PO])
                            mm(po[:, rows], vnew[:, :], qkm[:, rows], False, True, [VNEW, QKM], [PO])
                            pds, PDS = o.pstep.next()
                            kdec, KDEC = cur["kdec"][ch]
                            mm(pds, kdec[:, :], vnew[:, :], True, True, [KDEC, VNEW], [PDS])
                            yield
                            gc = (ch * 64 + 63) if d == 0 else ch * 64
                            stt("dve", o.Sf[:], o.Sf[:], ecb[:, gc:gc + 1], pds, ALU.mult, ALU.add, [o.SF, ECB, PDS], [o.SF])
                            yield
                            cp("act", o.Sb[:], o.Sf[:], [o.SF], [o.SB])
                            yield
                        tt("dve", oacc[:, hh, blk], oacc[:, hh, blk], po, ALU.add, [OACC, PO], [OACC])

                    roundrobin([prep(c, ORDER[chains[c][1]][0]) for c in range(4)])
                    if chk("G2"):
                        return
                    for n in range(NT):
                        curs = [CH[c].cur for c in range(4)]
                        gens = [steps(c, ORDER[chains[c][1]][n], curs[c]) for c in range(4)]
                        if n + 1 < NT:
                            gens += [prep(c, ORDER[chains[c][1]][n + 1]) for c in range(4)]
                        roundrobin(gens)
                        if n == 0 and chk("G3"):
                            return
                    S.barrier()
                    S.build()
                    if chk("G4"):
                        return
                with ExitStack() as ph4:
                    pss = PsRing(S, "pss", 2, 512, F32, ph4)
                    headnorm(ph4, L, oacc, OACC, ZS, ZSD, 1, hp, yT, YT, pss)
                    S.barrier()
                    S.build()

        def hgrn_headpair(L, b, hp, hT, HT, yT, YT):
            l = L.l
            with ExitStack() as ph:
                qs, QS = S.sb("qs", [128, 2, T], BF16, ph)
                VT, VTD = S.sb("VTh", [128, 2, T], BF16, ph)
                ZS, ZSD = S.sb("ZSh", [128, 2, T], BF16, ph)
                oacc, OACC = S.sb("oacch", [128, 2, T], F32, ph)
                S.op("pool", lambda e: e.memset(oacc[:], 0.0), (), [OACC])
                chains = [(hh, d) for hh in range(2) for d in range(2)]
                CH = []
                for c in range(4):
                    o = NS()
                    o.qd, o.QD = S.sb("qd", [128, T], BF16, ph)
                    o.kd, o.KD = S.sb("kd", [128, T], BF16, ph)
                    o.gch, o.GCH = S.sb("gch", [128, T // 32], F32, ph)
                    CH.append(o)
                with ExitStack() as ph2:
                    tA, TA = S.sb("tA", [128, T], F32, ph2)
                    tB, TB = S.sb("tB", [128, T], F32, ph2)
                    tC, TCD = S.sb("tC", [128, T], F32, ph2)
                    rst, RST = S.sb("rst", [128, T], BF16, ph2)
                    S.op("pool", lambda e: e.memset(rst[:], 1.0), (), [RST])
                    S.op("pool", lambda e: e.memset(rst[:].rearrange("p (c k) -> p c k", k=32)[:, :, 0:1], 0.0), (), [RST])
                    wring = Ring(S, "sb", "wbh", [128, KD, 128], BF16, 2, ph2)
                    pin = PsRing(S, "pinh", 4, 512, F32, ph2)
                    for hh in range(2):
                        h = 2 * hp + hh
                        inproj(L, hT, HT, wring, pin, HG_Q // 128 + h,
                               lambda ps, PS, s, n, hh=hh: act(qs[:, hh, s:s + n], ps[:, 0:n], AF.Silu, [PS], [QS]))
                        inproj(L, hT, HT, wring, pin, HG_I // 128 + h,
                               lambda ps, PS, s, n, hh=hh: cp("dve", VT[:, hh, s:s + n], ps[:, 0:n], [PS], [VTD]))
                        inproj(L, hT, HT, wring, pin, HG_G // 128 + h,
                               lambda ps, PS, s, n, hh=hh: act(ZS[:, hh, s:s + n], ps[:, 0:n], AF.Silu, [PS], [ZSD]))
                    for c in range(4):
                        o = CH[c]
                        hh, d = chains[c]
                        h = 2 * hp + hh
                        dh = d * 4 + h
                        inproj(L, hT, HT, wring, pin, (HG_FF if d == 0 else HG_FB) // 128 + h,
                               lambda ps, PS, s, n: act(tA[:, s:s + n], ps[:, 0:n], AF.Sigmoid, [PS], [TA]))
                        ts2("dve", tA[:], tA[:], oml[:, l, dh:dh + 1], lbt[:, l, dh:dh + 1], ALU.mult, ALU.add,
                            [TA, OML, LBT], [TA])
                        act(tB[:], tA[:], AF.Ln, [TA], [TB])
                        ts2("dve", tA[:], tA[:], -1.0, 1.0, ALU.mult, ALU.add, [TA], [TA])
                        S.op("dve", lambda e: e.tensor_tensor_scan(tC[:], rst[:], tB[:], 0.0, ALU.mult, ALU.add),
                             [RST, TB], [TCD])
                        if d == 0:
                            cum, CUM, oth, OTH = tC, TCD, tB, TB
                            gcol = 31
                        else:
                            tt("pool", tB[:], tB[:], tC[:], ALU.subtract, [TB, TCD], [TB])
                            tB3 = tB[:].rearrange("p (c k) -> p c k", k=32)
                            tC3 = tC[:].rearrange("p (c k) -> p c k", k=32)
                            tt("dve", tB3, tB3, tC3[:, :, 31:32].to_broadcast([128, T // 32, 32]), ALU.add,
                               [TB, TCD], [TB])
                            cum, CUM, oth, OTH = tB, TB, tC, TCD
                            gcol = 0
                        act(oth[:], cum[:], AF.Exp, [CUM], [OTH])
                        tt("pool", o.qd[:], qs[:, hh, :], oth[:], ALU.mult, [QS, OTH], [o.QD])
                        cp("dve", o.gch[:], oth[:].rearrange("p (c k) -> p c k", k=32)[:, :, gcol], [OTH], [o.GCH])
                        act(oth[:], cum[:], AF.Exp, [CUM, o.QD, o.GCH], [OTH], scale=-1.0)
                        tt("dve", o.kd[:], tA[:], oth[:], ALU.mult, [TA, OTH], [o.KD])
                    S.barrier()
                    S.build()
                with ExitStack() as ph3:
                    pa = PsRing(S, "pah", 2, 128, F32, ph3)
                    pb16 = PsRing(S, "pb16h", 1, 128, BF16, ph3)
                    pstep = PsRing(S, "psteph", 2, 128, F32, ph3)
                    pout = PsRing(S, "pouth", 2, 128, F32, ph3)
                    for c in range(4):
                        o = CH[c]
                        o.Sf, o.SF = S.sb("Sfh", [128, 128], F32, ph3)
                        o.Sb, o.SB = S.sb("Sbh", [128, 128], BF16, ph3)
                        o.tS, o.TS = S.sb("tSh", [128, 128], F32, ph3)
                        S.op("dve", lambda e, o=o: e.memset(o.Sf[:], 0.0), (), [o.SF])
                        S.op("dve", lambda e, o=o: e.memset(o.Sb[:], 0.0), (), [o.SB])
                        for nm, dt, n in (("kdtok0", BF16, 2), ("kdtok1", BF16, 2), ("vtok", BF16, 2), ("attm", BF16, 2)):
                            setattr(o, nm, Ring(S, "sb", nm + "h", [128, 128], dt, n, ph3))

                    NBH = T // 64
                    ORDH = {0: list(range(NBH)), 1: [3, 2, 1, 0] + list(range(NBH - 1, 3, -1))}

                    def prep(c, i):
                        o = CH[c]
                        hh, d = chains[c]
                        blk = slice(i * 64, (i + 1) * 64)
                        INC = (cst[0:64, C_INCF32 * 128:C_INCF32 * 128 + 64] if d == 0 else
                               cst[0:64, C_INCB32 * 128:C_INCB32 * 128 + 64])
                        pt1, PT1 = pb16.next()
                        tr(pt1[0:64, :], o.kd[:, blk], idb[:], [o.KD, IDB], [PT1])
                        pt2, PT2 = pb16.next()
                        tr(pt2[0:64, :], VT[:, hh, blk], idb[:], [VTD, IDB], [PT2])
                        pat, PAT = pa.next()
                        mm(pat[0:64, 0:64], o.kd[:, blk], o.qd[:, blk], True, True, [o.KD, o.QD], [PAT])
                        yield
                        kdtok0, KDTOK0 = o.kdtok0.next()
                        act(kdtok0[0:64, :], pt1[0:64, :], AF.Copy, [PT1, CST], [KDTOK0],
                            scale=cst[0:64, C_INCF32 * 128 + 31:C_INCF32 * 128 + 32])
                        kdtok1, KDTOK1 = o.kdtok1.next()
                        act(kdtok1[0:64, :], pt1[0:64, :], AF.Copy, [PT1, CST], [KDTOK1],
                            scale=cst[0:64, C_INCB32 * 128 + 32:C_INCB32 * 128 + 33])
                        vtok, VTOK = o.vtok.next()
                        cp("act", vtok[0:64, :], pt2[0:64, :], [PT2], [VTOK])
                        attm, ATTM = o.attm.next()
                        tt("dve", attm[0:64, 0:64], pat[0:64, 0:64], INC, ALU.mult, [PAT, CST], [ATTM])
                        yield
                        o.cur = dict(kdtok=((kdtok0, KDTOK0), (kdtok1, KDTOK1)), vtok=(vtok, VTOK), attm=(attm, ATTM))

                    def steps(c, i, cur):
                        o = CH[c]
                        hh, d = chains[c]
                        blk0 = i * 64
                        vtok, VTOK = cur["vtok"]
                        attm, ATTM = cur["attm"]
                        po, PO = pout.next()
                        for ch in ((0, 1) if d == 0 else (1, 0)):
                            rows = slice(ch * 32, ch * 32 + 32)
                            cols = slice(blk0 + ch * 32, blk0 + ch * 32 + 32)
                            mm(po[:, rows], o.Sb[:], o.qd[:, cols], True, False, [o.SB, o.QD], [PO])
                            mm(po[:, rows], vtok[0:64, :], attm[0:64, rows], False, True, [VTOK, ATTM], [PO])
                            pds, PDS = pstep.next()
                            kdtok, KDTOK = cur["kdtok"][ch]
                            mm(pds, kdtok[0:64, :], vtok[0:64, :], True, True, [KDTOK, VTOK], [PDS])
                            yield
                            tt("dve", o.tS[:], o.Sf[:], pds, ALU.add, [o.SF, PDS], [o.TS])
                            yield
                            ci = i * 2 + ch
                            act(o.Sb[:], o.tS[:], AF.Copy, [o.TS, o.GCH], [o.SB], scale=o.gch[:, ci:ci + 1])
                            ts1("dve", o.Sf[:], o.tS[:], o.gch[:, ci:ci + 1], ALU.mult, [o.TS, o.GCH], [o.SF])
                            yield
                        tt("dve", oacc[:, hh, blk0:blk0 + 64], oacc[:, hh, blk0:blk0 + 64], po[:, 0:64], ALU.add,
                           [OACC, PO], [OACC])

                    roundrobin([prep(c, ORDH[chains[c][1]][0]) for c in range(4)])
                    for n in range(NBH):
                        curs = [CH[c].cur for c in range(4)]
                        gens = [steps(c, ORDH[chains[c][1]][n], curs[c]) for c in range(4)]
                        if n + 1 < NBH:
                            gens += [prep(c, ORDH[chains[c][1]][n + 1]) for c in range(4)]
                        roundrobin(gens)
                    S.barrier()
                    S.build()
                with ExitStack() as ph4:
                    pss = PsRing(S, "pssh", 2, 512, F32, ph4)
                    headnorm(ph4, L, oacc, OACC, ZS, ZSD, 0, hp, yT, YT, pss)
                    S.barrier()
                    S.build()

        def outproj_phase(L, b, src, SRC, mid, MID, yT, YT, tstart):
            with ExitStack() as ph:
                wo, WO = S.sb("wo", [128, KD, D], BF16, ph)
                S.dma("pool", wo[:], wout_d[L.l].rearrange("(k p) n -> p k n", p=128), writes=[WO])
                pin = PsRing(S, "pino", 4, 512, F32, ph)
                Gx, GX = gbcast(ph, L, b, 0, pin)
                Gc, GC = (None, None) if tstart > 0 else gbcast(ph, L, 2, 0, pin)
                xr = Ring(S, "sb", "xo", [128, D], F32, 3, ph)
                tr_ = Ring(S, "sb", "to", [128, D], F32, 2, ph)
                for i in range(tstart, NT):
                    x, X = xr.next()
                    S.dma("sp", x[:], src[b, i * 128:(i + 1) * 128, :], reads=[SRC], writes=[X])
                    G, GD = (Gc, GC) if i < 2 else (Gx, GX)
                    t_, TD_ = tr_.next()
                    for hf in range(2):
                        ps, PS = pin.next()
                        for k in range(KD):
                            mm(ps[:, :], yT[:, k, i * 128:(i + 1) * 128], wo[:, k, hf * 512:(hf + 1) * 512],
                               k == 0, k == KD - 1, [YT, WO], [PS])
                        tt("dve", t_[:, hf * 512:(hf + 1) * 512], ps[:, :], G[:, hf * 512:(hf + 1) * 512], ALU.mult,
                           [PS, GD], [TD_])
                    tt("pool", x[:], x[:], t_[:], ALU.add, [X, TD_], [X])
                    S.dma("sp", mid[b, i * 128:(i + 1) * 128, :], x[:], reads=[X], writes=[MID])
                S.barrier()
                S.build()

        def moe_phase(L, b, mid, MID, dst, DST, last, tstart):
            l = L.l
            ntm = NT - tstart
            with ExitStack() as bp:
                hT, HT = S.sb("hT2", [128, KD, T], BF16, bp)
                gT, GT_ = S.sb("gT", [16, T], F32, bp)
                norm_phase(L, b, mid, MID, 1, hT, HT, tstart)
                with ExitStack() as ph:
                    lg, LG = S.sb("lg", [128, ntm, 20], F32, ph)
                    pr = PsRing(S, "plg", 2, 32, F32, ph)
                    for ii in range(ntm):
                        i = tstart + ii
                        ps, PS = pr.next()
                        for k in range(KD):
                            mm(ps[:, 0:20], hT[:, k, i * 128:(i + 1) * 128], L.wr[:, k, :], k == 0, False, [HT, L.WR], [PS])
                        mm(ps[:, 0:20], cst[0:1, C_ONES * 128:C_ONES * 128 + 128], L.brow[0:1, :], False, True,
                           [CST, L.BROW], [PS])
                        cp("dve", lg[:, ii, :], ps[:, 0:20], [PS], [LG])

                    def sbt(name, shape):
                        return S.sb(name, shape, F32, ph)
                    gl = lg[:, :, 0:4]
                    el = lg[:, :, 4:20]
                    gmax, GMAX = sbt("gmax", [128, ntm, 1])
                    gmask, GMASK = sbt("gmask", [128, ntm, 4])
                    gex, GEX = sbt("gex", [128, ntm, 4])
                    gw, GW = sbt("gw", [128, ntm, 1])
                    elm, ELM = sbt("elm", [128, ntm, 16])
                    m1, M1 = sbt("m1", [128, ntm, 1])
                    m2, M2 = sbt("m2", [128, ntm, 1])
                    mk1, MK1 = sbt("mk1", [128, ntm, 16])
                    mk2, MK2 = sbt("mk2", [128, ntm, 16])
                    w1, W1 = sbt("w1", [128, ntm, 1])
                    w2, W2 = sbt("w2", [128, ntm, 1])
                    gates, GATES = sbt("gates", [128, ntm, 16])
                    BIG = 1.0e4
                    S.op("dve", lambda e: e.tensor_reduce(gmax[:], gl, AX.X, ALU.max), [LG], [GMAX])
                    tt("dve", gmask[:], gl, gmax[:].to_broadcast([128, ntm, 4]), ALU.is_ge, [LG, GMAX], [GMASK])
                    tt("dve", gex[:], gl, gmax[:].to_broadcast([128, ntm, 4]), ALU.subtract, [LG, GMAX], [GEX])
                    act(gex[:], gex[:], AF.Exp, [GEX], [GEX])
                    S.op("dve", lambda e: e.tensor_reduce(gw[:], gex[:], AX.X, ALU.add), [GEX], [GW])
                    S.op("dve", lambda e: e.reciprocal(gw[:], gw[:]), [GW], [GW])
                    ts2("dve", gmask[:], gmask[:], BIG, -BIG, ALU.mult, ALU.add, [GMASK], [GMASK])
                    tt("dve", elm[:].rearrange("p n (g e) -> p n g e", e=4), el.rearrange("p n (g e) -> p n g e", e=4),
                       gmask[:].unsqueeze(3).to_broadcast([128, ntm, 4, 4]), ALU.add, [LG, GMASK], [ELM])
                    S.op("dve", lambda e: e.tensor_reduce(m1[:], elm[:], AX.X, ALU.max), [ELM], [M1])
                    tt("dve", mk1[:], elm[:], m1[:].to_broadcast([128, ntm, 16]), ALU.is_ge, [ELM, M1], [MK1])
                    stt("dve", elm[:], mk1[:], -BIG, elm[:], ALU.mult, ALU.add, [MK1, ELM], [ELM])
                    S.op("dve", lambda e: e.tensor_reduce(m2[:], elm[:], AX.X, ALU.max), [ELM], [M2])
                    tt("dve", mk2[:], elm[:], m2[:].to_broadcast([128, ntm, 16]), ALU.is_ge, [ELM, M2], [MK2])
                    tt("dve", w2[:], m2[:], m1[:], ALU.subtract, [M1, M2], [W2])
                    act(w2[:], w2[:], AF.Exp, [W2], [W2])
                    ts1("dve", w1[:], w2[:], 1.0, ALU.add, [W2], [W1])
                    S.op("dve", lambda e: e.reciprocal(w1[:], w1[:]), [W1], [W1])
                    tt("dve", w2[:], w2[:], w1[:], ALU.mult, [W1, W2], [W2])
                    tt("dve", w1[:], w1[:], gw[:], ALU.mult, [W1, GW], [W1])
                    tt("dve", w2[:], w2[:], gw[:], ALU.mult, [W2, GW], [W2])
                    tt("dve", mk1[:], mk1[:], w1[:].to_broadcast([128, ntm, 16]), ALU.mult, [MK1, W1], [MK1])
                    tt("dve", mk2[:], mk2[:], w2[:].to_broadcast([128, ntm, 16]), ALU.mult, [MK2, W2], [MK2])
                    tt("dve", gates[:], mk1[:], mk2[:], ALU.add, [MK1, MK2], [GATES])
                    pg = PsRing(S, "pgt", 2, 128, F32, ph)
                    for ii in range(ntm):
                        i = tstart + ii
                        ps, PS = pg.next()
                        tr(ps[0:16, :], gates[:, ii, :], C(C_ID), [GATES, CST], [PS])
                        cp("act", gT[0:16, i * 128:(i + 1) * 128], ps[0:16, :], [PS], [GT_])
                    S.barrier()
                    S.build()
                acc, ACC = S.sb("acc", [128, NT, D], F32, bp)
                tiles = [tt_ for tt_ in TTILES if tt_[0] >= tstart * 128]
                with ExitStack() as ph:
                    wgr = Ring(S, "sb", "wg", [128, KD, FF], BF16, 2, ph)
                    wur = Ring(S, "sb", "wu", [128, KD, FF], BF16, 2, ph)
                    wdr = Ring(S, "sb", "wd", [128, 4, D], BF16, 2, ph)
                    gselr = Ring(S, "sb", "gsel", [16, 512], F32, 2, ph)
                    gbr = Ring(S, "sb", "gbs", [128, 512], F32, 2, ph)
                    sgr = Ring(S, "sb", "sg", [128, 512], F32, 2, ph)
                    t1r = Ring(S, "sb", "t1", [128, 512], F32, 2, ph)
                    atr = Ring(S, "sb", "aT", [128, 4, 512], BF16, 2, ph)
                    pin = PsRing(S, "pinm", 4, 512, F32, ph)
                    pyr = PsRing(S, "pym", 2, 512, F32, ph)
                    pgb = PsRing(S, "pgb", 1, 512, F32, ph)
                    for e_ in range(NE):
                        wg, WG = wgr.next()
                        wu, WU = wur.next()
                        wd, WD = wdr.next()
                        S.dma("pool", wg[:], wg_d[l, e_].rearrange("(k p) f -> p k f", p=128), writes=[WG])
                        S.dma("pool", wu[:], wu_d[l, e_].rearrange("(k p) f -> p k f", p=128), writes=[WU])
                        S.dma("pool", wd[:], wd_d[l, e_].rearrange("(k p) n -> p k n", p=128), writes=[WD])
                        for (s, n) in tiles:
                            gsel, GSEL = gselr.next()
                            ts1("pool", gsel[0:16, 0:n], gT[0:16, s:s + n], cst[0:16, C_ID * 128 + e_:C_ID * 128 + e_ + 1],
                                ALU.mult, [GT_, CST], [GSEL])
                            pb_, PB_ = pgb.next()
                            mm(pb_[:, 0:n], cst[0:16, C_ONES * 128:C_ONES * 128 + 128], gsel[0:16, 0:n], True, True,
                               [CST, GSEL], [PB_])
                            gbs, GBS = gbr.next()
                            cp("act", gbs[:, 0:n], pb_[:, 0:n], [PB_], [GBS])
                            aT, AT = atr.next()
                            for f in range(4):
                                pg_, PG_ = pin.next()
                                for k in range(KD):
                                    mm(pg_[:, 0:n], wg[:, k, f * 128:(f + 1) * 128], hT[:, k, s:s + n], k == 0, k == KD - 1,
                                       [WG, HT], [PG_])
                                pu_, PU_ = pin.next()
                                for k in range(KD):
                                    mm(pu_[:, 0:n], wu[:, k, f * 128:(f + 1) * 128], hT[:, k, s:s + n], k == 0, k == KD - 1,
                                       [WU, HT], [PU_])
                                sg, SG = sgr.next()
                                act(sg[:, 0:n], pg_[:, 0:n], AF.Silu, [PG_], [SG])
                                t1, T1 = t1r.next()
                                tt("dve", t1[:, 0:n], pu_[:, 0:n], sg[:, 0:n], ALU.mult, [PU_, SG], [T1])
                                tt("pool", aT[:, f, 0:n], t1[:, 0:n], gbs[:, 0:n], ALU.mult, [T1, GBS], [AT])
                            for j in range(n // 128):
                                i = s // 128 + j
                                for hf in range(2):
                                    py, PY = pyr.next()
                                    for f in range(4):
                                        mm(py[:, :], aT[:, f, j * 128:(j + 1) * 128], wd[:, f, hf * 512:(hf + 1) * 512],
                                           f == 0, f == 3, [AT, WD], [PY])
                                    a_ = acc[:, i, hf * 512:(hf + 1) * 512]
                                    if e_ == 0:
                                        cp("act", a_, py[:, :], [PY], [ACC])
                                    else:
                                        tt("dve", a_, a_, py[:, :], ALU.add, [ACC, PY], [ACC])
                    S.barrier()
                    S.build()
                with ExitStack() as ph:
                    pin = PsRing(S, "pinr", 2, 512, F32, ph)
                    Gx, GX = gbcast(ph, L, b, 1, pin)
                    Gc, GC = (None, None) if tstart > 0 else gbcast(ph, L, 2, 1, pin)
                    xr = Ring(S, "sb", "xm", [128, D], F32, 3, ph)
                    tr_ = Ring(S, "sb", "tm", [128, D], F32, 2, ph)
                    if last:
                        fnw, FNW = S.sb("fnw", [128, D], F32, ph)
                        S.dma("sp", fnw[:], fnw_d, writes=[FNW])
                        junk, JUNK = S.sb("junkf", [128, D], BF16, ph)
                        str_ = Ring(S, "sb", "stf", [128, 4], F32, 3, ph)
                    for i in range(tstart, NT):
                        x, X = xr.next()
                        S.dma("sp", x[:], mid[b, i * 128:(i + 1) * 128, :], reads=[MID], writes=[X])
                        G, GD = (Gc, GC) if i < 2 else (Gx, GX)
                        t_, TD_ = tr_.next()
                        tt("dve", t_[:], acc[:, i, :], G[:], ALU.mult, [ACC, GD], [TD_])
                        tt("pool", x[:], x[:], t_[:], ALU.add, [X, TD_], [X])
                        if not last:
                            S.dma("sp", dst[b, i * 128:(i + 1) * 128, :], x[:], reads=[X], writes=[DST])
                        else:
                            st, ST = str_.next()
                            act(junk[:], x[:], AF.Square, [X], [JUNK, ST], accum_out=st[:, 0:1])
                            act(st[:, 1:2], st[:, 0:1], AF.Sqrt, [ST, EPSC], [ST], scale=1.0 / D, bias=epsc[:, 0:1])
                            S.op("dve", lambda e, st=st: e.reciprocal(st[:, 2:3], st[:, 1:2]), [ST], [ST])
                            stt("dve", t_[:], x[:], st[:, 2:3], fnw[:], ALU.mult, ALU.mult, [X, ST, FNW], [TD_])
                            S.dma("sp", out_d[b, (i - 2) * 128:(i - 1) * 128, :], t_[:], reads=[TD_], writes=[ROUT[b]])
                    S.barrier()
                    S.build()

        def run_pass(L, b, last, tstart, src, SRC, mid, MID, dst, DST):
            with ExitStack() as bp:
                hT, HT = S.sb("hT", [128, KD, T], BF16, bp)
                yT, YT = S.sb("yT", [128, KD, T], BF16, bp)
                norm_phase(L, b, src, SRC, 0, hT, HT, 0)
                if "hT" in dbg_out and L.l == 0 and b == 0:
                    dd = Dep("dbg_hT")
                    S.dma("pool", dbg_out["hT"].rearrange("p (k t) -> p k t", k=KD), hT[:], reads=[HT], writes=[dd])
                if stop == "A":
                    S.barrier()
                    S.build()
                    return
                with ExitStack() as scs:
                    SC = small_cols(L, b, hT, HT, scs)
                    if chk("SC"):
                        return
                    for hp in range(2):
                        gdn_headpair(L, b, hp, hT, HT, yT, YT, SC)
                        if STOPPED[0]:
                            return
                    S.barrier()
                    S.build()
                for hp in range(2):
                    hgrn_headpair(L, b, hp, hT, HT, yT, YT)
                    if STOPPED[0]:
                        return
                if "yT" in dbg_out and L.l == 0 and b == 0:
                    dd = Dep("dbg_yT")
                    S.dma("pool", dbg_out["yT"].rearrange("p (k t) -> p k t", k=KD), yT[:], reads=[YT], writes=[dd])
                if stop == "Y":
                    S.barrier()
                    S.build()
                    return
                outproj_phase(L, b, src, SRC, mid, MID, yT, YT, tstart)
            if stop == "M":
                return
            moe_phase(L, b, mid, MID, dst, DST, last, tstart)

        RXIN = Dep("rxin")
        RA = [Dep(f"resA{b}") for b in range(NB)]
        RB = [Dep(f"resB{b}") for b in range(NB)]
        ROUT = [Dep(f"rout{b}") for b in range(NB)]

        for l in range(nlayers):
          try:
            last = (l == DEPTH - 1)
            tstart = 2 if last else 0
            with ExitStack() as lay:
                L = NS()
                L.l = l
                L.modT, L.MODT = S.sb("modT", [128, 48, 3], F32, lay)
                L.abc, L.ABC = S.sb("abc", [128, 4, KD, 3], F32, lay)
                L.wr, L.WR = S.sb("wr", [128, KD, 20], BF16, lay)
                L.brow, L.BROW = S.sb("brow", [1, 20], F32, lay)
                S.dma("pool", L.wr[:], wr_d[l], writes=[L.WR])
                S.dma("sp", L.brow[:], br_d[l:l + 1, :], writes=[L.BROW])
                with ExitStack() as ph:
                    wmr = Ring(S, "sb", "wm", [128, KD, 512], F32, 2, ph)
                    bmr = Ring(S, "sb", "bm", [1, 512], F32, 2, ph)
                    pst = PsRing(S, "pmT", 2, 16, F32, ph)
                    for n in range(12):
                        wm, WM = wmr.next()
                        bm, BM = bmr.next()
                        S.dma("sp", wm[:], wmod_d[l].rearrange("(k p) n -> p k n", p=128)[:, :, n * 512:(n + 1) * 512],
                              writes=[WM])
                        S.dma("sp", bm[:], bmod_d[l:l + 1, n * 512:(n + 1) * 512], writes=[BM])
                        pt, PT = pst.next()
                        for sub in range(4):
                            o = pt[:, sub * 3:sub * 3 + 3]
                            for k in range(KD):
                                mm(o, wm[:, k, sub * 128:(sub + 1) * 128], scT[:, k, :], k == 0, False,
                                   [WM, SCT], [PT])
                            mm(o, bm[0:1, sub * 128:(sub + 1) * 128], cst[0:1, C_ONES * 128:C_ONES * 128 + 3],
                               False, True, [BM, CST], [PT])
                        cp("dve", L.modT[:, n * 4:(n + 1) * 4, :], pt[:, 0:12].rearrange("p (a b) -> p a b", b=3),
                           [PT], [L.MODT])
                    for wi, (csc, csh) in enumerate(((8, 0), (32, 24))):
                        ts1("dve", L.abc[:, 2 * wi, :, :], L.modT[:, csc:csc + 8, :], 1.0, ALU.add, [L.MODT], [L.ABC])
                        tt("dve", L.abc[:, 2 * wi, :, :], L.abc[:, 2 * wi, :, :],
                           normw[:, l, wi, :].unsqueeze(2).to_broadcast([128, KD, 3]), ALU.mult,
                           [L.ABC, NORMW], [L.ABC])
                        cp("dve", L.abc[:, 2 * wi + 1, :, :], L.modT[:, csh:csh + 8, :], [L.MODT], [L.ABC])
                    if "abc" in dbg_out and l == 0:
                        S.dma("sp", dbg_out["abc"], L.abc[:].rearrange("p a b c -> p (a b c)"), reads=[L.ABC], writes=[Dep("dbg_abc")])
                        S.dma("sp", dbg_out["modT"], L.modT[:].rearrange("p a b -> p (a b)"), reads=[L.MODT], writes=[Dep("dbg_modT")])
                        S.dma("sp", dbg_out["scT"], scT[:].rearrange("p a b -> p (a b)"), reads=[SCT], writes=[Dep("dbg_scT")])
                    S.barrier()
                    S.build()
                for b in range(nb_run):
                    src, SRC = (xin, RXIN) if l == 0 else (resB, RB[b])
                    run_pass(L, b, last, tstart, src, SRC, resA, RA[b], resB, RB[b])
                    if STOPPED[0]:
                        break
          except _Stop:
            break
          if STOPPED[0]:
            break
        if "res_out" in dbg_out:
            dd = Dep("dbg_res")
            for b in range(nb_run):
                if stop == "M":
                    S.dma("sp", dbg_out["res_out"][b], resA[b], reads=[RA[b]], writes=[dd])
                else:
                    S.dma("sp", dbg_out["res_out"][b], resB[b], reads=[RB[b]], writes=[dd])
        S.barrier()
        S.build()
    return nc


def make_consts():
    c = np.zeros((128, NCONST), np.float32)
    idx = np.arange(128)
    s = idx[:, None]
    t = idx[None, :]
    same = (s // 64) == (t // 64)

    def put(i, m):
        c[:, i * 128:(i + 1) * 128] = m.astype(np.float32)
    put(C_ID, s == t)
    put(C_ONES, np.ones((128, 128)))
    incf = same & (s <= t)
    incb = same & (s >= t)
    put(C_INCF, incf)
    put(C_STRF, same & (s < t))
    put(C_INCB, incb)
    put(C_STRB, same & (s > t))
    put(C_NEGF, np.where(incf, 0.0, -30000.0))
    put(C_NEGB, np.where(incb, 0.0, -30000.0))
    put(C_GTF, s > t)
    put(C_GTB, s < t)
    same32 = (s // 32) == (t // 32)
    put(C_INCF32, same32 & (s <= t))
    put(C_INCB32, same32 & (s >= t))
    return c


def prepare_shared(inp):
    f = np.float32
    sh = {}
    sh["consts"] = make_consts()
    sh["w_mod"] = np.ascontiguousarray(inp["w_mod"], dtype=f)
    sh["b_mod"] = np.ascontiguousarray(inp["b_mod"], dtype=f)
    nw = np.stack([inp["norm1_w"], inp["norm2_w"]], axis=1)
    sh["normw"] = np.ascontiguousarray(nw.reshape(DEPTH, 2, KD, 128).transpose(3, 0, 1, 2).reshape(128, -1), dtype=f)
    lb = inp["hg_lb"].reshape(DEPTH, 2, 4, 128)
    sh["lbh"] = np.ascontiguousarray(lb.transpose(3, 0, 1, 2).reshape(128, -1), dtype=f)
    hn = np.stack([inp["hg_norm_w"], inp["dn_norm_w"]], axis=1)
    sh["headnw"] = np.ascontiguousarray(hn.transpose(2, 0, 1).reshape(128, -1), dtype=f)
    cw = inp["dn_conv_w"].reshape(DEPTH, 9, 12, 128)
    sh["convw"] = np.ascontiguousarray(cw.transpose(3, 0, 2, 1).reshape(128, -1), dtype=f)
    al = np.concatenate([inp["dn_a_log"].reshape(DEPTH, 8), inp["dn_dt_bias"].reshape(DEPTH, 8)], axis=1)
    sh["alog"] = np.ascontiguousarray(np.broadcast_to(al.reshape(1, -1), (128, DEPTH * 16)), dtype=f)
    wrr = np.concatenate([inp["w_group"], inp["w_expert"]], axis=2)
    sh["wr"] = np.ascontiguousarray(wrr.reshape(DEPTH, KD, 128, 20).transpose(0, 2, 1, 3), dtype=f)
    sh["br"] = np.ascontiguousarray(np.concatenate([inp["b_group"], inp["b_expert"]], axis=1), dtype=f)
    sh["fnw"] = np.ascontiguousarray(np.broadcast_to(inp["final_norm_w"].reshape(1, D), (128, D)), dtype=f)
    wi = np.zeros((DEPTH, D, NGRP * 128), f)
    wi[:, :, :IN_COLS] = inp["w_in"]
    sh["win"] = np.ascontiguousarray(wi.reshape(DEPTH, KD, 128, NGRP, 128).transpose(0, 3, 2, 1, 4))
    sh["w_out"] = np.ascontiguousarray(inp["w_out"], dtype=f)
    sh["w_gate"] = np.ascontiguousarray(inp["w_gate"], dtype=f)
    sh["w_up"] = np.ascontiguousarray(inp["w_up"], dtype=f)
    sh["w_down"] = np.ascontiguousarray(inp["w_down"], dtype=f)
    return sh


def prepare_core(inp, core):
    f = np.float32
    b0 = core * NB
    m = {}
    m["xin"] = np.ascontiguousarray(np.concatenate([inp["ctx"][b0:b0 + NB], inp["x"][b0:b0 + NB]], axis=1), dtype=f)
    cv = np.concatenate([inp["c"][b0:b0 + NB], inp["c_ctx"].reshape(1, D)], axis=0)
    m["cvec"] = np.ascontiguousarray(cv.reshape(3, KD, 128).transpose(2, 1, 0), dtype=f)
    return m


_CACHE = {}


def kernel(**inputs):
    inp = {k: np.asarray(v) for k, v in inputs.items()}
    n_cores = 8
    if "nc" not in _CACHE:
        _CACHE["nc"] = build_program(DEPTH)
    nc = _CACHE["nc"]
    shared = prepare_shared(inp)
    in_maps = []
    for core in range(n_cores):
        m = dict(shared)
        m.update(prepare_core(inp, core))
        in_maps.append(m)
    res = run_bass_kernel_spmd(nc, in_maps, core_ids=list(range(n_cores)))
    out = np.concatenate([np.asarray(r["out"], dtype=np.float32) for r in res.results], axis=0)
    return out
```

```python
import numpy as np
from contextlib import ExitStack
import concourse.bass as bass
import concourse.mybir as mybir
from concourse.bass_utils import run_bass_kernel_spmd

F32 = mybir.dt.float32
BF16 = mybir.dt.bfloat16
ALU = mybir.AluOpType
AF = mybir.ActivationFunctionType
AX = mybir.AxisListType

D = 1024
KD = 8
DEPTH = 4
NB = 2
TC = 256
TL = 2048
T = TC + TL
NT = T // 128
GRID_W = 64
IN_COLS = 4624
NGRP = 37
FF = 512
NE = 16
EPS = 1e-6
HG_Q, HG_I, HG_G, HG_FF, HG_FB = 0, 512, 1024, 1536, 2048
DN_Q, DN_K, DN_V, DN_Z = 2560, 3072, 3584, 4096
SMALL0 = 4608
C_ID, C_ONES, C_INCF, C_STRF, C_INCB, C_STRB, C_NEGF, C_NEGB, C_GTF, C_GTB, C_INCF32, C_INCB32 = range(12)
NCONST = 12 * 128
TTILES = [(0, 256), (256, 512), (768, 512), (1280, 512), (1792, 512)]


class Dep:
    __slots__ = ("name", "lw", "rd", "sem", "semv", "excl")

    def __init__(self, name, excl=False):
        self.name = name
        self.excl = excl
        self.lw = None
        self.rd = {}
        self.sem = None
        self.semv = 0


class Sched:
    ENGS = ("pe", "dve", "act", "pool", "sp")

    def __init__(self, nc, stack):
        self.nc = nc
        self.stack = stack
        self.q = {e: [] for e in self.ENGS}
        self.cnt = {e: 0 for e in self.ENGS}
        self.seen = {}
        for e in self.ENGS:
            self.seen[e] = {}
            self.seen["dmaq_" + e] = {}
        self.esem = {e: stack.enter_context(nc.semaphore("prog_" + e)) for e in self.ENGS}
        self.dsems = []
        self.free_sems = []
        self.nsem_alloc = 0
        self.ninst = 0
        self.uid = 0

    def sb(self, name, shape, dt, stack=None):
        self.uid += 1
        nm = f"{name}_{self.uid}"
        t = (stack or self.stack).enter_context(self.nc.sbuf_tensor(nm, list(shape), dt))
        return t, Dep(nm)

    def ps(self, name, shape, dt, stack=None):
        self.uid += 1
        nm = f"{name}_{self.uid}"
        t = (stack or self.stack).enter_context(self.nc.psum_tensor(nm, list(shape), dt))
        return t, Dep(nm)

    def _dsem(self, d):
        if d.sem is None:
            if self.free_sems:
                d.sem, d.semv = self.free_sems.pop()
            else:
                self.nsem_alloc += 1
                d.sem = self.stack.enter_context(self.nc.semaphore("dsem%d" % self.nsem_alloc))
                d.semv = 0
            self.dsems.append(d)
        return d.sem

    def _need(self, eng, ev, waits, is_dma, raw=False):
        if ev is None:
            return
        key, val, sem = ev
        if key == eng and not is_dma and not (raw and eng != "pe"):
            return
        k2 = ("dmaq_" + eng) if is_dma else eng
        if self.seen[k2].get(key, 0) >= val:
            return
        self.seen[k2][key] = val
        if not is_dma:
            pass
        waits.append((sem, val))

    def _collect(self, eng, reads, writes, is_dma=False):
        waits = []
        for d in reads:
            self._need(eng, d.lw, waits, is_dma, raw=True)
        for d in writes:
            self._need(eng, d.lw, waits, is_dma)
            for ev in d.rd.values():
                self._need(eng, ev, waits, is_dma)
        return waits

    def _mark(self, ev, reads, writes):
        for d in reads:
            old = d.rd.get(ev[0])
            if old is None or old[1] < ev[1]:
                d.rd[ev[0]] = ev
        for d in writes:
            d.lw = ev
            d.rd = {}

    def op(self, eng, fn, reads=(), writes=()):
        if any(d.excl for d in reads):
            writes = list(writes) + [d for d in reads if d.excl]
            reads = [d for d in reads if not d.excl]
        waits = self._collect(eng, reads, writes)
        self.cnt[eng] += 1
        ev = (eng, self.cnt[eng], self.esem[eng])
        self._mark(ev, reads, writes)
        self.q[eng].append((waits, fn, self.esem[eng], 1))
        self.ninst += 1

    def dma(self, eng, out, in_, reads=(), writes=(), **kw):
        waits = self._collect(eng, reads, writes, True)
        anchor = (list(writes) + list(reads))[0]
        sem = self._dsem(anchor)
        anchor.semv += 16
        ev = ("dma_%d" % id(sem), anchor.semv, sem)
        self._mark(ev, reads, writes)
        self.q[eng].append((waits, lambda e: e.dma_start(out=out, in_=in_, **kw), sem, 16))
        self.ninst += 1

    def barrier(self):
        for e in self.ENGS:
            waits = []
            for o in self.ENGS:
                if o != e and self.cnt[o] > self.seen[e].get(o, 0):
                    self.seen[e][o] = self.cnt[o]
                    waits.append((self.esem[o], self.cnt[o]))
            for d in self.dsems:
                key = "dma_%d" % id(d.sem)
                if d.semv > self.seen[e].get(key, 0):
                    self.seen[e][key] = d.semv
                    waits.append((d.sem, d.semv))
            for k, v in self.seen[e].items():
                if self.seen["dmaq_" + e].get(k, 0) < v:
                    self.seen["dmaq_" + e][k] = v
            self.q[e].append((waits, None, None, 0))
        for d in self.dsems:
            self.free_sems.append((d.sem, d.semv))
            d.sem = None
        self.dsems = []

    def build(self):
        nc = self.nc
        with nc.Block() as block:
            def run(eng_name):
                def body(e):
                    for waits, fn, sem, inc in self.q[eng_name]:
                        if fn is None:
                            for (s, v) in waits:
                                e.wait_ge(s, v)
                            continue
                        for (s, v) in waits[:-1]:
                            e.wait_ge(s, v)
                        ins = fn(e)
                        if waits:
                            ins._wait_ge(waits[-1][0], waits[-1][1])
                        ins.then_inc(sem, inc)
                return body
            block.tensor(run("pe"))
            block.vector(run("dve"))
            block.scalar(run("act"))
            block.gpsimd(run("pool"))
            block.sync(run("sp"))
        self.q = {e: [] for e in self.ENGS}


class Ring:
    def __init__(self, S, kind, name, shape, dt, n, stack):
        mk = S.sb if kind == "sb" else S.ps
        self.items = [mk(f"{name}{i}", shape, dt, stack) for i in range(n)]
        self.i = 0

    def next(self):
        it = self.items[self.i % len(self.items)]
        self.i += 1
        return it


class PsRing:
    def __init__(self, S, name, nbanks, width, dt, stack):
        per = (2048 // (4 if dt == F32 else 2))
        banks = []
        for b in range(nbanks):
            t, _ = S.ps(f"{name}{b}", [128, per], dt, stack)
            banks.append((t, Dep(f"{name}{b}", excl=True)))
        self.items = []
        for j in range(per // width):
            for (t, dep) in banks:
                self.items.append((t[:, j * width:(j + 1) * width], dep))
        self.i = 0

    def next(self):
        it = self.items[self.i % len(self.items)]
        self.i += 1
        return it


def roundrobin(gens):
    gens = list(gens)
    while gens:
        for g in list(gens):
            try:
                next(g)
            except StopIteration:
                gens.remove(g)


def build_program(nlayers=DEPTH, dbg=(), stop=None, nb_run=NB):
    nc = bass.Bass("TRN2", target_bir_lowering=False)

    def din(name, shape):
        return nc.dram_tensor(name, list(shape), F32, kind="ExternalInput").ap()

    xin = din("xin", [NB, T, D])
    cvec_d = din("cvec", [128, KD, 3])
    consts_d = din("consts", [128, NCONST])
    wmod_d = din("w_mod", [DEPTH, D, 6 * D])
    bmod_d = din("b_mod", [DEPTH, 6 * D])
    normw_d = din("normw", [128, DEPTH * 2 * KD])
    lb_d = din("lbh", [128, DEPTH * 8])
    headnw_d = din("headnw", [128, DEPTH * 2])
    convw_d = din("convw", [128, DEPTH * 12 * 9])
    alog_d = din("alog", [128, DEPTH * 16])
    wr_d = din("wr", [DEPTH, 128, KD, 20])
    br_d = din("br", [DEPTH, 20])
    fnw_d = din("fnw", [128, D])
    win_d = din("win", [DEPTH, NGRP, 128, KD, 128])
    wout_d = din("w_out", [DEPTH, D, D])
    wg_d = din("w_gate", [DEPTH, NE, D, FF])
    wu_d = din("w_up", [DEPTH, NE, D, FF])
    wd_d = din("w_down", [DEPTH, NE, FF, D])
    out_d = nc.dram_tensor("out", [NB, TL, D], F32, kind="ExternalOutput").ap()
    resA = nc.dram_tensor("resA", [NB, T, D], F32, kind="Internal").ap()
    resB = nc.dram_tensor("resB", [NB, T, D], F32, kind="Internal").ap()
    dbg_out = {}
    for name, shape in dbg:
        dbg_out[name] = nc.dram_tensor(name, list(shape), F32, kind="ExternalOutput").ap()

    with ExitStack() as top:
        S = Sched(nc, top)

        def mm(out, lhsT, rhs, st, sp, R, W):
            S.op("pe", lambda e: e.matmul(out, lhsT=lhsT, rhs=rhs, start=st, stop=sp), R, W)

        def tr(out, in_, ident, R, W):
            S.op("pe", lambda e: e.transpose(out, in_, ident), R, W)

        def act(out, in_, func, R, W, **kw):
            S.op("act", lambda e: e.activation(out, in_, func, **kw), R, W)

        def tt(eng, out, a, b, op, R, W):
            S.op(eng, lambda e: e.tensor_tensor(out, a, b, op), R, W)

        def ts1(eng, out, a, s, op, R, W):
            S.op(eng, lambda e: e.tensor_single_scalar(out, a, s, op), R, W)

        def ts2(eng, out, a, s1, s2, op0, op1, R, W):
            S.op(eng, lambda e: e.tensor_scalar(out, a, s1, s2, op0, op1), R, W)

        def stt(eng, out, a, s, b, op0, op1, R, W):
            S.op(eng, lambda e: e.scalar_tensor_tensor(out, a, s, b, op0, op1), R, W)

        def cp(eng, out, a, R, W):
            if eng == "act":
                S.op("act", lambda e: e.copy(out, a), R, W)
            else:
                S.op(eng, lambda e: e.tensor_copy(out, a), R, W)

        def dump(name, ap_sb, DEP, dram_ap=None):
            if name in dbg_out:
                dd = Dep("dbg_" + name)
                S.dma("sp", dram_ap if dram_ap is not None else dbg_out[name], ap_sb, reads=[DEP], writes=[dd])

        cst, CST = S.sb("cst", [128, NCONST], F32)
        S.dma("sp", cst[:], consts_d, writes=[CST])

        def C(i):
            return cst[:, i * 128:(i + 1) * 128]

        idb, IDB = S.sb("idb", [128, 128], BF16)
        onb, ONB = S.sb("onb", [128, 128], BF16)
        cp("dve", idb[:], C(C_ID), [CST], [IDB])
        cp("dve", onb[:], C(C_ONES), [CST], [ONB])
        epsc, EPSC = S.sb("epsc", [128, 1], F32)
        S.op("dve", lambda e: e.memset(epsc[:], EPS), (), [EPSC])
        normw, NORMW = S.sb("normw", [128, DEPTH, 2, KD], F32)
        S.dma("sp", normw[:].rearrange("p a b c -> p (a b c)"), normw_d, writes=[NORMW])
        headnw, HEADNW = S.sb("headnw", [128, DEPTH, 2], F32)
        S.dma("sp", headnw[:].rearrange("p a b -> p (a b)"), headnw_d, writes=[HEADNW])
        convw, CONVW = S.sb("convw", [128, DEPTH, 12, 9], F32)
        S.dma("sp", convw[:].rearrange("p a b c -> p (a b c)"), convw_d, writes=[CONVW])
        alog, ALOG = S.sb("alog", [128, DEPTH, 16], F32)
        S.dma("sp", alog[:].rearrange("p a b -> p (a b)"), alog_d, writes=[ALOG])
        act(alog[:, :, 0:8], alog[:, :, 0:8], AF.Exp, [ALOG], [ALOG])
        ts1("dve", alog[:, :, 0:8], alog[:, :, 0:8], -1.0, ALU.mult, [ALOG], [ALOG])
        scT, SCT = S.sb("scT", [128, KD, 3], F32)
        S.dma("sp", scT[:].rearrange("p a b -> p (a b)"), cvec_d.rearrange("p a b -> p (a b)"), writes=[SCT])
        act(scT[:], scT[:], AF.Silu, [SCT], [SCT])
        lbt, LBT = S.sb("lbt", [128, DEPTH, 8], F32)
        oml, OML = S.sb("oml", [128, DEPTH, 8], F32)
        with ExitStack() as ph:
            raw, RAWL = S.sb("lbraw", [128, DEPTH, 8], F32, ph)
            m8, M8 = S.sb("lbm", [128, 8], F32, ph)
            S.dma("sp", raw[:].rearrange("p a b -> p (a b)"), lb_d, writes=[RAWL])
            tt("dve", m8[:], raw[:, 0, :], raw[:, 1, :], ALU.max, [RAWL], [M8])
            tt("dve", m8[:], m8[:], raw[:, 2, :], ALU.max, [RAWL, M8], [M8])
            tt("dve", m8[:], m8[:], raw[:, 3, :], ALU.max, [RAWL, M8], [M8])
            for l in range(DEPTH):
                tt("dve", raw[:, l, :], raw[:, l, :], m8[:], ALU.subtract, [RAWL, M8], [RAWL])
            act(raw[:], raw[:], AF.Exp, [RAWL], [RAWL])
            tt("dve", m8[:], raw[:, 0, :], raw[:, 1, :], ALU.add, [RAWL], [M8])
            tt("dve", m8[:], m8[:], raw[:, 2, :], ALU.add, [RAWL, M8], [M8])
            tt("dve", m8[:], m8[:], raw[:, 3, :], ALU.add, [RAWL, M8], [M8])
            S.op("dve", lambda e: e.reciprocal(m8[:], m8[:]), [M8], [M8])
            for l in range(DEPTH):
                tt("dve", raw[:, l, :], raw[:, l, :], m8[:], ALU.mult, [RAWL, M8], [RAWL])
            S.op("dve", lambda e: e.memset(lbt[:, 0, :], 0.0), (), [LBT])
            cp("dve", lbt[:, 1, :], raw[:, 1, :], [RAWL], [LBT])
            tt("dve", lbt[:, 2, :], lbt[:, 1, :], raw[:, 2, :], ALU.add, [RAWL, LBT], [LBT])
            tt("dve", lbt[:, 3, :], lbt[:, 2, :], raw[:, 3, :], ALU.add, [RAWL, LBT], [LBT])
            ts2("dve", oml[:], lbt[:], -1.0, 1.0, ALU.mult, ALU.add, [LBT], [OML])
            S.barrier()
            S.build()

        class NS:
            pass

        class _Stop(Exception):
            pass

        STOPPED = [False]

        def chk(tag):
            if stop == tag and not STOPPED[0]:
                S.barrier()
                S.build()
                STOPPED[0] = True
            return STOPPED[0]

        ORDER = {0: list(range(NT)), 1: [1, 0] + list(range(NT - 1, 1, -1))}

        def dmask(d):
            return (C(C_INCF), C(C_STRF), C(C_NEGF), C(C_GTF)) if d == 0 else \
                   (C(C_INCB), C(C_STRB), C(C_NEGB), C(C_GTB))

        def gbcast(ph, L, r, w, psr):
            G, GD = S.sb("G", [128, D], F32, ph)
            lr = Ring(S, "sb", "gl", [128, 128], F32, 2, ph)
            base = (2 + 3 * w) * 8
            for hf in range(2):
                ps, PS = psr.next()
                for k4 in range(4):
                    k = hf * 4 + k4
                    lh, LH = lr.next()
                    cp("dve", lh[:], L.modT[:, base + k, r:r + 1].to_broadcast([128, 128]), [L.MODT], [LH])
                    mm(ps[:, k4 * 128:(k4 + 1) * 128], lh[:], C(C_ID), True, True, [LH, CST], [PS])
                cp("act", G[:, hf * 512:(hf + 1) * 512], ps[:, :], [PS], [GD])
            return G, GD

        def norm_phase(L, b, src, SRC, wi, hT, HT, tstart):
            with ExitStack() as ph:
                xr = Ring(S, "sb", "xt", [128, D], F32, 3, ph)
                xnr = Ring(S, "sb", "xn", [128, D], BF16, 3, ph)
                junk, JUNK = S.sb("junk", [128, D], BF16, ph)
                stt_r = Ring(S, "sb", "st", [128, 4], F32, 4, ph)
                ptr = PsRing(S, "ptr", 4, 1024, BF16, ph)
                tmpr = Ring(S, "sb", "mt", [128, KD, 128], F32, 3, ph)
                for i in range(tstart, NT):
                    x, X = xr.next()
                    S.dma("sp", x[:], src[b, i * 128:(i + 1) * 128, :], reads=[SRC], writes=[X])
                    st, ST = stt_r.next()
                    act(junk[:], x[:], AF.Square, [X], [JUNK, ST], accum_out=st[:, 0:1])
                    act(st[:, 1:2], st[:, 0:1], AF.Sqrt, [ST, EPSC], [ST], scale=1.0 / D, bias=epsc[:, 0:1])
                    S.op("dve", lambda e, st=st: e.reciprocal(st[:, 2:3], st[:, 1:2]), [ST], [ST])
                    xn, XN = xnr.next()
                    act(xn[:], x[:], AF.Copy, [X, ST], [XN], scale=st[:, 2:3])
                    r = 2 if i < 2 else b
                    pp, PP = ptr.next()
                    for k in range(KD):
                        tr(pp[:, k * 128:(k + 1) * 128], xn[:, k * 128:(k + 1) * 128], idb[:], [XN, IDB], [PP])
                    m, M = tmpr.next()
                    A = L.abc[:, 2 * wi, :, r:r + 1].to_broadcast([128, KD, 128])
                    B = L.abc[:, 2 * wi + 1, :, r:r + 1].to_broadcast([128, KD, 128])
                    tt("dve", m[:], pp.rearrange("p (a b) -> p a b", b=128), A, ALU.mult, [PP, L.ABC], [M])
                    tt("pool", hT[:, :, i * 128:(i + 1) * 128], m[:], B, ALU.add, [M, L.ABC], [HT])
                S.barrier()
                S.build()

        def inproj(L, hT, HT, wring, pin, g, evac):
            wb, WB = wring.next()
            S.dma("pool", wb[:], win_d[L.l, g], writes=[WB])
            for (s, n) in TTILES:
                ps, PS = pin.next()
                for k in range(KD):
                    mm(ps[:, 0:n], wb[:, k, :], hT[:, k, s:s + n], k == 0, k == KD - 1, [WB, HT], [PS])
                evac(ps, PS, s, n)

        def conv(eng, raw, RAW, acc, ACC, wcol):
            rl = raw[:, TC:T].rearrange("p (r c) -> p r c", c=GRID_W)
            al = acc[:, TC:T].rearrange("p (r c) -> p r c", c=GRID_W)
            ts1(eng, acc[:, TC:T], raw[:, TC:T], wcol(4), ALU.mult, [RAW, CONVW], [ACC])
            for a in range(3):
                for b3 in range(3):
                    if a == 1 and b3 == 1:
                        continue
                    dr, dc = a - 1, b3 - 1
                    r0, r1 = max(0, -dr), 32 - max(0, dr)
                    c0, c1 = max(0, -dc), GRID_W - max(0, dc)
                    stt(eng, al[:, r0:r1, c0:c1], rl[:, r0 + dr:r1 + dr, c0 + dc:c1 + dc], wcol(a * 3 + b3),
                        al[:, r0:r1, c0:c1], ALU.mult, ALU.add, [RAW, ACC, CONVW], [ACC])
            ts1(eng, acc[:, 0:TC], raw[:, 0:TC], wcol(4), ALU.mult, [RAW, CONVW], [ACC])
            stt(eng, acc[:, 1:TC], raw[:, 0:TC - 1], wcol(3), acc[:, 1:TC], ALU.mult, ALU.add, [RAW, ACC, CONVW], [ACC])
            stt(eng, acc[:, 0:TC - 1], raw[:, 1:TC], wcol(5), acc[:, 0:TC - 1], ALU.mult, ALU.add,
                [RAW, ACC, CONVW], [ACC])

        def headnorm(ph, L, oacc, OACC, ZS, ZSD, which, hp, yT, YT, pss):
            sqr = Ring(S, "sb", "hsq", [128, 512], BF16, 2, ph)
            rtr = Ring(S, "sb", "hrt", [128, 512], F32, 2, ph)
            tfr = Ring(S, "sb", "htf", [128, 512], F32, 2, ph)
            for hh in range(2):
                for (s, n) in TTILES:
                    sq, SQ = sqr.next()
                    act(sq[:, 0:n], oacc[:, hh, s:s + n], AF.Square, [OACC], [SQ])
                    ps, PS = pss.next()
                    mm(ps[:, 0:n], onb[:], sq[:, 0:n], True, True, [ONB, SQ], [PS])
                    rt, RT = rtr.next()
                    act(rt[:, 0:n], ps[:, 0:n], AF.Sqrt, [PS, EPSC], [RT], scale=1.0 / 128, bias=epsc[:, 0:1])
                    S.op("dve", lambda e, rt=rt, n=n: e.reciprocal(rt[:, 0:n], rt[:, 0:n]), [RT], [RT])
                    tf, TF = tfr.next()
                    stt("dve", tf[:, 0:n], oacc[:, hh, s:s + n], headnw[:, L.l, which:which + 1], rt[:, 0:n],
                        ALU.mult, ALU.mult, [OACC, HEADNW, RT], [TF])
                    tt("pool", yT[:, which * 4 + 2 * hp + hh, s:s + n], tf[:, 0:n], ZS[:, hh, s:s + n], ALU.mult,
                       [TF, ZSD], [YT])

        def small_cols(L, b, hT, HT, bp):
            P_ = NS()
            P_.beta, P_.BETA = S.sb("beta", [128, NT, 8], F32, bp)
            P_.la, P_.LA = S.sb("la", [128, NT, 8], F32, bp)
            P_.ecols, P_.ECOLS = S.sb("ecols", [128, NT, 16], F32, bp)
            P_.er, P_.ER = S.sb("er", [128, NT, 2, 8], F32, bp)
            with ExitStack() as ph:
                wb, WB = S.sb("wbs", [128, KD, 128], BF16, ph)
                S.dma("pool", wb[:], win_d[L.l, NGRP - 1], writes=[WB])
                ba, BA = S.sb("ba", [128, NT, 16], F32, ph)
                pr = PsRing(S, "pba", 2, 16, F32, ph)
                for i in range(NT):
                    ps, PS = pr.next()
                    for k in range(KD):
                        mm(ps, hT[:, k, i * 128:(i + 1) * 128], wb[:, k, 0:16], k == 0, k == KD - 1, [HT, WB], [PS])
                    cp("dve", ba[:, i, :], ps, [PS], [BA])
                act(P_.beta[:], ba[:, :, 0:8], AF.Sigmoid, [BA], [P_.BETA])
                tt("dve", P_.la[:], ba[:, :, 8:16], alog[:, L.l, 8:16].unsqueeze(1).to_broadcast([128, NT, 8]), ALU.add,
                   [BA, ALOG], [P_.LA])
                act(P_.la[:], P_.la[:], AF.Exp, [P_.LA], [P_.LA])
                act(P_.la[:], P_.la[:], AF.Ln, [P_.LA], [P_.LA], bias=1.0)
                tt("dve", P_.la[:], P_.la[:], alog[:, L.l, 0:8].unsqueeze(1).to_broadcast([128, NT, 8]), ALU.mult,
                   [P_.LA, ALOG], [P_.LA])
                pc = PsRing(S, "pcol", 2, 16, F32, ph)
                for i in range(NT):
                    ps, PS = pc.next()
                    for d in range(2):
                        INC, STR, NEG, GT = dmask(d)
                        STRO = dmask(1 - d)[1]
                        mm(ps[:, d * 4:d * 4 + 4], INC, P_.la[:, i, d * 4:d * 4 + 4], True, True, [CST, P_.LA], [PS])
                        mm(ps[:, 8 + d * 4:8 + d * 4 + 4], STRO, P_.la[:, i, d * 4:d * 4 + 4], True, True,
                           [CST, P_.LA], [PS])
                    act(P_.ecols[:, i, :], ps, AF.Exp, [PS], [P_.ECOLS])
                ts1("dve", P_.ecols[:, :, 0:8], P_.ecols[:, :, 0:8], -1.0, ALU.mult, [P_.ECOLS], [P_.ECOLS])
                ts1("dve", P_.er[:, :, 0, :], P_.ecols[:, :, 8:16], cst[:, C_INCF * 128 + 63:C_INCF * 128 + 64], ALU.mult,
                    [P_.ECOLS, CST], [P_.ER])
                ts1("dve", P_.er[:, :, 1, :], P_.ecols[:, :, 8:16], cst[:, C_INCB * 128 + 64:C_INCB * 128 + 65], ALU.mult,
                    [P_.ECOLS, CST], [P_.ER])
                S.barrier()
                S.build()
            return P_

        def gdn_headpair(L, b, hp, hT, HT, yT, YT, SC):
            l = L.l
            with ExitStack() as ph:
                QT, QTD = S.sb("QT", [128, 2, T], BF16, ph)
                KT, KTD = S.sb("KT", [128, 2, T], BF16, ph)
                VT, VTD = S.sb("VT", [128, 2, T], BF16, ph)
                ZS, ZSD = S.sb("ZS", [128, 2, T], BF16, ph)
                oacc, OACC = S.sb("oacc", [128, 2, T], F32, ph)
                S.op("pool", lambda e: e.memset(oacc[:], 0.0), (), [OACC])
                with ExitStack() as ph2:
                    rawr = Ring(S, "sb", "raw", [128, T], F32, 1, ph2)
                    caccr = Ring(S, "sb", "cacc", [128, T], F32, 1, ph2)
                    sq32, SQ32 = S.sb("sq32", [128, T], F32, ph2)
                    sqb, SQB = S.sb("sqb", [128, T], BF16, ph2)
                    rt, RT = S.sb("rt", [128, T], F32, ph2)
                    wring = Ring(S, "sb", "wb", [128, KD, 128], BF16, 3, ph2)
                    pin = PsRing(S, "pin", 3, 512, F32, ph2)
                    pss = PsRing(S, "pss", 2, 512, F32, ph2)
                    gi = 0
                    for kind, colbase in (("z", DN_Z), ("v", DN_V), ("q", DN_Q), ("k", DN_K)):
                        for hh in range(2):
                            h = 2 * hp + hh
                            g = colbase // 128 + h
                            if kind == "z":
                                inproj(L, hT, HT, wring, pin, g,
                                       lambda ps, PS, s, n, hh=hh: act(ZS[:, hh, s:s + n], ps[:, 0:n], AF.Silu, [PS], [ZSD]))
                                continue
                            raw, RAW = rawr.next()
                            cacc, CACC = caccr.next()
                            inproj(L, hT, HT, wring, pin, g,
                                   lambda ps, PS, s, n, raw=raw, RAW=RAW: cp("act", raw[:, s:s + n], ps[:, 0:n], [PS], [RAW]))
                            cg = {"q": 0, "k": 1, "v": 2}[kind] * 4 + h
                            conv("dve", raw, RAW, cacc, CACC,
                                 lambda tap, cg=cg: convw[:, l, cg, tap:tap + 1])
                            gi += 1
                            if kind == "v":
                                act(VT[:, hh, :], cacc[:], AF.Silu, [CACC], [VTD])
                                continue
                            act(sq32[:], cacc[:], AF.Silu, [CACC], [SQ32])
                            tt("pool", sqb[:], sq32[:], sq32[:], ALU.mult, [SQ32], [SQB])
                            for (s, n) in TTILES:
                                ps, PS = pss.next()
                                mm(ps[:, 0:n], onb[:], sqb[:, s:s + n], True, True, [ONB, SQB], [PS])
                                act(rt[:, s:s + n], ps[:, 0:n], AF.Sqrt, [PS, EPSC], [RT], bias=epsc[:, 0:1])
                            S.op("dve", lambda e: e.reciprocal(rt[:], rt[:]), [RT], [RT])
                            if kind == "q":
                                stt("dve", QT[:, hh, :], sq32[:], float(128 ** -0.5), rt[:], ALU.mult, ALU.mult,
                                    [SQ32, RT], [QTD])
                            else:
                                tt("dve", KT[:, hh, :], sq32[:], rt[:], ALU.mult, [SQ32, RT], [KTD])
                    if stop == "G1" and hp == 0 and b == 0 and l == 0:
                        for j, (tt_, TD_) in enumerate(((QT, QTD), (KT, KTD), (VT, VTD), (ZS, ZSD))):
                            for hh_ in range(2):
                                cp("act", yT[:, j * 2 + hh_, :], tt_[:, hh_, :], [TD_], [YT])
                        S.dma("pool", dbg_out["yT"].rearrange("p (k t) -> p k t", k=KD), yT[:], reads=[YT], writes=[Dep("dbg_yT1")])
                    S.barrier()
                    S.build()
                    if chk("G1"):
                        return
                with ExitStack() as ph3:
                    chains = [(hh, d) for hh in range(2) for d in range(2)]
                    pab = [S.ps(f"pa{j}", [128, 512], F32, ph3)[0] for j in range(3)]
                    pabd = [Dep(f"pa{j}", excl=True) for j in range(3)]
                    pbb = S.ps("pb16", [128, 1024], BF16, ph3)[0]
                    pbbd = Dep("pb16", excl=True)
                    psb = [S.ps(f"pst{j}", [128, 512], F32, ph3)[0] for j in range(4)]
                    psbd = [Dep(f"pst{j}", excl=True) for j in range(4)]

                    class Cyc:
                        def __init__(self, items):
                            self.items = items
                            self.i = 0

                        def next(self):
                            it = self.items[self.i % len(self.items)]
                            self.i += 1
                            return it
                    CH = []
                    for c in range(4):
                        o = NS()
                        o.pa = Cyc([(pab[j][:, c * 128:(c + 1) * 128], pabd[j]) for j in range(3)])
                        o.pb16 = Cyc([(pbb[:, (2 * c + j) * 128:(2 * c + j + 1) * 128], pbbd) for j in range(2)])
                        o.pstep = Cyc([(psb[c][:, j * 128:(j + 1) * 128], psbd[c]) for j in (0, 2, 3)])
                        o.pout = Cyc([(psb[c][:, 128:256], psbd[c])])
                        o.Sf, o.SF = S.sb("Sf", [128, 128], F32, ph3)
                        o.Sb, o.SB = S.sb("Sb", [128, 128], BF16, ph3)
                        S.op("dve", lambda e, o=o: e.memset(o.Sf[:], 0.0), (), [o.SF])
                        S.op("dve", lambda e, o=o: e.memset(o.Sb[:], 0.0), (), [o.SB])
                        for nm, dt, n in (("lam", F32, 2), ("ecb", F32, 2), ("dec", F32, 2), ("decs", F32, 2),
                                          ("Qd", BF16, 2), ("qkm", BF16, 2), ("A", F32, 2), ("Bm", F32, 2),
                                          ("Pt", F32, 2), ("Pf", BF16, 2), ("kdec0", BF16, 2), ("kdec1", BF16, 2),
                                          ("vtok", BF16, 2), ("Y0", BF16, 1), ("vnew", BF16, 1)):
                            setattr(o, nm, Ring(S, "sb", nm, [128, 128], dt, n, ph3))
                        for rg in (o.Y0, o.vnew):
                            for (t_, TD_) in rg.items:
                                S.op("pool", lambda e, t_=t_: e.memset(t_[:], 0.0), (), [TD_])
                        CH.append(o)

                    def prep(c, i):
                        o = CH[c]
                        hh, d = chains[c]
                        h = 2 * hp + hh
                        dh = d * 4 + h
                        blk = slice(i * 128, (i + 1) * 128)
                        INC, STR, NEG, GT = dmask(d)
                        lam, LAM = o.lam.next()
                        ts1("pool", lam[:], INC, SC.la[:, i, dh:dh + 1], ALU.mult, [CST, SC.LA], [LAM])
                        pc, PC = o.pa.next()
                        mm(pc, C(C_ONES), lam[:], True, True, [CST, LAM], [PC])
                        pd, PD = o.pa.next()
                        mm(pd, GT, lam[:], True, False, [CST, LAM], [PD])
                        mm(pd, C(C_ID), NEG, False, True, [CST], [PD])
                        pt2, PT2 = o.pb16.next()
                        tr(pt2, KT[:, hh, blk], idb[:], [KTD, IDB], [PT2])
                        pt3, PT3 = o.pb16.next()
                        tr(pt3, VT[:, hh, blk], idb[:], [VTD, IDB], [PT3])
                        yield
                        ecb, ECB = o.ecb.next()
                        act(ecb[:], pc, AF.Exp, [PC], [ECB])
                        dec, DEC = o.dec.next()
                        act(dec[:], pd, AF.Exp, [PD], [DEC])
                        kdec0, KDEC0 = o.kdec0.next()
                        ts1("dve", kdec0[:], pt2, SC.er[:, i, 0, dh:dh + 1], ALU.mult, [PT2, SC.ER], [KDEC0])
                        kdec1, KDEC1 = o.kdec1.next()
                        ts1("dve", kdec1[:], pt2, SC.er[:, i, 1, dh:dh + 1], ALU.mult, [PT2, SC.ER], [KDEC1])
                        vtok, VTOK = o.vtok.next()
                        cp("act", vtok[:], pt3, [PT3], [VTOK])
                        yield
                        pkk, PKK = o.pa.next()
                        mm(pkk, KT[:, hh, blk], KT[:, hh, blk], True, True, [KTD], [PKK])
                        pqk, PQK = o.pa.next()
                        mm(pqk, KT[:, hh, blk], QT[:, hh, blk], True, True, [KTD, QTD], [PQK])
                        yield
                        Qd, QD = o.Qd.next()
                        tt("pool", Qd[:], QT[:, hh, blk], ecb[:], ALU.mult, [QTD, ECB], [QD])
                        qkm, QKM = o.qkm.next()
                        tt("dve", qkm[:], pqk, dec[:], ALU.mult, [PQK, DEC], [QKM])
                        decs, DECS = o.decs.next()
                        tt("pool", decs[:], dec[:], STR, ALU.mult, [DEC, CST], [DECS])
                        A0, A0D = o.A.next()
                        stt("dve", A0[:], pkk, SC.beta[:, i, dh:dh + 1], decs[:], ALU.mult, ALU.mult,
                            [PKK, SC.BETA, DECS], [A0D])
                        yield
                        pt1, PT1 = o.pa.next()
                        tr(pt1, A0[:], C(C_ID), [A0D, CST], [PT1])
                        P0, P0D = o.Pt.next()
                        tt("pool", P0[:], C(C_ID), A0[:], ALU.subtract, [CST, A0D], [P0D])
                        yield
                        B0, B0D = o.Bm.next()
                        cp("act", B0[:], pt1, [PT1], [B0D])
                        yield
                        Ap, APD, Bp, BPD, Pp, PPD = A0, A0D, B0, B0D, P0, P0D
                        for lev in range(1, 6):
                            if lev < 5:
                                pA, PA_ = o.pa.next()
                                mm(pA, Bp[:], Ap[:], True, True, [BPD, APD], [PA_])
                            pB, PB_ = o.pa.next()
                            mm(pB, Ap[:], Bp[:], True, True, [APD, BPD], [PB_])
                            yield
                            if lev < 5:
                                An, AND_ = o.A.next()
                                cp("act", An[:], pA, [PA_], [AND_])
                            Bn, BND = o.Bm.next()
                            cp("dve", Bn[:], pB, [PB_], [BND])
                            yield
                            pP, PP_ = o.pa.next()
                            mm(pP, Bn[:], Pp[:], True, True, [BND, PPD], [PP_])
                            yield
                            Pn, PND = (o.Pf if lev == 5 else o.Pt).next()
                            tt("dve", Pn[:], pP, Pp[:], ALU.add, [PP_, PPD], [PND])
                            yield
                            if lev < 5:
                                Ap, APD = An, AND_
                            Bp, BPD, Pp, PPD = Bn, BND, Pn, PND
                        o.cur = dict(ecb=(ecb, ECB), Qd=(Qd, QD), qkm=(qkm, QKM), Pf=(Pp, PPD), kdec=((kdec0, KDEC0), (kdec1, KDEC1)),
                                     vtok=(vtok, VTOK))

                    def steps(c, i, cur):
                        o = CH[c]
                        hh, d = chains[c]
                        h = 2 * hp + hh
                        dh = d * 4 + h
                        blk = slice(i * 128, (i + 1) * 128)
                        ecb, ECB = cur["ecb"]
                        Qd, QD = cur["Qd"]
                        qkm, QKM = cur["qkm"]
                        Pf, PFD = cur["Pf"]
                        vtok, VTOK = cur["vtok"]
                        po, PO = o.pout.next()
                        Y0, Y0D = o.Y0.next()
                        vnew, VNEW = o.vnew.next()
                        for ch in ((0, 1) if d == 0 else (1, 0)):
                            rows = slice(ch * 64, ch * 64 + 64)
                            pks, PKS = o.pstep.next()
                            mm(pks, KT[:, hh, blk], o.Sb[:], True, True, [KTD, o.SB], [PKS])
                            yield
                            stt("dve", Y0[rows, :], pks[rows, :], SC.ecols[rows, i, dh:dh + 1], vtok[rows, :],
                                ALU.mult, ALU.add, [PKS, SC.ECOLS, VTOK], [Y0D])
                            yield
                            pz, PZ = o.pstep.next()
                            mm(pz, Pf[:, :], Y0[:, :], True, True, [PFD, Y0D], [PZ])
                            yield
                            act(vnew[rows, :], pz[rows, :], AF.Copy, [PZ, SC.BETA], [VNEW], scale=SC.beta[rows, i, dh:dh + 1])
                            yield
                            mm(po[:, rows], o.Sb[:], Qd[:, rows], True, False, [o.SB, QD], [PO])
                            mm(po[:, rows], vnew[:, :], qkm[:, rows], False, True, [VNEW, QKM], [PO])
                            pds, PDS = o.pstep.next()
                            kdec, KDEC = cur["kdec"][ch]
                            mm(pds, kdec[:, :], vnew[:, :], True, True, [KDEC, VNEW], [PDS])
                            yield
                            gc = (ch * 64 + 63) if d == 0 else ch * 64
                            stt("dve", o.Sf[:], o.Sf[:], ecb[:, gc:gc + 1], pds, ALU.mult, ALU.add, [o.SF, ECB, PDS], [o.SF])
                            yield
                            cp("act", o.Sb[:], o.Sf[:], [o.SF], [o.SB])
                            yield
                        tt("dve", oacc[:, hh, blk], oacc[:, hh, blk], po, ALU.add, [OACC, PO], [OACC])

                    roundrobin([prep(c, ORDER[chains[c][1]][0]) for c in range(4)])
                    if chk("G2"):
                        return
                    for n in range(NT):
                        curs = [CH[c].cur for c in range(4)]
                        gens = [steps(c, ORDER[chains[c][1]][n], curs[c]) for c in range(4)]
                        if n + 1 < NT:
                            gens += [prep(c, ORDER[chains[c][1]][n + 1]) for c in range(4)]
                        roundrobin(gens)
                        if n == 0 and chk("G3"):
                            return
                    S.barrier()
                    S.build()
                    if chk("G4"):
                        return
                with ExitStack() as ph4:
                    pss = PsRing(S, "pss", 2, 512, F32, ph4)
                    headnorm(ph4, L, oacc, OACC, ZS, ZSD, 1, hp, yT, YT, pss)
                    S.barrier()
                    S.build()

        def hgrn_headpair(L, b, hp, hT, HT, yT, YT):
            l = L.l
            with ExitStack() as ph:
                qs, QS = S.sb("qs", [128, 2, T], BF16, ph)
                VT, VTD = S.sb("VTh", [128, 2, T], BF16, ph)
                ZS, ZSD = S.sb("ZSh", [128, 2, T], BF16, ph)
                oacc, OACC = S.sb("oacch", [128, 2, T], F32, ph)
                S.op("pool", lambda e: e.memset(oacc[:], 0.0), (), [OACC])
                chains = [(hh, d) for hh in range(2) for d in range(2)]
                CH = []
                for c in range(4):
                    o = NS()
                    o.qd, o.QD = S.sb("qd", [128, T], BF16, ph)
                    o.kd, o.KD = S.sb("kd", [128, T], BF16, ph)
                    o.gch, o.GCH = S.sb("gch", [128, T // 32], F32, ph)
                    CH.append(o)
                with ExitStack() as ph2:
                    tA, TA = S.sb("tA", [128, T], F32, ph2)
                    tB, TB = S.sb("tB", [128, T], F32, ph2)
                    tC, TCD = S.sb("tC", [128, T], F32, ph2)
                    rst, RST = S.sb("rst", [128, T], BF16, ph2)
                    S.op("pool", lambda e: e.memset(rst[:], 1.0), (), [RST])
                    S.op("pool", lambda e: e.memset(rst[:].rearrange("p (c k) -> p c k", k=32)[:, :, 0:1], 0.0), (), [RST])
                    wring = Ring(S, "sb", "wbh", [128, KD, 128], BF16, 2, ph2)
                    pin = PsRing(S, "pinh", 4, 512, F32, ph2)
                    for hh in range(2):
                        h = 2 * hp + hh
                        inproj(L, hT, HT, wring, pin, HG_Q // 128 + h,
                               lambda ps, PS, s, n, hh=hh: act(qs[:, hh, s:s + n], ps[:, 0:n], AF.Silu, [PS], [QS]))
                        inproj(L, hT, HT, wring, pin, HG_I // 128 + h,
                               lambda ps, PS, s, n, hh=hh: cp("dve", VT[:, hh, s:s + n], ps[:, 0:n], [PS], [VTD]))
                        inproj(L, hT, HT, wring, pin, HG_G // 128 + h,
                               lambda ps, PS, s, n, hh=hh: act(ZS[:, hh, s:s + n], ps[:, 0:n], AF.Silu, [PS], [ZSD]))
                    for c in range(4):
                        o = CH[c]
                        hh, d = chains[c]
                        h = 2 * hp + hh
                        dh = d * 4 + h
                        inproj(L, hT, HT, wring, pin, (HG_FF if d == 0 else HG_FB) // 128 + h,
                               lambda ps, PS, s, n: act(tA[:, s:s + n], ps[:, 0:n], AF.Sigmoid, [PS], [TA]))
                        ts2("dve", tA[:], tA[:], oml[:, l, dh:dh + 1], lbt[:, l, dh:dh + 1], ALU.mult, ALU.add,
                            [TA, OML, LBT], [TA])
                        act(tB[:], tA[:], AF.Ln, [TA], [TB])
                        ts2("dve", tA[:], tA[:], -1.0, 1.0, ALU.mult, ALU.add, [TA], [TA])
                        S.op("dve", lambda e: e.tensor_tensor_scan(tC[:], rst[:], tB[:], 0.0, ALU.mult, ALU.add),
                             [RST, TB], [TCD])
                        if d == 0:
                            cum, CUM, oth, OTH = tC, TCD, tB, TB
                            gcol = 31
                        else:
                            tt("pool", tB[:], tB[:], tC[:], ALU.subtract, [TB, TCD], [TB])
                            tB3 = tB[:].rearrange("p (c k) -> p c k", k=32)
                            tC3 = tC[:].rearrange("p (c k) -> p c k", k=32)
                            tt("dve", tB3, tB3, tC3[:, :, 31:32].to_broadcast([128, T // 32, 32]), ALU.add,
                               [TB, TCD], [TB])
                            cum, CUM, oth, OTH = tB, TB, tC, TCD
                            gcol = 0
                        act(oth[:], cum[:], AF.Exp, [CUM], [OTH])
                        tt("pool", o.qd[:], qs[:, hh, :], oth[:], ALU.mult, [QS, OTH], [o.QD])
                        cp("dve", o.gch[:], oth[:].rearrange("p (c k) -> p c k", k=32)[:, :, gcol], [OTH], [o.GCH])
                        act(oth[:], cum[:], AF.Exp, [CUM, o.QD, o.GCH], [OTH], scale=-1.0)
                        tt("dve", o.kd[:], tA[:], oth[:], ALU.mult, [TA, OTH], [o.KD])
                    S.barrier()
                    S.build()
                with ExitStack() as ph3:
                    pa = PsRing(S, "pah", 2, 128, F32, ph3)
                    pb16 = PsRing(S, "pb16h", 1, 128, BF16, ph3)
                    psbh = [S.ps(f"psth{j}", [128, 512], F32, ph3)[0] for j in range(4)]
                    psbhd = [Dep(f"psth{j}", excl=True) for j in range(4)]

                    class CycH:
                        def __init__(self, items):
                            self.items = items
                            self.i = 0

                        def next(self):
                            it = self.items[self.i % len(self.items)]
                            self.i += 1
                            return it
                    for c in range(4):
                        o = CH[c]
                        o.pstep = CycH([(psbh[c][:, j * 128:(j + 1) * 128], psbhd[c]) for j in (0, 2, 3)])
                        o.pout = CycH([(psbh[c][:, 128:256], psbhd[c])])
                        o.Sf, o.SF = S.sb("Sfh", [128, 128], F32, ph3)
                        o.Sb, o.SB = S.sb("Sbh", [128, 128], BF16, ph3)
                        o.tS, o.TS = S.sb("tSh", [128, 128], F32, ph3)
                        S.op("dve", lambda e, o=o: e.memset(o.Sf[:], 0.0), (), [o.SF])
                        S.op("dve", lambda e, o=o: e.memset(o.Sb[:], 0.0), (), [o.SB])
                        for nm, dt, n in (("kdtok0", BF16, 2), ("kdtok1", BF16, 2), ("vtok", BF16, 2), ("attm", BF16, 2)):
                            setattr(o, nm, Ring(S, "sb", nm + "h", [128, 128], dt, n, ph3))

                    NBH = T // 64
                    ORDH = {0: list(range(NBH)), 1: [3, 2, 1, 0] + list(range(NBH - 1, 3, -1))}

                    def prep(c, i):
                        o = CH[c]
                        hh, d = chains[c]
                        blk = slice(i * 64, (i + 1) * 64)
                        INC = (cst[0:64, C_INCF32 * 128:C_INCF32 * 128 + 64] if d == 0 else
                               cst[0:64, C_INCB32 * 128:C_INCB32 * 128 + 64])
                        pt1, PT1 = pb16.next()
                        tr(pt1[0:64, :], o.kd[:, blk], idb[:], [o.KD, IDB], [PT1])
                        pt2, PT2 = pb16.next()
                        tr(pt2[0:64, :], VT[:, hh, blk], idb[:], [VTD, IDB], [PT2])
                        pat, PAT = pa.next()
                        mm(pat[0:64, 0:64], o.kd[:, blk], o.qd[:, blk], True, True, [o.KD, o.QD], [PAT])
                        yield
                        kdtok0, KDTOK0 = o.kdtok0.next()
                        act(kdtok0[0:64, :], pt1[0:64, :], AF.Copy, [PT1, CST], [KDTOK0],
                            scale=cst[0:64, C_INCF32 * 128 + 31:C_INCF32 * 128 + 32])
                        kdtok1, KDTOK1 = o.kdtok1.next()
                        act(kdtok1[0:64, :], pt1[0:64, :], AF.Copy, [PT1, CST], [KDTOK1],
                            scale=cst[0:64, C_INCB32 * 128 + 32:C_INCB32 * 128 + 33])
                        vtok, VTOK = o.vtok.next()
                        cp("act", vtok[0:64, :], pt2[0:64, :], [PT2], [VTOK])
                        attm, ATTM = o.attm.next()
                        tt("dve", attm[0:64, 0:64], pat[0:64, 0:64], INC, ALU.mult, [PAT, CST], [ATTM])
                        yield
                        o.cur = dict(kdtok=((kdtok0, KDTOK0), (kdtok1, KDTOK1)), vtok=(vtok, VTOK), attm=(attm, ATTM))

                    def steps(c, i, cur):
                        o = CH[c]
                        hh, d = chains[c]
                        blk0 = i * 64
                        vtok, VTOK = cur["vtok"]
                        attm, ATTM = cur["attm"]
                        po, PO = o.pout.next()
                        for ch in ((0, 1) if d == 0 else (1, 0)):
                            rows = slice(ch * 32, ch * 32 + 32)
                            cols = slice(blk0 + ch * 32, blk0 + ch * 32 + 32)
                            mm(po[:, rows], o.Sb[:], o.qd[:, cols], True, False, [o.SB, o.QD], [PO])
                            mm(po[:, rows], vtok[0:64, :], attm[0:64, rows], False, True, [VTOK, ATTM], [PO])
                            pds, PDS = o.pstep.next()
                            kdtok, KDTOK = cur["kdtok"][ch]
                            mm(pds, kdtok[0:64, :], vtok[0:64, :], True, True, [KDTOK, VTOK], [PDS])
                            yield
                            tt("dve", o.tS[:], o.Sf[:], pds, ALU.add, [o.SF, PDS], [o.TS])
                            yield
                            ci = i * 2 + ch
                            act(o.Sb[:], o.tS[:], AF.Copy, [o.TS, o.GCH], [o.SB], scale=o.gch[:, ci:ci + 1])
                            ts1("dve", o.Sf[:], o.tS[:], o.gch[:, ci:ci + 1], ALU.mult, [o.TS, o.GCH], [o.SF])
                            yield
                        tt("dve", oacc[:, hh, blk0:blk0 + 64], oacc[:, hh, blk0:blk0 + 64], po[:, 0:64], ALU.add,
                           [OACC, PO], [OACC])

                    roundrobin([prep(c, ORDH[chains[c][1]][0]) for c in range(4)])
                    for n in range(NBH):
                        curs = [CH[c].cur for c in range(4)]
                        gens = [steps(c, ORDH[chains[c][1]][n], curs[c]) for c in range(4)]
                        if n + 1 < NBH:
                            gens += [prep(c, ORDH[chains[c][1]][n + 1]) for c in range(4)]
                        roundrobin(gens)
                    S.barrier()
                    S.build()
                with ExitStack() as ph4:
                    pss = PsRing(S, "pssh", 2, 512, F32, ph4)
                    headnorm(ph4, L, oacc, OACC, ZS, ZSD, 0, hp, yT, YT, pss)
                    S.barrier()
                    S.build()

        def outproj_phase(L, b, src, SRC, mid, MID, yT, YT, tstart):
            with ExitStack() as ph:
                wo, WO = S.sb("wo", [128, KD, D], BF16, ph)
                S.dma("pool", wo[:], wout_d[L.l].rearrange("(k p) n -> p k n", p=128), writes=[WO])
                pin = PsRing(S, "pino", 4, 512, F32, ph)
                Gx, GX = gbcast(ph, L, b, 0, pin)
                Gc, GC = (None, None) if tstart > 0 else gbcast(ph, L, 2, 0, pin)
                xr = Ring(S, "sb", "xo", [128, D], F32, 3, ph)
                tr_ = Ring(S, "sb", "to", [128, D], F32, 2, ph)
                for i in range(tstart, NT):
                    x, X = xr.next()
                    S.dma("sp", x[:], src[b, i * 128:(i + 1) * 128, :], reads=[SRC], writes=[X])
                    G, GD = (Gc, GC) if i < 2 else (Gx, GX)
                    t_, TD_ = tr_.next()
                    for hf in range(2):
                        ps, PS = pin.next()
                        for k in range(KD):
                            mm(ps[:, :], yT[:, k, i * 128:(i + 1) * 128], wo[:, k, hf * 512:(hf + 1) * 512],
                               k == 0, k == KD - 1, [YT, WO], [PS])
                        tt("dve", t_[:, hf * 512:(hf + 1) * 512], ps[:, :], G[:, hf * 512:(hf + 1) * 512], ALU.mult,
                           [PS, GD], [TD_])
                    tt("pool", x[:], x[:], t_[:], ALU.add, [X, TD_], [X])
                    S.dma("sp", mid[b, i * 128:(i + 1) * 128, :], x[:], reads=[X], writes=[MID])
                S.barrier()
                S.build()

        def moe_phase(L, b, mid, MID, dst, DST, last, tstart):
            l = L.l
            ntm = NT - tstart
            with ExitStack() as bp:
                hT, HT = S.sb("hT2", [128, KD, T], BF16, bp)
                gT, GT_ = S.sb("gT", [16, T], F32, bp)
                norm_phase(L, b, mid, MID, 1, hT, HT, tstart)
                with ExitStack() as ph:
                    lg, LG = S.sb("lg", [128, ntm, 20], F32, ph)
                    pr = PsRing(S, "plg", 2, 32, F32, ph)
                    for ii in range(ntm):
                        i = tstart + ii
                        ps, PS = pr.next()
                        for k in range(KD):
                            mm(ps[:, 0:20], hT[:, k, i * 128:(i + 1) * 128], L.wr[:, k, :], k == 0, False, [HT, L.WR], [PS])
                        mm(ps[:, 0:20], cst[0:1, C_ONES * 128:C_ONES * 128 + 128], L.brow[0:1, :], False, True,
                           [CST, L.BROW], [PS])
                        cp("dve", lg[:, ii, :], ps[:, 0:20], [PS], [LG])

                    def sbt(name, shape):
                        return S.sb(name, shape, F32, ph)
                    gl = lg[:, :, 0:4]
                    el = lg[:, :, 4:20]
                    gmax, GMAX = sbt("gmax", [128, ntm, 1])
                    gmask, GMASK = sbt("gmask", [128, ntm, 4])
                    gex, GEX = sbt("gex", [128, ntm, 4])
                    gw, GW = sbt("gw", [128, ntm, 1])
                    elm, ELM = sbt("elm", [128, ntm, 16])
                    m1, M1 = sbt("m1", [128, ntm, 1])
                    m2, M2 = sbt("m2", [128, ntm, 1])
                    mk1, MK1 = sbt("mk1", [128, ntm, 16])
                    mk2, MK2 = sbt("mk2", [128, ntm, 16])
                    w1, W1 = sbt("w1", [128, ntm, 1])
                    w2, W2 = sbt("w2", [128, ntm, 1])
                    gates, GATES = sbt("gates", [128, ntm, 16])
                    BIG = 1.0e4
                    S.op("dve", lambda e: e.tensor_reduce(gmax[:], gl, AX.X, ALU.max), [LG], [GMAX])
                    tt("dve", gmask[:], gl, gmax[:].to_broadcast([128, ntm, 4]), ALU.is_ge, [LG, GMAX], [GMASK])
                    tt("dve", gex[:], gl, gmax[:].to_broadcast([128, ntm, 4]), ALU.subtract, [LG, GMAX], [GEX])
                    act(gex[:], gex[:], AF.Exp, [GEX], [GEX])
                    S.op("dve", lambda e: e.tensor_reduce(gw[:], gex[:], AX.X, ALU.add), [GEX], [GW])
                    S.op("dve", lambda e: e.reciprocal(gw[:], gw[:]), [GW], [GW])
                    ts2("dve", gmask[:], gmask[:], BIG, -BIG, ALU.mult, ALU.add, [GMASK], [GMASK])
                    tt("dve", elm[:].rearrange("p n (g e) -> p n g e", e=4), el.rearrange("p n (g e) -> p n g e", e=4),
                       gmask[:].unsqueeze(3).to_broadcast([128, ntm, 4, 4]), ALU.add, [LG, GMASK], [ELM])
                    S.op("dve", lambda e: e.tensor_reduce(m1[:], elm[:], AX.X, ALU.max), [ELM], [M1])
                    tt("dve", mk1[:], elm[:], m1[:].to_broadcast([128, ntm, 16]), ALU.is_ge, [ELM, M1], [MK1])
                    stt("dve", elm[:], mk1[:], -BIG, elm[:], ALU.mult, ALU.add, [MK1, ELM], [ELM])
                    S.op("dve", lambda e: e.tensor_reduce(m2[:], elm[:], AX.X, ALU.max), [ELM], [M2])
                    tt("dve", mk2[:], elm[:], m2[:].to_broadcast([128, ntm, 16]), ALU.is_ge, [ELM, M2], [MK2])
                    tt("dve", w2[:], m2[:], m1[:], ALU.subtract, [M1, M2], [W2])
                    act(w2[:], w2[:], AF.Exp, [W2], [W2])
                    ts1("dve", w1[:], w2[:], 1.0, ALU.add, [W2], [W1])
                    S.op("dve", lambda e: e.reciprocal(w1[:], w1[:]), [W1], [W1])
                    tt("dve", w2[:], w2[:], w1[:], ALU.mult, [W1, W2], [W2])
                    tt("dve", w1[:], w1[:], gw[:], ALU.mult, [W1, GW], [W1])
                    tt("dve", w2[:], w2[:], gw[:], ALU.mult, [W2, GW], [W2])
                    tt("dve", mk1[:], mk1[:], w1[:].to_broadcast([128, ntm, 16]), ALU.mult, [MK1, W1], [MK1])
                    tt("dve", mk2[:], mk2[:], w2[:].to_broadcast([128, ntm, 16]), ALU.mult, [MK2, W2], [MK2])
                    tt("dve", gates[:], mk1[:], mk2[:], ALU.add, [MK1, MK2], [GATES])
                    pg = PsRing(S, "pgt", 2, 128, F32, ph)
                    for ii in range(ntm):
                        i = tstart + ii
                        ps, PS = pg.next()
                        tr(ps[0:16, :], gates[:, ii, :], C(C_ID), [GATES, CST], [PS])
                        cp("act", gT[0:16, i * 128:(i + 1) * 128], ps[0:16, :], [PS], [GT_])
                    S.barrier()
                    S.build()
                acc, ACC = S.sb("acc", [128, NT, D], F32, bp)
                tiles = [tt_ for tt_ in TTILES if tt_[0] >= tstart * 128]
                with ExitStack() as ph:
                    wgr = Ring(S, "sb", "wg", [128, KD, FF], BF16, 2, ph)
                    wur = Ring(S, "sb", "wu", [128, KD, FF], BF16, 2, ph)
                    wdr = Ring(S, "sb", "wd", [128, 4, D], BF16, 2, ph)
                    gselr = Ring(S, "sb", "gsel", [16, 512], F32, 2, ph)
                    gbr = Ring(S, "sb", "gbs", [128, 512], F32, 2, ph)
                    sgr = Ring(S, "sb", "sg", [128, 512], F32, 2, ph)
                    t1r = Ring(S, "sb", "t1", [128, 512], F32, 2, ph)
                    atr = Ring(S, "sb", "aT", [128, 4, 512], BF16, 2, ph)
                    pin = PsRing(S, "pinm", 4, 512, F32, ph)
                    pyr = PsRing(S, "pym", 2, 512, F32, ph)
                    pgb = PsRing(S, "pgb", 1, 512, F32, ph)
                    for e_ in range(NE):
                        wg, WG = wgr.next()
                        wu, WU = wur.next()
                        wd, WD = wdr.next()
                        S.dma("pool", wg[:], wg_d[l, e_].rearrange("(k p) f -> p k f", p=128), writes=[WG])
                        S.dma("pool", wu[:], wu_d[l, e_].rearrange("(k p) f -> p k f", p=128), writes=[WU])
                        S.dma("pool", wd[:], wd_d[l, e_].rearrange("(k p) n -> p k n", p=128), writes=[WD])
                        for (s, n) in tiles:
                            gsel, GSEL = gselr.next()
                            ts1("pool", gsel[0:16, 0:n], gT[0:16, s:s + n], cst[0:16, C_ID * 128 + e_:C_ID * 128 + e_ + 1],
                                ALU.mult, [GT_, CST], [GSEL])
                            pb_, PB_ = pgb.next()
                            mm(pb_[:, 0:n], cst[0:16, C_ONES * 128:C_ONES * 128 + 128], gsel[0:16, 0:n], True, True,
                               [CST, GSEL], [PB_])
                            gbs, GBS = gbr.next()
                            cp("act", gbs[:, 0:n], pb_[:, 0:n], [PB_], [GBS])
                            aT, AT = atr.next()
                            for f in range(4):
                                pg_, PG_ = pin.next()
                                for k in range(KD):
                                    mm(pg_[:, 0:n], wg[:, k, f * 128:(f + 1) * 128], hT[:, k, s:s + n], k == 0, k == KD - 1,
                                       [WG, HT], [PG_])
                                pu_, PU_ = pin.next()
                                for k in range(KD):
                                    mm(pu_[:, 0:n], wu[:, k, f * 128:(f + 1) * 128], hT[:, k, s:s + n], k == 0, k == KD - 1,
                                       [WU, HT], [PU_])
                                sg, SG = sgr.next()
                                act(sg[:, 0:n], pg_[:, 0:n], AF.Silu, [PG_], [SG])
                                t1, T1 = t1r.next()
                                tt("dve", t1[:, 0:n], pu_[:, 0:n], sg[:, 0:n], ALU.mult, [PU_, SG], [T1])
                                tt("pool", aT[:, f, 0:n], t1[:, 0:n], gbs[:, 0:n], ALU.mult, [T1, GBS], [AT])
                            for j in range(n // 128):
                                i = s // 128 + j
                                for hf in range(2):
                                    py, PY = pyr.next()
                                    for f in range(4):
                                        mm(py[:, :], aT[:, f, j * 128:(j + 1) * 128], wd[:, f, hf * 512:(hf + 1) * 512],
                                           f == 0, f == 3, [AT, WD], [PY])
                                    a_ = acc[:, i, hf * 512:(hf + 1) * 512]
                                    if e_ == 0:
                                        cp("act", a_, py[:, :], [PY], [ACC])
                                    else:
                                        tt("dve", a_, a_, py[:, :], ALU.add, [ACC, PY], [ACC])
                    S.barrier()
                    S.build()
                with ExitStack() as ph:
                    pin = PsRing(S, "pinr", 2, 512, F32, ph)
                    Gx, GX = gbcast(ph, L, b, 1, pin)
                    Gc, GC = (None, None) if tstart > 0 else gbcast(ph, L, 2, 1, pin)
                    xr = Ring(S, "sb", "xm", [128, D], F32, 3, ph)
                    tr_ = Ring(S, "sb", "tm", [128, D], F32, 2, ph)
                    if last:
                        fnw, FNW = S.sb("fnw", [128, D], F32, ph)
                        S.dma("sp", fnw[:], fnw_d, writes=[FNW])
                        junk, JUNK = S.sb("junkf", [128, D], BF16, ph)
                        str_ = Ring(S, "sb", "stf", [128, 4], F32, 3, ph)
                    for i in range(tstart, NT):
                        x, X = xr.next()
                        S.dma("sp", x[:], mid[b, i * 128:(i + 1) * 128, :], reads=[MID], writes=[X])
                        G, GD = (Gc, GC) if i < 2 else (Gx, GX)
                        t_, TD_ = tr_.next()
                        tt("dve", t_[:], acc[:, i, :], G[:], ALU.mult, [ACC, GD], [TD_])
                        tt("pool", x[:], x[:], t_[:], ALU.add, [X, TD_], [X])
                        if not last:
                            S.dma("sp", dst[b, i * 128:(i + 1) * 128, :], x[:], reads=[X], writes=[DST])
                        else:
                            st, ST = str_.next()
                            act(junk[:], x[:], AF.Square, [X], [JUNK, ST], accum_out=st[:, 0:1])
                            act(st[:, 1:2], st[:, 0:1], AF.Sqrt, [ST, EPSC], [ST], scale=1.0 / D, bias=epsc[:, 0:1])
                            S.op("dve", lambda e, st=st: e.reciprocal(st[:, 2:3], st[:, 1:2]), [ST], [ST])
                            stt("dve", t_[:], x[:], st[:, 2:3], fnw[:], ALU.mult, ALU.mult, [X, ST, FNW], [TD_])
                            S.dma("sp", out_d[b, (i - 2) * 128:(i - 1) * 128, :], t_[:], reads=[TD_], writes=[ROUT[b]])
                    S.barrier()
                    S.build()

        def run_pass(L, b, last, tstart, src, SRC, mid, MID, dst, DST):
            with ExitStack() as bp:
                hT, HT = S.sb("hT", [128, KD, T], BF16, bp)
                yT, YT = S.sb("yT", [128, KD, T], BF16, bp)
                norm_phase(L, b, src, SRC, 0, hT, HT, 0)
                if "hT" in dbg_out and L.l == 0 and b == 0:
                    dd = Dep("dbg_hT")
                    S.dma("pool", dbg_out["hT"].rearrange("p (k t) -> p k t", k=KD), hT[:], reads=[HT], writes=[dd])
                if stop == "A":
                    S.barrier()
                    S.build()
                    return
                with ExitStack() as scs:
                    SC = small_cols(L, b, hT, HT, scs)
                    if chk("SC"):
                        return
                    for hp in range(2):
                        gdn_headpair(L, b, hp, hT, HT, yT, YT, SC)
                        if STOPPED[0]:
                            return
                    S.barrier()
                    S.build()
                for hp in range(2):
                    hgrn_headpair(L, b, hp, hT, HT, yT, YT)
                    if STOPPED[0]:
                        return
                if "yT" in dbg_out and L.l == 0 and b == 0:
                    dd = Dep("dbg_yT")
                    S.dma("pool", dbg_out["yT"].rearrange("p (k t) -> p k t", k=KD), yT[:], reads=[YT], writes=[dd])
                if stop == "Y":
                    S.barrier()
                    S.build()
                    return
                outproj_phase(L, b, src, SRC, mid, MID, yT, YT, tstart)
            if stop == "M":
                return
            moe_phase(L, b, mid, MID, dst, DST, last, tstart)

        RXIN = Dep("rxin")
        RA = [Dep(f"resA{b}") for b in range(NB)]
        RB = [Dep(f"resB{b}") for b in range(NB)]
        ROUT = [Dep(f"rout{b}") for b in range(NB)]

        for l in range(nlayers):
          try:
            last = (l == DEPTH - 1)
            tstart = 2 if last else 0
            with ExitStack() as lay:
                L = NS()
                L.l = l
                L.modT, L.MODT = S.sb("modT", [128, 48, 3], F32, lay)
                L.abc, L.ABC = S.sb("abc", [128, 4, KD, 3], F32, lay)
                L.wr, L.WR = S.sb("wr", [128, KD, 20], BF16, lay)
                L.brow, L.BROW = S.sb("brow", [1, 20], F32, lay)
                S.dma("pool", L.wr[:], wr_d[l], writes=[L.WR])
                S.dma("sp", L.brow[:], br_d[l:l + 1, :], writes=[L.BROW])
                with ExitStack() as ph:
                    wmr = Ring(S, "sb", "wm", [128, KD, 512], F32, 2, ph)
                    bmr = Ring(S, "sb", "bm", [1, 512], F32, 2, ph)
                    pst = PsRing(S, "pmT", 2, 16, F32, ph)
                    for n in range(12):
                        wm, WM = wmr.next()
                        bm, BM = bmr.next()
                        S.dma("sp", wm[:], wmod_d[l].rearrange("(k p) n -> p k n", p=128)[:, :, n * 512:(n + 1) * 512],
                              writes=[WM])
                        S.dma("sp", bm[:], bmod_d[l:l + 1, n * 512:(n + 1) * 512], writes=[BM])
                        pt, PT = pst.next()
                        for sub in range(4):
                            o = pt[:, sub * 3:sub * 3 + 3]
                            for k in range(KD):
                                mm(o, wm[:, k, sub * 128:(sub + 1) * 128], scT[:, k, :], k == 0, False,
                                   [WM, SCT], [PT])
                            mm(o, bm[0:1, sub * 128:(sub + 1) * 128], cst[0:1, C_ONES * 128:C_ONES * 128 + 3],
                               False, True, [BM, CST], [PT])
                        cp("dve", L.modT[:, n * 4:(n + 1) * 4, :], pt[:, 0:12].rearrange("p (a b) -> p a b", b=3),
                           [PT], [L.MODT])
                    for wi, (csc, csh) in enumerate(((8, 0), (32, 24))):
                        ts1("dve", L.abc[:, 2 * wi, :, :], L.modT[:, csc:csc + 8, :], 1.0, ALU.add, [L.MODT], [L.ABC])
                        tt("dve", L.abc[:, 2 * wi, :, :], L.abc[:, 2 * wi, :, :],
                           normw[:, l, wi, :].unsqueeze(2).to_broadcast([128, KD, 3]), ALU.mult,
                           [L.ABC, NORMW], [L.ABC])
                        cp("dve", L.abc[:, 2 * wi + 1, :, :], L.modT[:, csh:csh + 8, :], [L.MODT], [L.ABC])
                    if "abc" in dbg_out and l == 0:
                        S.dma("sp", dbg_out["abc"], L.abc[:].rearrange("p a b c -> p (a b c)"), reads=[L.ABC], writes=[Dep("dbg_abc")])
                        S.dma("sp", dbg_out["modT"], L.modT[:].rearrange("p a b -> p (a b)"), reads=[L.MODT], writes=[Dep("dbg_modT")])
                        S.dma("sp", dbg_out["scT"], scT[:].rearrange("p a b -> p (a b)"), reads=[SCT], writes=[Dep("dbg_scT")])
                    S.barrier()
                    S.build()
                for b in range(nb_run):
                    src, SRC = (xin, RXIN) if l == 0 else (resB, RB[b])
                    run_pass(L, b, last, tstart, src, SRC, resA, RA[b], resB, RB[b])
                    if STOPPED[0]:
                        break
          except _Stop:
            break
          if STOPPED[0]:
            break
        if "res_out" in dbg_out:
            dd = Dep("dbg_res")
            for b in range(nb_run):
                if stop == "M":
                    S.dma("sp", dbg_out["res_out"][b], resA[b], reads=[RA[b]], writes=[dd])
                else:
                    S.dma("sp", dbg_out["res_out"][b], resB[b], reads=[RB[b]], writes=[dd])
        S.barrier()
        S.build()
    return nc


def make_consts():
    c = np.zeros((128, NCONST), np.float32)
    idx = np.arange(128)
    s = idx[:, None]
    t = idx[None, :]
    same = (s // 64) == (t // 64)

    def put(i, m):
        c[:, i * 128:(i + 1) * 128] = m.astype(np.float32)
    put(C_ID, s == t)
    put(C_ONES, np.ones((128, 128)))
    incf = same & (s <= t)
    incb = same & (s >= t)
    put(C_INCF, incf)
    put(C_STRF, same & (s < t))
    put(C_INCB, incb)
    put(C_STRB, same & (s > t))
    put(C_NEGF, np.where(incf, 0.0, -30000.0))
    put(C_NEGB, np.where(incb, 0.0, -30000.0))
    put(C_GTF, s > t)
    put(C_GTB, s < t)
    same32 = (s // 32) == (t // 32)
    put(C_INCF32, same32 & (s <= t))
    put(C_INCB32, same32 & (s >= t))
    return c


def prepare_shared(inp):
    f = np.float32
    sh = {}
    sh["consts"] = make_consts()
    sh["w_mod"] = np.ascontiguousarray(inp["w_mod"], dtype=f)
    sh["b_mod"] = np.ascontiguousarray(inp["b_mod"], dtype=f)
    nw = np.stack([inp["norm1_w"], inp["norm2_w"]], axis=1)
    sh["normw"] = np.ascontiguousarray(nw.reshape(DEPTH, 2, KD, 128).transpose(3, 0, 1, 2).reshape(128, -1), dtype=f)
    lb = inp["hg_lb"].reshape(DEPTH, 2, 4, 128)
    sh["lbh"] = np.ascontiguousarray(lb.transpose(3, 0, 1, 2).reshape(128, -1), dtype=f)
    hn = np.stack([inp["hg_norm_w"], inp["dn_norm_w"]], axis=1)
    sh["headnw"] = np.ascontiguousarray(hn.transpose(2, 0, 1).reshape(128, -1), dtype=f)
    cw = inp["dn_conv_w"].reshape(DEPTH, 9, 12, 128)
    sh["convw"] = np.ascontiguousarray(cw.transpose(3, 0, 2, 1).reshape(128, -1), dtype=f)
    al = np.concatenate([inp["dn_a_log"].reshape(DEPTH, 8), inp["dn_dt_bias"].reshape(DEPTH, 8)], axis=1)
    sh["alog"] = np.ascontiguousarray(np.broadcast_to(al.reshape(1, -1), (128, DEPTH * 16)), dtype=f)
    wrr = np.concatenate([inp["w_group"], inp["w_expert"]], axis=2)
    sh["wr"] = np.ascontiguousarray(wrr.reshape(DEPTH, KD, 128, 20).transpose(0, 2, 1, 3), dtype=f)
    sh["br"] = np.ascontiguousarray(np.concatenate([inp["b_group"], inp["b_expert"]], axis=1), dtype=f)
    sh["fnw"] = np.ascontiguousarray(np.broadcast_to(inp["final_norm_w"].reshape(1, D), (128, D)), dtype=f)
    wi = np.zeros((DEPTH, D, NGRP * 128), f)
    wi[:, :, :IN_COLS] = inp["w_in"]
    sh["win"] = np.ascontiguousarray(wi.reshape(DEPTH, KD, 128, NGRP, 128).transpose(0, 3, 2, 1, 4))
    sh["w_out"] = np.ascontiguousarray(inp["w_out"], dtype=f)
    sh["w_gate"] = np.ascontiguousarray(inp["w_gate"], dtype=f)
    sh["w_up"] = np.ascontiguousarray(inp["w_up"], dtype=f)
    sh["w_down"] = np.ascontiguousarray(inp["w_down"], dtype=f)
    return sh


def prepare_core(inp, core):
    f = np.float32
    b0 = core * NB
    m = {}
    m["xin"] = np.ascontiguousarray(np.concatenate([inp["ctx"][b0:b0 + NB], inp["x"][b0:b0 + NB]], axis=1), dtype=f)
    cv = np.concatenate([inp["c"][b0:b0 + NB], inp["c_ctx"].reshape(1, D)], axis=0)
    m["cvec"] = np.ascontiguousarray(cv.reshape(3, KD, 128).transpose(2, 1, 0), dtype=f)
    return m


_CACHE = {}


def kernel(**inputs):
    inp = {k: np.asarray(v) for k, v in inputs.items()}
    n_cores = 8
    if "nc" not in _CACHE:
        _CACHE["nc"] = build_program(DEPTH)
    nc = _CACHE["nc"]
    shared = prepare_shared(inp)
    in_maps = []
    for core in range(n_cores):
        m = dict(shared)
        m.update(prepare_core(inp, core))
        in_maps.append(m)
    res = run_bass_kernel_spmd(nc, in_maps, core_ids=list(range(n_cores)))
    out = np.concatenate([np.asarray(r["out"], dtype=np.float32) for r in res.results], axis=0)
    return out
```

```python
import numpy as np
from contextlib import ExitStack
import concourse.bass as bass
import concourse.mybir as mybir
from concourse.bass_utils import run_bass_kernel_spmd

F32 = mybir.dt.float32
BF16 = mybir.dt.bfloat16
ALU = mybir.AluOpType
AF = mybir.ActivationFunctionType
AX = mybir.AxisListType

D = 1024
KD = 8
DEPTH = 4
NB = 2
TC = 256
TL = 2048
T = TC + TL
NT = T // 128
GRID_W = 64
IN_COLS = 4624
NGRP = 37
FF = 512
NE = 16
EPS = 1e-6
HG_Q, HG_I, HG_G, HG_FF, HG_FB = 0, 512, 1024, 1536, 2048
DN_Q, DN_K, DN_V, DN_Z = 2560, 3072, 3584, 4096
SMALL0 = 4608
C_ID, C_ONES, C_INCF, C_STRF, C_INCB, C_STRB, C_NEGF, C_NEGB, C_GTF, C_GTB, C_INCF32, C_INCB32 = range(12)
NCONST = 12 * 128
TTILES = [(0, 256), (256, 512), (768, 512), (1280, 512), (1792, 512)]


class Dep:
    __slots__ = ("name", "lw", "rd", "sem", "semv", "excl")

    def __init__(self, name, excl=False):
        self.name = name
        self.excl = excl
        self.lw = None
        self.rd = {}
        self.sem = None
        self.semv = 0


class Sched:
    ENGS = ("pe", "dve", "act", "pool", "sp")

    def __init__(self, nc, stack):
        self.nc = nc
        self.stack = stack
        self.q = {e: [] for e in self.ENGS}
        self.cnt = {e: 0 for e in self.ENGS}
        self.seen = {}
        for e in self.ENGS:
            self.seen[e] = {}
            self.seen["dmaq_" + e] = {}
        self.esem = {e: stack.enter_context(nc.semaphore("prog_" + e)) for e in self.ENGS}
        self.dsems = []
        self.free_sems = []
        self.nsem_alloc = 0
        self.ninst = 0
        self.uid = 0

    def sb(self, name, shape, dt, stack=None):
        self.uid += 1
        nm = f"{name}_{self.uid}"
        t = (stack or self.stack).enter_context(self.nc.sbuf_tensor(nm, list(shape), dt))
        return t, Dep(nm)

    def ps(self, name, shape, dt, stack=None):
        self.uid += 1
        nm = f"{name}_{self.uid}"
        t = (stack or self.stack).enter_context(self.nc.psum_tensor(nm, list(shape), dt))
        return t, Dep(nm)

    def _dsem(self, d):
        if d.sem is None:
            if self.free_sems:
                d.sem, d.semv = self.free_sems.pop()
            else:
                self.nsem_alloc += 1
                d.sem = self.stack.enter_context(self.nc.semaphore("dsem%d" % self.nsem_alloc))
                d.semv = 0
            self.dsems.append(d)
        return d.sem

    def _need(self, eng, ev, waits, is_dma, raw=False):
        if ev is None:
            return
        key, val, sem = ev
        if key == eng and not is_dma and not (raw and eng != "pe"):
            return
        k2 = ("dmaq_" + eng) if is_dma else eng
        if self.seen[k2].get(key, 0) >= val:
            return
        self.seen[k2][key] = val
        if not is_dma:
            pass
        waits.append((sem, val))

    def _collect(self, eng, reads, writes, is_dma=False):
        waits = []
        for d in reads:
            self._need(eng, d.lw, waits, is_dma, raw=True)
        for d in writes:
            self._need(eng, d.lw, waits, is_dma)
            for ev in d.rd.values():
                self._need(eng, ev, waits, is_dma)
        return waits

    def _mark(self, ev, reads, writes):
        for d in reads:
            old = d.rd.get(ev[0])
            if old is None or old[1] < ev[1]:
                d.rd[ev[0]] = ev
        for d in writes:
            d.lw = ev
            d.rd = {}

    def op(self, eng, fn, reads=(), writes=()):
        if any(d.excl for d in reads):
            writes = list(writes) + [d for d in reads if d.excl]
            reads = [d for d in reads if not d.excl]
        waits = self._collect(eng, reads, writes)
        self.cnt[eng] += 1
        ev = (eng, self.cnt[eng], self.esem[eng])
        self._mark(ev, reads, writes)
        self.q[eng].append((waits, fn, self.esem[eng], 1))
        self.ninst += 1

    def dma(self, eng, out, in_, reads=(), writes=(), **kw):
        waits = self._collect(eng, reads, writes, True)
        anchor = (list(writes) + list(reads))[0]
        sem = self._dsem(anchor)
        anchor.semv += 16
        ev = ("dma_%d" % id(sem), anchor.semv, sem)
        self._mark(ev, reads, writes)
        self.q[eng].append((waits, lambda e: e.dma_start(out=out, in_=in_, **kw), sem, 16))
        self.ninst += 1

    def barrier(self):
        for e in self.ENGS:
            waits = []
            for o in self.ENGS:
                if o != e and self.cnt[o] > self.seen[e].get(o, 0):
                    self.seen[e][o] = self.cnt[o]
                    waits.append((self.esem[o], self.cnt[o]))
            for d in self.dsems:
                key = "dma_%d" % id(d.sem)
                if d.semv > self.seen[e].get(key, 0):
                    self.seen[e][key] = d.semv
                    waits.append((d.sem, d.semv))
            for k, v in self.seen[e].items():
                if self.seen["dmaq_" + e].get(k, 0) < v:
                    self.seen["dmaq_" + e][k] = v
            self.q[e].append((waits, None, None, 0))
        for d in self.dsems:
            self.free_sems.append((d.sem, d.semv))
            d.sem = None
        self.dsems = []

    def build(self):
        nc = self.nc
        with nc.Block() as block:
            def run(eng_name):
                def body(e):
                    for waits, fn, sem, inc in self.q[eng_name]:
                        if fn is None:
                            for (s, v) in waits:
                                e.wait_ge(s, v)
                            continue
                        for (s, v) in waits[:-1]:
                            e.wait_ge(s, v)
                        ins = fn(e)
                        if waits:
                            ins._wait_ge(waits[-1][0], waits[-1][1])
                        ins.then_inc(sem, inc)
                return body
            block.tensor(run("pe"))
            block.vector(run("dve"))
            block.scalar(run("act"))
            block.gpsimd(run("pool"))
            block.sync(run("sp"))
        self.q = {e: [] for e in self.ENGS}


class Ring:
    def __init__(self, S, kind, name, shape, dt, n, stack):
        mk = S.sb if kind == "sb" else S.ps
        self.items = [mk(f"{name}{i}", shape, dt, stack) for i in range(n)]
        self.i = 0

    def next(self):
        it = self.items[self.i % len(self.items)]
        self.i += 1
        return it


class PsRing:
    def __init__(self, S, name, nbanks, width, dt, stack):
        per = (2048 // (4 if dt == F32 else 2))
        banks = []
        for b in range(nbanks):
            t, _ = S.ps(f"{name}{b}", [128, per], dt, stack)
            banks.append((t, Dep(f"{name}{b}", excl=True)))
        self.items = []
        for j in range(per // width):
            for (t, dep) in banks:
                self.items.append((t[:, j * width:(j + 1) * width], dep))
        self.i = 0

    def next(self):
        it = self.items[self.i % len(self.items)]
        self.i += 1
        return it


def roundrobin(gens):
    gens = list(gens)
    while gens:
        for g in list(gens):
            try:
                next(g)
            except StopIteration:
                gens.remove(g)


def build_program(nlayers=DEPTH, dbg=(), stop=None, nb_run=NB):
    nc = bass.Bass("TRN2", target_bir_lowering=False)

    def din(name, shape):
        return nc.dram_tensor(name, list(shape), F32, kind="ExternalInput").ap()

    xin = din("xin", [NB, T, D])
    cvec_d = din("cvec", [128, KD, 3])
    consts_d = din("consts", [128, NCONST])
    wmod_d = din("w_mod", [DEPTH, D, 6 * D])
    bmod_d = din("b_mod", [DEPTH, 6 * D])
    normw_d = din("normw", [128, DEPTH * 2 * KD])
    lb_d = din("lbh", [128, DEPTH * 8])
    headnw_d = din("headnw", [128, DEPTH * 2])
    convw_d = din("convw", [128, DEPTH * 12 * 9])
    alog_d = din("alog", [128, DEPTH * 16])
    wr_d = din("wr", [DEPTH, 128, KD, 20])
    br_d = din("br", [DEPTH, 20])
    fnw_d = din("fnw", [128, D])
    win_d = din("win", [DEPTH, NGRP, 128, KD, 128])
    wout_d = din("w_out", [DEPTH, D, D])
    wg_d = din("w_gate", [DEPTH, NE, D, FF])
    wu_d = din("w_up", [DEPTH, NE, D, FF])
    wd_d = din("w_down", [DEPTH, NE, FF, D])
    out_d = nc.dram_tensor("out", [NB, TL, D], F32, kind="ExternalOutput").ap()
    resA = nc.dram_tensor("resA", [NB, T, D], F32, kind="Internal").ap()
    resB = nc.dram_tensor("resB", [NB, T, D], F32, kind="Internal").ap()
    dbg_out = {}
    for name, shape in dbg:
        dbg_out[name] = nc.dram_tensor(name, list(shape), F32, kind="ExternalOutput").ap()

    with ExitStack() as top:
        S = Sched(nc, top)

        def mm(out, lhsT, rhs, st, sp, R, W):
            S.op("pe", lambda e: e.matmul(out, lhsT=lhsT, rhs=rhs, start=st, stop=sp), R, W)

        def tr(out, in_, ident, R, W):
            S.op("pe", lambda e: e.transpose(out, in_, ident), R, W)

        def act(out, in_, func, R, W, **kw):
            S.op("act", lambda e: e.activation(out, in_, func, **kw), R, W)

        def tt(eng, out, a, b, op, R, W):
            S.op(eng, lambda e: e.tensor_tensor(out, a, b, op), R, W)

        def ts1(eng, out, a, s, op, R, W):
            S.op(eng, lambda e: e.tensor_single_scalar(out, a, s, op), R, W)

        def ts2(eng, out, a, s1, s2, op0, op1, R, W):
            S.op(eng, lambda e: e.tensor_scalar(out, a, s1, s2, op0, op1), R, W)

        def stt(eng, out, a, s, b, op0, op1, R, W):
            S.op(eng, lambda e: e.scalar_tensor_tensor(out, a, s, b, op0, op1), R, W)

        def cp(eng, out, a, R, W):
            if eng == "act":
                S.op("act", lambda e: e.copy(out, a), R, W)
            else:
                S.op(eng, lambda e: e.tensor_copy(out, a), R, W)

        def dump(name, ap_sb, DEP, dram_ap=None):
            if name in dbg_out:
                dd = Dep("dbg_" + name)
                S.dma("sp", dram_ap if dram_ap is not None else dbg_out[name], ap_sb, reads=[DEP], writes=[dd])

        cst, CST = S.sb("cst", [128, NCONST], F32)
        S.dma("sp", cst[:], consts_d, writes=[CST])

        def C(i):
            return cst[:, i * 128:(i + 1) * 128]

        idb, IDB = S.sb("idb", [128, 128], BF16)
        onb, ONB = S.sb("onb", [128, 128], BF16)
        cp("dve", idb[:], C(C_ID), [CST], [IDB])
        cp("dve", onb[:], C(C_ONES), [CST], [ONB])
        epsc, EPSC = S.sb("epsc", [128, 1], F32)
        S.op("dve", lambda e: e.memset(epsc[:], EPS), (), [EPSC])
        normw, NORMW = S.sb("normw", [128, DEPTH, 2, KD], F32)
        S.dma("sp", normw[:].rearrange("p a b c -> p (a b c)"), normw_d, writes=[NORMW])
        headnw, HEADNW = S.sb("headnw", [128, DEPTH, 2], F32)
        S.dma("sp", headnw[:].rearrange("p a b -> p (a b)"), headnw_d, writes=[HEADNW])
        convw, CONVW = S.sb("convw", [128, DEPTH, 12, 9], F32)
        S.dma("sp", convw[:].rearrange("p a b c -> p (a b c)"), convw_d, writes=[CONVW])
        alog, ALOG = S.sb("alog", [128, DEPTH, 16], F32)
        S.dma("sp", alog[:].rearrange("p a b -> p (a b)"), alog_d, writes=[ALOG])
        act(alog[:, :, 0:8], alog[:, :, 0:8], AF.Exp, [ALOG], [ALOG])
        ts1("dve", alog[:, :, 0:8], alog[:, :, 0:8], -1.0, ALU.mult, [ALOG], [ALOG])
        scT, SCT = S.sb("scT", [128, KD, 3], F32)
        S.dma("sp", scT[:].rearrange("p a b -> p (a b)"), cvec_d.rearrange("p a b -> p (a b)"), writes=[SCT])
        act(scT[:], scT[:], AF.Silu, [SCT], [SCT])
        lbt, LBT = S.sb("lbt", [128, DEPTH, 8], F32)
        oml, OML = S.sb("oml", [128, DEPTH, 8], F32)
        with ExitStack() as ph:
            raw, RAWL = S.sb("lbraw", [128, DEPTH, 8], F32, ph)
            m8, M8 = S.sb("lbm", [128, 8], F32, ph)
            S.dma("sp", raw[:].rearrange("p a b -> p (a b)"), lb_d, writes=[RAWL])
            tt("dve", m8[:], raw[:, 0, :], raw[:, 1, :], ALU.max, [RAWL], [M8])
            tt("dve", m8[:], m8[:], raw[:, 2, :], ALU.max, [RAWL, M8], [M8])
            tt("dve", m8[:], m8[:], raw[:, 3, :], ALU.max, [RAWL, M8], [M8])
            for l in range(DEPTH):
                tt("dve", raw[:, l, :], raw[:, l, :], m8[:], ALU.subtract, [RAWL, M8], [RAWL])
            act(raw[:], raw[:], AF.Exp, [RAWL], [RAWL])
            tt("dve", m8[:], raw[:, 0, :], raw[:, 1, :], ALU.add, [RAWL], [M8])
            tt("dve", m8[:], m8[:], raw[:, 2, :], ALU.add, [RAWL, M8], [M8])
            tt("dve", m8[:], m8[:], raw[:, 3, :], ALU.add, [RAWL, M8], [M8])
            S.op("dve", lambda e: e.reciprocal(m8[:], m8[:]), [M8], [M8])
            for l in range(DEPTH):
                tt("dve", raw[:, l, :], raw[:, l, :], m8[:], ALU.mult, [RAWL, M8], [RAWL])
            S.op("dve", lambda e: e.memset(lbt[:, 0, :], 0.0), (), [LBT])
            cp("dve", lbt[:, 1, :], raw[:, 1, :], [RAWL], [LBT])
            tt("dve", lbt[:, 2, :], lbt[:, 1, :], raw[:, 2, :], ALU.add, [RAWL, LBT], [LBT])
            tt("dve", lbt[:, 3, :], lbt[:, 2, :], raw[:, 3, :], ALU.add, [RAWL, LBT], [LBT])
            ts2("dve", oml[:], lbt[:], -1.0, 1.0, ALU.mult, ALU.add, [LBT], [OML])
            S.barrier()
            S.build()

        class NS:
            pass

        class _Stop(Exception):
            pass

        STOPPED = [False]

        def chk(tag):
            if stop == tag and not STOPPED[0]:
                S.barrier()
                S.build()
                STOPPED[0] = True
            return STOPPED[0]

        ORDER = {0: list(range(NT)), 1: [1, 0] + list(range(NT - 1, 1, -1))}

        def dmask(d):
            return (C(C_INCF), C(C_STRF), C(C_NEGF), C(C_GTF)) if d == 0 else \
                   (C(C_INCB), C(C_STRB), C(C_NEGB), C(C_GTB))

        def gbcast(ph, L, r, w, psr):
            G, GD = S.sb("G", [128, D], F32, ph)
            lr = Ring(S, "sb", "gl", [128, 128], F32, 2, ph)
            base = (2 + 3 * w) * 8
            for hf in range(2):
                ps, PS = psr.next()
                for k4 in range(4):
                    k = hf * 4 + k4
                    lh, LH = lr.next()
                    cp("dve", lh[:], L.modT[:, base + k, r:r + 1].to_broadcast([128, 128]), [L.MODT], [LH])
                    mm(ps[:, k4 * 128:(k4 + 1) * 128], lh[:], C(C_ID), True, True, [LH, CST], [PS])
                cp("act", G[:, hf * 512:(hf + 1) * 512], ps[:, :], [PS], [GD])
            return G, GD

        def norm_phase(L, b, src, SRC, wi, hT, HT, tstart):
            with ExitStack() as ph:
                xr = Ring(S, "sb", "xt", [128, D], F32, 3, ph)
                xnr = Ring(S, "sb", "xn", [128, D], BF16, 3, ph)
                junk, JUNK = S.sb("junk", [128, D], BF16, ph)
                stt_r = Ring(S, "sb", "st", [128, 4], F32, 4, ph)
                ptr = PsRing(S, "ptr", 4, 1024, BF16, ph)
                tmpr = Ring(S, "sb", "mt", [128, KD, 128], F32, 3, ph)
                for i in range(tstart, NT):
                    x, X = xr.next()
                    S.dma("sp", x[:], src[b, i * 128:(i + 1) * 128, :], reads=[SRC], writes=[X])
                    st, ST = stt_r.next()
                    act(junk[:], x[:], AF.Square, [X], [JUNK, ST], accum_out=st[:, 0:1])
                    act(st[:, 1:2], st[:, 0:1], AF.Sqrt, [ST, EPSC], [ST], scale=1.0 / D, bias=epsc[:, 0:1])
                    S.op("dve", lambda e, st=st: e.reciprocal(st[:, 2:3], st[:, 1:2]), [ST], [ST])
                    xn, XN = xnr.next()
                    act(xn[:], x[:], AF.Copy, [X, ST], [XN], scale=st[:, 2:3])
                    r = 2 if i < 2 else b
                    pp, PP = ptr.next()
                    for k in range(KD):
                        tr(pp[:, k * 128:(k + 1) * 128], xn[:, k * 128:(k + 1) * 128], idb[:], [XN, IDB], [PP])
                    m, M = tmpr.next()
                    A = L.abc[:, 2 * wi, :, r:r + 1].to_broadcast([128, KD, 128])
                    B = L.abc[:, 2 * wi + 1, :, r:r + 1].to_broadcast([128, KD, 128])
                    tt("dve", m[:], pp.rearrange("p (a b) -> p a b", b=128), A, ALU.mult, [PP, L.ABC], [M])
                    tt("pool", hT[:, :, i * 128:(i + 1) * 128], m[:], B, ALU.add, [M, L.ABC], [HT])
                S.barrier()
                S.build()

        def inproj(L, hT, HT, wring, pin, g, evac):
            wb, WB = wring.next()
            S.dma("pool", wb[:], win_d[L.l, g], writes=[WB])
            for (s, n) in TTILES:
                ps, PS = pin.next()
                for k in range(KD):
                    mm(ps[:, 0:n], wb[:, k, :], hT[:, k, s:s + n], k == 0, k == KD - 1, [WB, HT], [PS])
                evac(ps, PS, s, n)

        def conv(eng, raw, RAW, acc, ACC, wcol):
            rl = raw[:, TC:T].rearrange("p (r c) -> p r c", c=GRID_W)
            al = acc[:, TC:T].rearrange("p (r c) -> p r c", c=GRID_W)
            ts1(eng, acc[:, TC:T], raw[:, TC:T], wcol(4), ALU.mult, [RAW, CONVW], [ACC])
            for a in range(3):
                for b3 in range(3):
                    if a == 1 and b3 == 1:
                        continue
                    dr, dc = a - 1, b3 - 1
                    r0, r1 = max(0, -dr), 32 - max(0, dr)
                    c0, c1 = max(0, -dc), GRID_W - max(0, dc)
                    stt(eng, al[:, r0:r1, c0:c1], rl[:, r0 + dr:r1 + dr, c0 + dc:c1 + dc], wcol(a * 3 + b3),
                        al[:, r0:r1, c0:c1], ALU.mult, ALU.add, [RAW, ACC, CONVW], [ACC])
            ts1(eng, acc[:, 0:TC], raw[:, 0:TC], wcol(4), ALU.mult, [RAW, CONVW], [ACC])
            stt(eng, acc[:, 1:TC], raw[:, 0:TC - 1], wcol(3), acc[:, 1:TC], ALU.mult, ALU.add, [RAW, ACC, CONVW], [ACC])
            stt(eng, acc[:, 0:TC - 1], raw[:, 1:TC], wcol(5), acc[:, 0:TC - 1], ALU.mult, ALU.add,
                [RAW, ACC, CONVW], [ACC])

        def headnorm(ph, L, oacc, OACC, ZS, ZSD, which, hp, yT, YT, pss):
            sqr = Ring(S, "sb", "hsq", [128, 512], BF16, 2, ph)
            rtr = Ring(S, "sb", "hrt", [128, 512], F32, 2, ph)
            tfr = Ring(S, "sb", "htf", [128, 512], F32, 2, ph)
            for hh in range(2):
                for (s, n) in TTILES:
                    sq, SQ = sqr.next()
                    act(sq[:, 0:n], oacc[:, hh, s:s + n], AF.Square, [OACC], [SQ])
                    ps, PS = pss.next()
                    mm(ps[:, 0:n], onb[:], sq[:, 0:n], True, True, [ONB, SQ], [PS])
                    rt, RT = rtr.next()
                    act(rt[:, 0:n], ps[:, 0:n], AF.Sqrt, [PS, EPSC], [RT], scale=1.0 / 128, bias=epsc[:, 0:1])
                    S.op("dve", lambda e, rt=rt, n=n: e.reciprocal(rt[:, 0:n], rt[:, 0:n]), [RT], [RT])
                    tf, TF = tfr.next()
                    stt("dve", tf[:, 0:n], oacc[:, hh, s:s + n], headnw[:, L.l, which:which + 1], rt[:, 0:n],
                        ALU.mult, ALU.mult, [OACC, HEADNW, RT], [TF])
                    tt("pool", yT[:, which * 4 + 2 * hp + hh, s:s + n], tf[:, 0:n], ZS[:, hh, s:s + n], ALU.mult,
                       [TF, ZSD], [YT])

        def small_cols(L, b, hT, HT, bp):
            P_ = NS()
            P_.beta, P_.BETA = S.sb("beta", [128, NT, 8], F32, bp)
            P_.la, P_.LA = S.sb("la", [128, NT, 8], F32, bp)
            P_.ecols, P_.ECOLS = S.sb("ecols", [128, NT, 16], F32, bp)
            P_.er, P_.ER = S.sb("er", [128, NT, 2, 8], F32, bp)
            P_.ncum, P_.NCUM = S.sb("ncum", [128, NT, 8], F32, bp)
            with ExitStack() as ph:
                wb, WB = S.sb("wbs", [128, KD, 128], BF16, ph)
                S.dma("pool", wb[:], win_d[L.l, NGRP - 1], writes=[WB])
                ba, BA = S.sb("ba", [128, NT, 16], F32, ph)
                pr = PsRing(S, "pba", 2, 16, F32, ph)
                for i in range(NT):
                    ps, PS = pr.next()
                    for k in range(KD):
                        mm(ps, hT[:, k, i * 128:(i + 1) * 128], wb[:, k, 0:16], k == 0, k == KD - 1, [HT, WB], [PS])
                    cp("dve", ba[:, i, :], ps, [PS], [BA])
                act(P_.beta[:], ba[:, :, 0:8], AF.Sigmoid, [BA], [P_.BETA])
                tt("dve", P_.la[:], ba[:, :, 8:16], alog[:, L.l, 8:16].unsqueeze(1).to_broadcast([128, NT, 8]), ALU.add,
                   [BA, ALOG], [P_.LA])
                act(P_.la[:], P_.la[:], AF.Exp, [P_.LA], [P_.LA])
                act(P_.la[:], P_.la[:], AF.Ln, [P_.LA], [P_.LA], bias=1.0)
                tt("dve", P_.la[:], P_.la[:], alog[:, L.l, 0:8].unsqueeze(1).to_broadcast([128, NT, 8]), ALU.mult,
                   [P_.LA, ALOG], [P_.LA])
                pc = PsRing(S, "pcol", 2, 16, F32, ph)
                for i in range(NT):
                    ps, PS = pc.next()
                    for d in range(2):
                        INC, STR, NEG, GT = dmask(d)
                        STRO = dmask(1 - d)[1]
                        mm(ps[:, d * 4:d * 4 + 4], INC, P_.la[:, i, d * 4:d * 4 + 4], True, True, [CST, P_.LA], [PS])
                        mm(ps[:, 8 + d * 4:8 + d * 4 + 4], STRO, P_.la[:, i, d * 4:d * 4 + 4], True, True,
                           [CST, P_.LA], [PS])
                    ts1("dve", P_.ncum[:, i, :], ps[:, 0:8], -1.0, ALU.mult, [PS], [P_.NCUM])
                    act(P_.ecols[:, i, :], ps, AF.Exp, [PS], [P_.ECOLS])
                ts1("dve", P_.ecols[:, :, 0:8], P_.ecols[:, :, 0:8], -1.0, ALU.mult, [P_.ECOLS], [P_.ECOLS])
                ts1("dve", P_.er[:, :, 0, :], P_.ecols[:, :, 8:16], cst[:, C_INCF * 128 + 63:C_INCF * 128 + 64], ALU.mult,
                    [P_.ECOLS, CST], [P_.ER])
                ts1("dve", P_.er[:, :, 1, :], P_.ecols[:, :, 8:16], cst[:, C_INCB * 128 + 64:C_INCB * 128 + 65], ALU.mult,
                    [P_.ECOLS, CST], [P_.ER])
                S.barrier()
                S.build()
            return P_

        def gdn_headpair(L, b, hp, hT, HT, yT, YT, SC):
            l = L.l
            with ExitStack() as ph:
                QT, QTD = S.sb("QT", [128, 2, T], BF16, ph)
                KT, KTD = S.sb("KT", [128, 2, T], BF16, ph)
                VT, VTD = S.sb("VT", [128, 2, T], BF16, ph)
                ZS, ZSD = S.sb("ZS", [128, 2, T], BF16, ph)
                oacc, OACC = S.sb("oacc", [128, 2, T], F32, ph)
                S.op("pool", lambda e: e.memset(oacc[:], 0.0), (), [OACC])
                with ExitStack() as ph2:
                    rawr = Ring(S, "sb", "raw", [128, T], F32, 1, ph2)
                    caccr = Ring(S, "sb", "cacc", [128, T], F32, 1, ph2)
                    sq32, SQ32 = S.sb("sq32", [128, T], F32, ph2)
                    sqb, SQB = S.sb("sqb", [128, T], BF16, ph2)
                    rt, RT = S.sb("rt", [128, T], F32, ph2)
                    wring = Ring(S, "sb", "wb", [128, KD, 128], BF16, 3, ph2)
                    pin = PsRing(S, "pin", 3, 512, F32, ph2)
                    pss = PsRing(S, "pss", 2, 512, F32, ph2)
                    gi = 0
                    for kind, colbase in (("z", DN_Z), ("v", DN_V), ("q", DN_Q), ("k", DN_K)):
                        for hh in range(2):
                            h = 2 * hp + hh
                            g = colbase // 128 + h
                            if kind == "z":
                                inproj(L, hT, HT, wring, pin, g,
                                       lambda ps, PS, s, n, hh=hh: act(ZS[:, hh, s:s + n], ps[:, 0:n], AF.Silu, [PS], [ZSD]))
                                continue
                            raw, RAW = rawr.next()
                            cacc, CACC = caccr.next()
                            inproj(L, hT, HT, wring, pin, g,
                                   lambda ps, PS, s, n, raw=raw, RAW=RAW: cp("act", raw[:, s:s + n], ps[:, 0:n], [PS], [RAW]))
                            cg = {"q": 0, "k": 1, "v": 2}[kind] * 4 + h
                            conv("dve", raw, RAW, cacc, CACC,
                                 lambda tap, cg=cg: convw[:, l, cg, tap:tap + 1])
                            gi += 1
                            if kind == "v":
                                act(VT[:, hh, :], cacc[:], AF.Silu, [CACC], [VTD])
                                continue
                            act(sq32[:], cacc[:], AF.Silu, [CACC], [SQ32])
                            tt("pool", sqb[:], sq32[:], sq32[:], ALU.mult, [SQ32], [SQB])
                            for (s, n) in TTILES:
                                ps, PS = pss.next()
                                mm(ps[:, 0:n], onb[:], sqb[:, s:s + n], True, True, [ONB, SQB], [PS])
                                act(rt[:, s:s + n], ps[:, 0:n], AF.Sqrt, [PS, EPSC], [RT], bias=epsc[:, 0:1])
                            S.op("dve", lambda e: e.reciprocal(rt[:], rt[:]), [RT], [RT])
                            if kind == "q":
                                stt("dve", QT[:, hh, :], sq32[:], float(128 ** -0.5), rt[:], ALU.mult, ALU.mult,
                                    [SQ32, RT], [QTD])
                            else:
                                tt("dve", KT[:, hh, :], sq32[:], rt[:], ALU.mult, [SQ32, RT], [KTD])
                    if stop == "G1" and hp == 0 and b == 0 and l == 0:
                        for j, (tt_, TD_) in enumerate(((QT, QTD), (KT, KTD), (VT, VTD), (ZS, ZSD))):
                            for hh_ in range(2):
                                cp("act", yT[:, j * 2 + hh_, :], tt_[:, hh_, :], [TD_], [YT])
                        S.dma("pool", dbg_out["yT"].rearrange("p (k t) -> p k t", k=KD), yT[:], reads=[YT], writes=[Dep("dbg_yT1")])
                    S.barrier()
                    S.build()
                    if chk("G1"):
                        return
                with ExitStack() as ph3:
                    chains = [(hh, d) for hh in range(2) for d in range(2)]
                    pab = [S.ps(f"pa{j}", [128, 512], F32, ph3)[0] for j in range(3)]
                    pabd = [Dep(f"pa{j}", excl=True) for j in range(3)]
                    pbb = S.ps("pb16", [128, 1024], BF16, ph3)[0]
                    pbbd = Dep("pb16", excl=True)
                    psb = [S.ps(f"pst{j}", [128, 512], F32, ph3)[0] for j in range(4)]
                    psbd = [Dep(f"pst{j}", excl=True) for j in range(4)]

                    class Cyc:
                        def __init__(self, items):
                            self.items = items
                            self.i = 0

                        def next(self):
                            it = self.items[self.i % len(self.items)]
                            self.i += 1
                            return it
                    CH = []
                    for c in range(4):
                        o = NS()
                        o.pa = Cyc([(pab[j][:, c * 128:(c + 1) * 128], pabd[j]) for j in range(3)])
                        o.pb16 = Cyc([(pbb[:, (2 * c + j) * 128:(2 * c + j + 1) * 128], pbbd) for j in range(2)])
                        o.pstep = Cyc([(psb[c][:, j * 128:(j + 1) * 128], psbd[c]) for j in (0, 2, 3)])
                        o.pout = Cyc([(psb[c][:, 128:256], psbd[c])])
                        o.Sf, o.SF = S.sb("Sf", [128, 128], F32, ph3)
                        o.Sb, o.SB = S.sb("Sb", [128, 128], BF16, ph3)
                        S.op("dve", lambda e, o=o: e.memset(o.Sf[:], 0.0), (), [o.SF])
                        S.op("dve", lambda e, o=o: e.memset(o.Sb[:], 0.0), (), [o.SB])
                        for nm, dt, n in (("lam", F32, 2), ("ecb", F32, 2), ("dec", F32, 2), ("decs", F32, 4),
                                          ("Qd", BF16, 2), ("qkm", BF16, 2), ("A", F32, 2), ("Bm", F32, 2),
                                          ("Pt", F32, 2), ("Pf", BF16, 2), ("kdec0", BF16, 2), ("kdec1", BF16, 2),
                                          ("vtok", BF16, 2), ("Y0", BF16, 1), ("vnew", BF16, 1)):
                            setattr(o, nm, Ring(S, "sb", nm, [128, 128], dt, n, ph3))
                        for rg in (o.Y0, o.vnew):
                            for (t_, TD_) in rg.items:
                                S.op("pool", lambda e, t_=t_: e.memset(t_[:], 0.0), (), [TD_])
                        CH.append(o)

                    def prep(c, i):
                        o = CH[c]
                        hh, d = chains[c]
                        h = 2 * hp + hh
                        dh = d * 4 + h
                        blk = slice(i * 128, (i + 1) * 128)
                        INC, STR, NEG, GT = dmask(d)
                        lam, LAM = o.lam.next()
                        ts1("pool", lam[:], INC, SC.la[:, i, dh:dh + 1], ALU.mult, [CST, SC.LA], [LAM])
                        pc, PC = o.pa.next()
                        mm(pc, C(C_ONES), lam[:], True, True, [CST, LAM], [PC])
                        pt2, PT2 = o.pb16.next()
                        tr(pt2, KT[:, hh, blk], idb[:], [KTD, IDB], [PT2])
                        pt3, PT3 = o.pb16.next()
                        tr(pt3, VT[:, hh, blk], idb[:], [VTD, IDB], [PT3])
                        yield
                        ecb, ECB = o.ecb.next()
                        act(ecb[:], pc, AF.Exp, [PC], [ECB])
                        dsum, DSUM = o.decs.next()
                        stt("dve", dsum[:], pc, SC.ncum[:, i, dh:dh + 1], NEG, ALU.add, ALU.add, [PC, SC.NCUM, CST], [DSUM])
                        dec, DEC = o.dec.next()
                        act(dec[:], dsum[:], AF.Exp, [DSUM], [DEC])
                        kdec0, KDEC0 = o.kdec0.next()
                        ts1("dve", kdec0[:], pt2, SC.er[:, i, 0, dh:dh + 1], ALU.mult, [PT2, SC.ER], [KDEC0])
                        kdec1, KDEC1 = o.kdec1.next()
                        act(kdec1[:], pt2, AF.Copy, [PT2, SC.ER], [KDEC1], scale=SC.er[:, i, 1, dh:dh + 1])
                        vtok, VTOK = o.vtok.next()
                        cp("act", vtok[:], pt3, [PT3], [VTOK])
                        yield
                        pkk, PKK = o.pa.next()
                        mm(pkk, KT[:, hh, blk], KT[:, hh, blk], True, True, [KTD], [PKK])
                        pqk, PQK = o.pa.next()
                        mm(pqk, KT[:, hh, blk], QT[:, hh, blk], True, True, [KTD, QTD], [PQK])
                        yield
                        Qd, QD = o.Qd.next()
                        tt("pool", Qd[:], QT[:, hh, blk], ecb[:], ALU.mult, [QTD, ECB], [QD])
                        qkm, QKM = o.qkm.next()
                        tt("dve", qkm[:], pqk, dec[:], ALU.mult, [PQK, DEC], [QKM])
                        decs, DECS = o.decs.next()
                        tt("pool", decs[:], dec[:], STR, ALU.mult, [DEC, CST], [DECS])
                        A0, A0D = o.A.next()
                        stt("dve", A0[:], pkk, SC.beta[:, i, dh:dh + 1], decs[:], ALU.mult, ALU.mult,
                            [PKK, SC.BETA, DECS], [A0D])
                        yield
                        pt1, PT1 = o.pa.next()
                        tr(pt1, A0[:], C(C_ID), [A0D, CST], [PT1])
                        P0, P0D = o.Pt.next()
                        tt("pool", P0[:], C(C_ID), A0[:], ALU.subtract, [CST, A0D], [P0D])
                        yield
                        B0, B0D = o.Bm.next()
                        cp("act", B0[:], pt1, [PT1], [B0D])
                        yield
                        Ap, APD, Bp, BPD, Pp, PPD = A0, A0D, B0, B0D, P0, P0D
                        for lev in range(1, 6):
                            if lev < 5:
                                pA, PA_ = o.pa.next()
                                mm(pA, Bp[:], Ap[:], True, True, [BPD, APD], [PA_])
                            pB, PB_ = o.pa.next()
                            mm(pB, Ap[:], Bp[:], True, True, [APD, BPD], [PB_])
                            yield
                            if lev < 5:
                                An, AND_ = o.A.next()
                                cp("act", An[:], pA, [PA_], [AND_])
                            Bn, BND = o.Bm.next()
                            cp("act", Bn[:], pB, [PB_], [BND])
                            yield
                            pP, PP_ = o.pa.next()
                            mm(pP, Bn[:], Pp[:], True, True, [BND, PPD], [PP_])
                            yield
                            Pn, PND = (o.Pf if lev == 5 else o.Pt).next()
                            tt("dve", Pn[:], pP, Pp[:], ALU.add, [PP_, PPD], [PND])
                            yield
                            if lev < 5:
                                Ap, APD = An, AND_
                            Bp, BPD, Pp, PPD = Bn, BND, Pn, PND
                        o.cur = dict(ecb=(ecb, ECB), Qd=(Qd, QD), qkm=(qkm, QKM), Pf=(Pp, PPD), kdec=((kdec0, KDEC0), (kdec1, KDEC1)),
                                     vtok=(vtok, VTOK))

                    def steps(c, i, cur):
                        o = CH[c]
                        hh, d = chains[c]
                        h = 2 * hp + hh
                        dh = d * 4 + h
                        blk = slice(i * 128, (i + 1) * 128)
                        ecb, ECB = cur["ecb"]
                        Qd, QD = cur["Qd"]
                        qkm, QKM = cur["qkm"]
                        Pf, PFD = cur["Pf"]
                        vtok, VTOK = cur["vtok"]
                        po, PO = o.pout.next()
                        Y0, Y0D = o.Y0.next()
                        vnew, VNEW = o.vnew.next()
                        for ch in ((0, 1) if d == 0 else (1, 0)):
                            rows = slice(ch * 64, ch * 64 + 64)
                            pks, PKS = o.pstep.next()
                            mm(pks, KT[:, hh, blk], o.Sb[:], True, True, [KTD, o.SB], [PKS])
                            yield
                            stt("dve", Y0[rows, :], pks[rows, :], SC.ecols[rows, i, dh:dh + 1], vtok[rows, :],
                                ALU.mult, ALU.add, [PKS, SC.ECOLS, VTOK], [Y0D])
                            yield
                            pz, PZ = o.pstep.next()
                            mm(pz, Pf[:, :], Y0[:, :], True, True, [PFD, Y0D], [PZ])
                            yield
                            act(vnew[rows, :], pz[rows, :], AF.Copy, [PZ, SC.BETA], [VNEW], scale=SC.beta[rows, i, dh:dh + 1])
                            yield
                            mm(po[:, rows], o.Sb[:], Qd[:, rows], True, False, [o.SB, QD], [PO])
                            mm(po[:, rows], vnew[:, :], qkm[:, rows], False, True, [VNEW, QKM], [PO])
                            pds, PDS = o.pstep.next()
                            kdec, KDEC = cur["kdec"][ch]
                            mm(pds, kdec[:, :], vnew[:, :], True, True, [KDEC, VNEW], [PDS])
                            yield
                            gc = (ch * 64 + 63) if d == 0 else ch * 64
                            stt("dve", o.Sb[:], o.Sf[:], ecb[:, gc:gc + 1], pds, ALU.mult, ALU.add, [o.SF, ECB, PDS], [o.SB])
                            stt("dve", o.Sf[:], o.Sf[:], ecb[:, gc:gc + 1], pds, ALU.mult, ALU.add, [o.SF, ECB, PDS], [o.SF])
                            yield
                        tt("dve", oacc[:, hh, blk], oacc[:, hh, blk], po, ALU.add, [OACC, PO], [OACC])

                    roundrobin([prep(c, ORDER[chains[c][1]][0]) for c in range(4)])
                    if chk("G2"):
                        return
                    for n in range(NT):
                        curs = [CH[c].cur for c in range(4)]
                        gens = [steps(c, ORDER[chains[c][1]][n], curs[c]) for c in range(4)]
                        if n + 1 < NT:
                            gens += [prep(c, ORDER[chains[c][1]][n + 1]) for c in range(4)]
                        roundrobin(gens)
                        if n == 0 and chk("G3"):
                            return
                    S.barrier()
                    S.build()
                    if chk("G4"):
                        return
                with ExitStack() as ph4:
                    pss = PsRing(S, "pss", 2, 512, F32, ph4)
                    headnorm(ph4, L, oacc, OACC, ZS, ZSD, 1, hp, yT, YT, pss)
                    S.barrier()
                    S.build()

        def hgrn_headpair(L, b, hp, hT, HT, yT, YT):
            l = L.l
            with ExitStack() as ph:
                qs, QS = S.sb("qs", [128, 2, T], BF16, ph)
                VT, VTD = S.sb("VTh", [128, 2, T], BF16, ph)
                ZS, ZSD = S.sb("ZSh", [128, 2, T], BF16, ph)
                oacc, OACC = S.sb("oacch", [128, 2, T], F32, ph)
                S.op("pool", lambda e: e.memset(oacc[:], 0.0), (), [OACC])
                chains = [(hh, d) for hh in range(2) for d in range(2)]
                CH = []
                for c in range(4):
                    o = NS()
                    o.qd, o.QD = S.sb("qd", [128, T], BF16, ph)
                    o.kd, o.KD = S.sb("kd", [128, T], BF16, ph)
                    o.gch, o.GCH = S.sb("gch", [128, T // 32], F32, ph)
                    CH.append(o)
                with ExitStack() as ph2:
                    tA, TA = S.sb("tA", [128, T], F32, ph2)
                    tB, TB = S.sb("tB", [128, T], F32, ph2)
                    tC, TCD = S.sb("tC", [128, T], F32, ph2)
                    rst, RST = S.sb("rst", [128, T], BF16, ph2)
                    S.op("pool", lambda e: e.memset(rst[:], 1.0), (), [RST])
                    S.op("pool", lambda e: e.memset(rst[:].rearrange("p (c k) -> p c k", k=32)[:, :, 0:1], 0.0), (), [RST])
                    wring = Ring(S, "sb", "wbh", [128, KD, 128], BF16, 2, ph2)
                    pin = PsRing(S, "pinh", 4, 512, F32, ph2)
                    for hh in range(2):
                        h = 2 * hp + hh
                        inproj(L, hT, HT, wring, pin, HG_Q // 128 + h,
                               lambda ps, PS, s, n, hh=hh: act(qs[:, hh, s:s + n], ps[:, 0:n], AF.Silu, [PS], [QS]))
                        inproj(L, hT, HT, wring, pin, HG_I // 128 + h,
                               lambda ps, PS, s, n, hh=hh: cp("dve", VT[:, hh, s:s + n], ps[:, 0:n], [PS], [VTD]))
                        inproj(L, hT, HT, wring, pin, HG_G // 128 + h,
                               lambda ps, PS, s, n, hh=hh: act(ZS[:, hh, s:s + n], ps[:, 0:n], AF.Silu, [PS], [ZSD]))
                    for c in range(4):
                        o = CH[c]
                        hh, d = chains[c]
                        h = 2 * hp + hh
                        dh = d * 4 + h
                        inproj(L, hT, HT, wring, pin, (HG_FF if d == 0 else HG_FB) // 128 + h,
                               lambda ps, PS, s, n: act(tA[:, s:s + n], ps[:, 0:n], AF.Sigmoid, [PS], [TA]))
                        ts2("dve", tA[:], tA[:], oml[:, l, dh:dh + 1], lbt[:, l, dh:dh + 1], ALU.mult, ALU.add,
                            [TA, OML, LBT], [TA])
                        act(tB[:], tA[:], AF.Ln, [TA], [TB])
                        ts2("dve", tA[:], tA[:], -1.0, 1.0, ALU.mult, ALU.add, [TA], [TA])
                        S.op("dve", lambda e: e.tensor_tensor_scan(tC[:], rst[:], tB[:], 0.0, ALU.mult, ALU.add),
                             [RST, TB], [TCD])
                        if d == 0:
                            cum, CUM, oth, OTH = tC, TCD, tB, TB
                            gcol = 31
                        else:
                            tt("pool", tB[:], tB[:], tC[:], ALU.subtract, [TB, TCD], [TB])
                            tB3 = tB[:].rearrange("p (c k) -> p c k", k=32)
                            tC3 = tC[:].rearrange("p (c k) -> p c k", k=32)
                            tt("dve", tB3, tB3, tC3[:, :, 31:32].to_broadcast([128, T // 32, 32]), ALU.add,
                               [TB, TCD], [TB])
                            cum, CUM, oth, OTH = tB, TB, tC, TCD
                            gcol = 0
                        act(oth[:], cum[:], AF.Exp, [CUM], [OTH])
                        tt("pool", o.qd[:], qs[:, hh, :], oth[:], ALU.mult, [QS, OTH], [o.QD])
                        cp("dve", o.gch[:], oth[:].rearrange("p (c k) -> p c k", k=32)[:, :, gcol], [OTH], [o.GCH])
                        act(oth[:], cum[:], AF.Exp, [CUM, o.QD, o.GCH], [OTH], scale=-1.0)
                        tt("dve", o.kd[:], tA[:], oth[:], ALU.mult, [TA, OTH], [o.KD])
                    S.barrier()
                    S.build()
                with ExitStack() as ph3:
                    pa = PsRing(S, "pah", 2, 128, F32, ph3)
                    pb16 = PsRing(S, "pb16h", 1, 128, BF16, ph3)
                    psbh = [S.ps(f"psth{j}", [128, 512], F32, ph3)[0] for j in range(4)]
                    psbhd = [Dep(f"psth{j}", excl=True) for j in range(4)]

                    class CycH:
                        def __init__(self, items):
                            self.items = items
                            self.i = 0

                        def next(self):
                            it = self.items[self.i % len(self.items)]
                            self.i += 1
                            return it
                    for c in range(4):
                        o = CH[c]
                        o.pstep = CycH([(psbh[c][:, j * 128:(j + 1) * 128], psbhd[c]) for j in (0, 2, 3)])
                        o.pout = CycH([(psbh[c][:, 128:256], psbhd[c])])
                        o.Sf, o.SF = S.sb("Sfh", [128, 128], F32, ph3)
                        o.Sb, o.SB = S.sb("Sbh", [128, 128], BF16, ph3)
                        o.tS, o.TS = S.sb("tSh", [128, 128], F32, ph3)
                        S.op("dve", lambda e, o=o: e.memset(o.Sf[:], 0.0), (), [o.SF])
                        S.op("dve", lambda e, o=o: e.memset(o.Sb[:], 0.0), (), [o.SB])
                        for nm, dt, n in (("kdtok0", BF16, 2), ("kdtok1", BF16, 2), ("vtok", BF16, 2), ("attm", BF16, 2)):
                            setattr(o, nm, Ring(S, "sb", nm + "h", [128, 128], dt, n, ph3))

                    NBH = T // 64
                    ORDH = {0: list(range(NBH)), 1: [3, 2, 1, 0] + list(range(NBH - 1, 3, -1))}

                    def prep(c, i):
                        o = CH[c]
                        hh, d = chains[c]
                        blk = slice(i * 64, (i + 1) * 64)
                        INC = (cst[0:64, C_INCF32 * 128:C_INCF32 * 128 + 64] if d == 0 else
                               cst[0:64, C_INCB32 * 128:C_INCB32 * 128 + 64])
                        pt1, PT1 = pb16.next()
                        tr(pt1[0:64, :], o.kd[:, blk], idb[:], [o.KD, IDB], [PT1])
                        pt2, PT2 = pb16.next()
                        tr(pt2[0:64, :], VT[:, hh, blk], idb[:], [VTD, IDB], [PT2])
                        pat, PAT = pa.next()
                        mm(pat[0:64, 0:64], o.kd[:, blk], o.qd[:, blk], True, True, [o.KD, o.QD], [PAT])
                        yield
                        kdtok0, KDTOK0 = o.kdtok0.next()
                        act(kdtok0[0:64, :], pt1[0:64, :], AF.Copy, [PT1, CST], [KDTOK0],
                            scale=cst[0:64, C_INCF32 * 128 + 31:C_INCF32 * 128 + 32])
                        kdtok1, KDTOK1 = o.kdtok1.next()
                        act(kdtok1[0:64, :], pt1[0:64, :], AF.Copy, [PT1, CST], [KDTOK1],
                            scale=cst[0:64, C_INCB32 * 128 + 32:C_INCB32 * 128 + 33])
                        vtok, VTOK = o.vtok.next()
                        cp("act", vtok[0:64, :], pt2[0:64, :], [PT2], [VTOK])
                        attm, ATTM = o.attm.next()
                        tt("dve", attm[0:64, 0:64], pat[0:64, 0:64], INC, ALU.mult, [PAT, CST], [ATTM])
                        yield
                        o.cur = dict(kdtok=((kdtok0, KDTOK0), (kdtok1, KDTOK1)), vtok=(vtok, VTOK), attm=(attm, ATTM))

                    def steps(c, i, cur):
                        o = CH[c]
                        hh, d = chains[c]
                        blk0 = i * 64
                        vtok, VTOK = cur["vtok"]
                        attm, ATTM = cur["attm"]
                        po, PO = o.pout.next()
                        for ch in ((0, 1) if d == 0 else (1, 0)):
                            rows = slice(ch * 32, ch * 32 + 32)
                            cols = slice(blk0 + ch * 32, blk0 + ch * 32 + 32)
                            mm(po[:, rows], o.Sb[:], o.qd[:, cols], True, False, [o.SB, o.QD], [PO])
                            mm(po[:, rows], vtok[0:64, :], attm[0:64, rows], False, True, [VTOK, ATTM], [PO])
                            pds, PDS = o.pstep.next()
                            kdtok, KDTOK = cur["kdtok"][ch]
                            mm(pds, kdtok[0:64, :], vtok[0:64, :], True, True, [KDTOK, VTOK], [PDS])
                            yield
                            tt("dve", o.tS[:], o.Sf[:], pds, ALU.add, [o.SF, PDS], [o.TS])
                            yield
                            ci = i * 2 + ch
                            ts1("dve", o.Sb[:], o.tS[:], o.gch[:, ci:ci + 1], ALU.mult, [o.TS, o.GCH], [o.SB])
                            ts1("dve", o.Sf[:], o.tS[:], o.gch[:, ci:ci + 1], ALU.mult, [o.TS, o.GCH], [o.SF])
                            yield
                        tt("dve", oacc[:, hh, blk0:blk0 + 64], oacc[:, hh, blk0:blk0 + 64], po[:, 0:64], ALU.add,
                           [OACC, PO], [OACC])

                    roundrobin([prep(c, ORDH[chains[c][1]][0]) for c in range(4)])
                    for n in range(NBH):
                        curs = [CH[c].cur for c in range(4)]
                        gens = [steps(c, ORDH[chains[c][1]][n], curs[c]) for c in range(4)]
                        if n + 1 < NBH:
                            gens += [prep(c, ORDH[chains[c][1]][n + 1]) for c in range(4)]
                        roundrobin(gens)
                    S.barrier()
                    S.build()
                with ExitStack() as ph4:
                    pss = PsRing(S, "pssh", 2, 512, F32, ph4)
                    headnorm(ph4, L, oacc, OACC, ZS, ZSD, 0, hp, yT, YT, pss)
                    S.barrier()
                    S.build()

        def outproj_phase(L, b, src, SRC, mid, MID, yT, YT, tstart):
            with ExitStack() as ph:
                wo, WO = S.sb("wo", [128, KD, D], BF16, ph)
                S.dma("pool", wo[:], wout_d[L.l].rearrange("(k p) n -> p k n", p=128), writes=[WO])
                pin = PsRing(S, "pino", 4, 512, F32, ph)
                Gx, GX = gbcast(ph, L, b, 0, pin)
                Gc, GC = (None, None) if tstart > 0 else gbcast(ph, L, 2, 0, pin)
                xr = Ring(S, "sb", "xo", [128, D], F32, 3, ph)
                tr_ = Ring(S, "sb", "to", [128, D], F32, 2, ph)
                for i in range(tstart, NT):
                    x, X = xr.next()
                    S.dma("sp", x[:], src[b, i * 128:(i + 1) * 128, :], reads=[SRC], writes=[X])
                    G, GD = (Gc, GC) if i < 2 else (Gx, GX)
                    t_, TD_ = tr_.next()
                    for hf in range(2):
                        ps, PS = pin.next()
                        for k in range(KD):
                            mm(ps[:, :], yT[:, k, i * 128:(i + 1) * 128], wo[:, k, hf * 512:(hf + 1) * 512],
                               k == 0, k == KD - 1, [YT, WO], [PS])
                        tt("dve", t_[:, hf * 512:(hf + 1) * 512], ps[:, :], G[:, hf * 512:(hf + 1) * 512], ALU.mult,
                           [PS, GD], [TD_])
                    tt("pool", x[:], x[:], t_[:], ALU.add, [X, TD_], [X])
                    S.dma("sp", mid[b, i * 128:(i + 1) * 128, :], x[:], reads=[X], writes=[MID])
                S.barrier()
                S.build()

        def moe_phase(L, b, mid, MID, dst, DST, last, tstart):
            l = L.l
            ntm = NT - tstart
            with ExitStack() as bp:
                hT, HT = S.sb("hT2", [128, KD, T], BF16, bp)
                gT, GT_ = S.sb("gT", [16, T], F32, bp)
                norm_phase(L, b, mid, MID, 1, hT, HT, tstart)
                with ExitStack() as ph:
                    lg, LG = S.sb("lg", [128, ntm, 20], F32, ph)
                    pr = PsRing(S, "plg", 2, 32, F32, ph)
                    for ii in range(ntm):
                        i = tstart + ii
                        ps, PS = pr.next()
                        for k in range(KD):
                            mm(ps[:, 0:20], hT[:, k, i * 128:(i + 1) * 128], L.wr[:, k, :], k == 0, False, [HT, L.WR], [PS])
                        mm(ps[:, 0:20], cst[0:1, C_ONES * 128:C_ONES * 128 + 128], L.brow[0:1, :], False, True,
                           [CST, L.BROW], [PS])
                        cp("dve", lg[:, ii, :], ps[:, 0:20], [PS], [LG])

                    def sbt(name, shape):
                        return S.sb(name, shape, F32, ph)
                    gl = lg[:, :, 0:4]
                    el = lg[:, :, 4:20]
                    gmax, GMAX = sbt("gmax", [128, ntm, 1])
                    gmask, GMASK = sbt("gmask", [128, ntm, 4])
                    gex, GEX = sbt("gex", [128, ntm, 4])
                    gw, GW = sbt("gw", [128, ntm, 1])
                    elm, ELM = sbt("elm", [128, ntm, 16])
                    m1, M1 = sbt("m1", [128, ntm, 1])
                    m2, M2 = sbt("m2", [128, ntm, 1])
                    mk1, MK1 = sbt("mk1", [128, ntm, 16])
                    mk2, MK2 = sbt("mk2", [128, ntm, 16])
                    w1, W1 = sbt("w1", [128, ntm, 1])
                    w2, W2 = sbt("w2", [128, ntm, 1])
                    gates, GATES = sbt("gates", [128, ntm, 16])
                    BIG = 1.0e4
                    S.op("dve", lambda e: e.tensor_reduce(gmax[:], gl, AX.X, ALU.max), [LG], [GMAX])
                    tt("dve", gmask[:], gl, gmax[:].to_broadcast([128, ntm, 4]), ALU.is_ge, [LG, GMAX], [GMASK])
                    tt("dve", gex[:], gl, gmax[:].to_broadcast([128, ntm, 4]), ALU.subtract, [LG, GMAX], [GEX])
                    act(gex[:], gex[:], AF.Exp, [GEX], [GEX])
                    S.op("dve", lambda e: e.tensor_reduce(gw[:], gex[:], AX.X, ALU.add), [GEX], [GW])
                    S.op("dve", lambda e: e.reciprocal(gw[:], gw[:]), [GW], [GW])
                    ts2("dve", gmask[:], gmask[:], BIG, -BIG, ALU.mult, ALU.add, [GMASK], [GMASK])
                    tt("dve", elm[:].rearrange("p n (g e) -> p n g e", e=4), el.rearrange("p n (g e) -> p n g e", e=4),
                       gmask[:].unsqueeze(3).to_broadcast([128, ntm, 4, 4]), ALU.add, [LG, GMASK], [ELM])
                    S.op("dve", lambda e: e.tensor_reduce(m1[:], elm[:], AX.X, ALU.max), [ELM], [M1])
                    tt("dve", mk1[:], elm[:], m1[:].to_broadcast([128, ntm, 16]), ALU.is_ge, [ELM, M1], [MK1])
                    stt("dve", elm[:], mk1[:], -BIG, elm[:], ALU.mult, ALU.add, [MK1, ELM], [ELM])
                    S.op("dve", lambda e: e.tensor_reduce(m2[:], elm[:], AX.X, ALU.max), [ELM], [M2])
                    tt("dve", mk2[:], elm[:], m2[:].to_broadcast([128, ntm, 16]), ALU.is_ge, [ELM, M2], [MK2])
                    tt("dve", w2[:], m2[:], m1[:], ALU.subtract, [M1, M2], [W2])
                    act(w2[:], w2[:], AF.Exp, [W2], [W2])
                    ts1("dve", w1[:], w2[:], 1.0, ALU.add, [W2], [W1])
                    S.op("dve", lambda e: e.reciprocal(w1[:], w1[:]), [W1], [W1])
                    tt("dve", w2[:], w2[:], w1[:], ALU.mult, [W1, W2], [W2])
                    tt("dve", w1[:], w1[:], gw[:], ALU.mult, [W1, GW], [W1])
                    tt("dve", w2[:], w2[:], gw[:], ALU.mult, [W2, GW], [W2])
                    tt("dve", mk1[:], mk1[:], w1[:].to_broadcast([128, ntm, 16]), ALU.mult, [MK1, W1], [MK1])
                    tt("dve", mk2[:], mk2[:], w2[:].to_broadcast([128, ntm, 16]), ALU.mult, [MK2, W2], [MK2])
                    tt("dve", gates[:], mk1[:], mk2[:], ALU.add, [MK1, MK2], [GATES])
                    pg = PsRing(S, "pgt", 2, 128, F32, ph)
                    for ii in range(ntm):
                        i = tstart + ii
                        ps, PS = pg.next()
                        tr(ps[0:16, :], gates[:, ii, :], C(C_ID), [GATES, CST], [PS])
                        cp("act", gT[0:16, i * 128:(i + 1) * 128], ps[0:16, :], [PS], [GT_])
                    S.barrier()
                    S.build()
                acc, ACC = S.sb("acc", [128, NT, D], F32, bp)
                tiles = [tt_ for tt_ in TTILES if tt_[0] >= tstart * 128]
                with ExitStack() as ph:
                    wgr = Ring(S, "sb", "wg", [128, KD, FF], BF16, 2, ph)
                    wur = Ring(S, "sb", "wu", [128, KD, FF], BF16, 2, ph)
                    wdr = Ring(S, "sb", "wd", [128, 4, D], BF16, 2, ph)
                    gselr = Ring(S, "sb", "gsel", [16, 512], F32, 2, ph)
                    gbr = Ring(S, "sb", "gbs", [128, 512], F32, 2, ph)
                    sgr = Ring(S, "sb", "sg", [128, 512], F32, 2, ph)
                    t1r = Ring(S, "sb", "t1", [128, 512], F32, 2, ph)
                    atr = Ring(S, "sb", "aT", [128, 4, 512], BF16, 2, ph)
                    pin = PsRing(S, "pinm", 4, 512, F32, ph)
                    pyr = PsRing(S, "pym", 2, 512, F32, ph)
                    pgb = PsRing(S, "pgb", 1, 512, F32, ph)
                    for e_ in range(NE):
                        wg, WG = wgr.next()
                        wu, WU = wur.next()
                        wd, WD = wdr.next()
                        S.dma("pool", wg[:], wg_d[l, e_].rearrange("(k p) f -> p k f", p=128), writes=[WG])
                        S.dma("pool", wu[:], wu_d[l, e_].rearrange("(k p) f -> p k f", p=128), writes=[WU])
                        S.dma("pool", wd[:], wd_d[l, e_].rearrange("(k p) n -> p k n", p=128), writes=[WD])
                        for (s, n) in tiles:
                            gsel, GSEL = gselr.next()
                            ts1("pool", gsel[0:16, 0:n], gT[0:16, s:s + n], cst[0:16, C_ID * 128 + e_:C_ID * 128 + e_ + 1],
                                ALU.mult, [GT_, CST], [GSEL])
                            pb_, PB_ = pgb.next()
                            mm(pb_[:, 0:n], cst[0:16, C_ONES * 128:C_ONES * 128 + 128], gsel[0:16, 0:n], True, True,
                               [CST, GSEL], [PB_])
                            gbs, GBS = gbr.next()
                            cp("act", gbs[:, 0:n], pb_[:, 0:n], [PB_], [GBS])
                            aT, AT = atr.next()
                            for f in range(4):
                                pg_, PG_ = pin.next()
                                for k in range(KD):
                                    mm(pg_[:, 0:n], wg[:, k, f * 128:(f + 1) * 128], hT[:, k, s:s + n], k == 0, k == KD - 1,
                                       [WG, HT], [PG_])
                                pu_, PU_ = pin.next()
                                for k in range(KD):
                                    mm(pu_[:, 0:n], wu[:, k, f * 128:(f + 1) * 128], hT[:, k, s:s + n], k == 0, k == KD - 1,
                                       [WU, HT], [PU_])
                                sg, SG = sgr.next()
                                act(sg[:, 0:n], pg_[:, 0:n], AF.Silu, [PG_], [SG])
                                t1, T1 = t1r.next()
                                tt("dve", t1[:, 0:n], pu_[:, 0:n], sg[:, 0:n], ALU.mult, [PU_, SG], [T1])
                                tt("pool", aT[:, f, 0:n], t1[:, 0:n], gbs[:, 0:n], ALU.mult, [T1, GBS], [AT])
                            for j in range(n // 128):
                                i = s // 128 + j
                                for hf in range(2):
                                    py, PY = pyr.next()
                                    for f in range(4):
                                        mm(py[:, :], aT[:, f, j * 128:(j + 1) * 128], wd[:, f, hf * 512:(hf + 1) * 512],
                                           f == 0, f == 3, [AT, WD], [PY])
                                    a_ = acc[:, i, hf * 512:(hf + 1) * 512]
                                    if e_ == 0:
                                        cp("act", a_, py[:, :], [PY], [ACC])
                                    else:
                                        tt("dve", a_, a_, py[:, :], ALU.add, [ACC, PY], [ACC])
                    S.barrier()
                    S.build()
                with ExitStack() as ph:
                    pin = PsRing(S, "pinr", 2, 512, F32, ph)
                    Gx, GX = gbcast(ph, L, b, 1, pin)
                    Gc, GC = (None, None) if tstart > 0 else gbcast(ph, L, 2, 1, pin)
                    xr = Ring(S, "sb", "xm", [128, D], F32, 3, ph)
                    tr_ = Ring(S, "sb", "tm", [128, D], F32, 2, ph)
                    if last:
                        fnw, FNW = S.sb("fnw", [128, D], F32, ph)
                        S.dma("sp", fnw[:], fnw_d, writes=[FNW])
                        junk, JUNK = S.sb("junkf", [128, D], BF16, ph)
                        str_ = Ring(S, "sb", "stf", [128, 4], F32, 3, ph)
                    for i in range(tstart, NT):
                        x, X = xr.next()
                        S.dma("sp", x[:], mid[b, i * 128:(i + 1) * 128, :], reads=[MID], writes=[X])
                        G, GD = (Gc, GC) if i < 2 else (Gx, GX)
                        t_, TD_ = tr_.next()
                        tt("dve", t_[:], acc[:, i, :], G[:], ALU.mult, [ACC, GD], [TD_])
                        tt("pool", x[:], x[:], t_[:], ALU.add, [X, TD_], [X])
                        if not last:
                            S.dma("sp", dst[b, i * 128:(i + 1) * 128, :], x[:], reads=[X], writes=[DST])
                        else:
                            st, ST = str_.next()
                            act(junk[:], x[:], AF.Square, [X], [JUNK, ST], accum_out=st[:, 0:1])
                            act(st[:, 1:2], st[:, 0:1], AF.Sqrt, [ST, EPSC], [ST], scale=1.0 / D, bias=epsc[:, 0:1])
                            S.op("dve", lambda e, st=st: e.reciprocal(st[:, 2:3], st[:, 1:2]), [ST], [ST])
                            stt("dve", t_[:], x[:], st[:, 2:3], fnw[:], ALU.mult, ALU.mult, [X, ST, FNW], [TD_])
                            S.dma("sp", out_d[b, (i - 2) * 128:(i - 1) * 128, :], t_[:], reads=[TD_], writes=[ROUT[b]])
                    S.barrier()
                    S.build()

        def run_pass(L, b, last, tstart, src, SRC, mid, MID, dst, DST):
            with ExitStack() as bp:
                hT, HT = S.sb("hT", [128, KD, T], BF16, bp)
                yT, YT = S.sb("yT", [128, KD, T], BF16, bp)
                norm_phase(L, b, src, SRC, 0, hT, HT, 0)
                if "hT" in dbg_out and L.l == 0 and b == 0:
                    dd = Dep("dbg_hT")
                    S.dma("pool", dbg_out["hT"].rearrange("p (k t) -> p k t", k=KD), hT[:], reads=[HT], writes=[dd])
                if stop == "A":
                    S.barrier()
                    S.build()
                    return
                with ExitStack() as scs:
                    SC = small_cols(L, b, hT, HT, scs)
                    if chk("SC"):
                        return
                    for hp in range(2):
                        gdn_headpair(L, b, hp, hT, HT, yT, YT, SC)
                        if STOPPED[0]:
                            return
                    S.barrier()
                    S.build()
                for hp in range(2):
                    hgrn_headpair(L, b, hp, hT, HT, yT, YT)
                    if STOPPED[0]:
                        return
                if "yT" in dbg_out and L.l == 0 and b == 0:
                    dd = Dep("dbg_yT")
                    S.dma("pool", dbg_out["yT"].rearrange("p (k t) -> p k t", k=KD), yT[:], reads=[YT], writes=[dd])
                if stop == "Y":
                    S.barrier()
                    S.build()
                    return
                outproj_phase(L, b, src, SRC, mid, MID, yT, YT, tstart)
            if stop == "M":
                return
            moe_phase(L, b, mid, MID, dst, DST, last, tstart)

        RXIN = Dep("rxin")
        RA = [Dep(f"resA{b}") for b in range(NB)]
        RB = [Dep(f"resB{b}") for b in range(NB)]
        ROUT = [Dep(f"rout{b}") for b in range(NB)]

        for l in range(nlayers):
          try:
            last = (l == DEPTH - 1)
            tstart = 2 if last else 0
            with ExitStack() as lay:
                L = NS()
                L.l = l
                L.modT, L.MODT = S.sb("modT", [128, 48, 3], F32, lay)
                L.abc, L.ABC = S.sb("abc", [128, 4, KD, 3], F32, lay)
                L.wr, L.WR = S.sb("wr", [128, KD, 20], BF16, lay)
                L.brow, L.BROW = S.sb("brow", [1, 20], F32, lay)
                S.dma("pool", L.wr[:], wr_d[l], writes=[L.WR])
                S.dma("sp", L.brow[:], br_d[l:l + 1, :], writes=[L.BROW])
                with ExitStack() as ph:
                    wmr = Ring(S, "sb", "wm", [128, KD, 512], F32, 2, ph)
                    bmr = Ring(S, "sb", "bm", [1, 512], F32, 2, ph)
                    pst = PsRing(S, "pmT", 2, 16, F32, ph)
                    for n in range(12):
                        wm, WM = wmr.next()
                        bm, BM = bmr.next()
                        S.dma("sp", wm[:], wmod_d[l].rearrange("(k p) n -> p k n", p=128)[:, :, n * 512:(n + 1) * 512],
                              writes=[WM])
                        S.dma("sp", bm[:], bmod_d[l:l + 1, n * 512:(n + 1) * 512], writes=[BM])
                        pt, PT = pst.next()
                        for sub in range(4):
                            o = pt[:, sub * 3:sub * 3 + 3]
                            for k in range(KD):
                                mm(o, wm[:, k, sub * 128:(sub + 1) * 128], scT[:, k, :], k == 0, False,
                                   [WM, SCT], [PT])
                            mm(o, bm[0:1, sub * 128:(sub + 1) * 128], cst[0:1, C_ONES * 128:C_ONES * 128 + 3],
                               False, True, [BM, CST], [PT])
                        cp("dve", L.modT[:, n * 4:(n + 1) * 4, :], pt[:, 0:12].rearrange("p (a b) -> p a b", b=3),
                           [PT], [L.MODT])
                    for wi, (csc, csh) in enumerate(((8, 0), (32, 24))):
                        ts1("dve", L.abc[:, 2 * wi, :, :], L.modT[:, csc:csc + 8, :], 1.0, ALU.add, [L.MODT], [L.ABC])
                        tt("dve", L.abc[:, 2 * wi, :, :], L.abc[:, 2 * wi, :, :],
                           normw[:, l, wi, :].unsqueeze(2).to_broadcast([128, KD, 3]), ALU.mult,
                           [L.ABC, NORMW], [L.ABC])
                        cp("dve", L.abc[:, 2 * wi + 1, :, :], L.modT[:, csh:csh + 8, :], [L.MODT], [L.ABC])
                    if "abc" in dbg_out and l == 0:
                        S.dma("sp", dbg_out["abc"], L.abc[:].rearrange("p a b c -> p (a b c)"), reads=[L.ABC], writes=[Dep("dbg_abc")])
                        S.dma("sp", dbg_out["modT"], L.modT[:].rearrange("p a b -> p (a b)"), reads=[L.MODT], writes=[Dep("dbg_modT")])
                        S.dma("sp", dbg_out["scT"], scT[:].rearrange("p a b -> p (a b)"), reads=[SCT], writes=[Dep("dbg_scT")])
                    S.barrier()
                    S.build()
                for b in range(nb_run):
                    src, SRC = (xin, RXIN) if l == 0 else (resB, RB[b])
                    run_pass(L, b, last, tstart, src, SRC, resA, RA[b], resB, RB[b])
                    if STOPPED[0]:
                        break
          except _Stop:
            break
          if STOPPED[0]:
            break
        if "res_out" in dbg_out:
            dd = Dep("dbg_res")
            for b in range(nb_run):
                if stop == "M":
                    S.dma("sp", dbg_out["res_out"][b], resA[b], reads=[RA[b]], writes=[dd])
                else:
                    S.dma("sp", dbg_out["res_out"][b], resB[b], reads=[RB[b]], writes=[dd])
        S.barrier()
        S.build()
    return nc


def make_consts():
    c = np.zeros((128, NCONST), np.float32)
    idx = np.arange(128)
    s = idx[:, None]
    t = idx[None, :]
    same = (s // 64) == (t // 64)

    def put(i, m):
        c[:, i * 128:(i + 1) * 128] = m.astype(np.float32)
    put(C_ID, s == t)
    put(C_ONES, np.ones((128, 128)))
    incf = same & (s <= t)
    incb = same & (s >= t)
    put(C_INCF, incf)
    put(C_STRF, same & (s < t))
    put(C_INCB, incb)
    put(C_STRB, same & (s > t))
    put(C_NEGF, np.where(incf, 0.0, -30000.0))
    put(C_NEGB, np.where(incb, 0.0, -30000.0))
    put(C_GTF, s > t)
    put(C_GTB, s < t)
    same32 = (s // 32) == (t // 32)
    put(C_INCF32, same32 & (s <= t))
    put(C_INCB32, same32 & (s >= t))
    return c


def prepare_shared(inp):
    f = np.float32
    sh = {}
    sh["consts"] = make_consts()
    sh["w_mod"] = np.ascontiguousarray(inp["w_mod"], dtype=f)
    sh["b_mod"] = np.ascontiguousarray(inp["b_mod"], dtype=f)
    nw = np.stack([inp["norm1_w"], inp["norm2_w"]], axis=1)
    sh["normw"] = np.ascontiguousarray(nw.reshape(DEPTH, 2, KD, 128).transpose(3, 0, 1, 2).reshape(128, -1), dtype=f)
    lb = inp["hg_lb"].reshape(DEPTH, 2, 4, 128)
    sh["lbh"] = np.ascontiguousarray(lb.transpose(3, 0, 1, 2).reshape(128, -1), dtype=f)
    hn = np.stack([inp["hg_norm_w"], inp["dn_norm_w"]], axis=1)
    sh["headnw"] = np.ascontiguousarray(hn.transpose(2, 0, 1).reshape(128, -1), dtype=f)
    cw = inp["dn_conv_w"].reshape(DEPTH, 9, 12, 128)
    sh["convw"] = np.ascontiguousarray(cw.transpose(3, 0, 2, 1).reshape(128, -1), dtype=f)
    al = np.concatenate([inp["dn_a_log"].reshape(DEPTH, 8), inp["dn_dt_bias"].reshape(DEPTH, 8)], axis=1)
    sh["alog"] = np.ascontiguousarray(np.broadcast_to(al.reshape(1, -1), (128, DEPTH * 16)), dtype=f)
    wrr = np.concatenate([inp["w_group"], inp["w_expert"]], axis=2)
    sh["wr"] = np.ascontiguousarray(wrr.reshape(DEPTH, KD, 128, 20).transpose(0, 2, 1, 3), dtype=f)
    sh["br"] = np.ascontiguousarray(np.concatenate([inp["b_group"], inp["b_expert"]], axis=1), dtype=f)
    sh["fnw"] = np.ascontiguousarray(np.broadcast_to(inp["final_norm_w"].reshape(1, D), (128, D)), dtype=f)
    wi = np.zeros((DEPTH, D, NGRP * 128), f)
    wi[:, :, :IN_COLS] = inp["w_in"]
    sh["win"] = np.ascontiguousarray(wi.reshape(DEPTH, KD, 128, NGRP, 128).transpose(0, 3, 2, 1, 4))
    sh["w_out"] = np.ascontiguousarray(inp["w_out"], dtype=f)
    sh["w_gate"] = np.ascontiguousarray(inp["w_gate"], dtype=f)
    sh["w_up"] = np.ascontiguousarray(inp["w_up"], dtype=f)
    sh["w_down"] = np.ascontiguousarray(inp["w_down"], dtype=f)
    return sh


def prepare_core(inp, core):
    f = np.float32
    b0 = core * NB
    m = {}
    m["xin"] = np.ascontiguousarray(np.concatenate([inp["ctx"][b0:b0 + NB], inp["x"][b0:b0 + NB]], axis=1), dtype=f)
    cv = np.concatenate([inp["c"][b0:b0 + NB], inp["c_ctx"].reshape(1, D)], axis=0)
    m["cvec"] = np.ascontiguousarray(cv.reshape(3, KD, 128).transpose(2, 1, 0), dtype=f)
    return m


_CACHE = {}


def kernel(**inputs):
    inp = {k: np.asarray(v) for k, v in inputs.items()}
    n_cores = 8
    if "nc" not in _CACHE:
        _CACHE["nc"] = build_program(DEPTH)
    nc = _CACHE["nc"]
    shared = prepare_shared(inp)
    in_maps = []
    for core in range(n_cores):
        m = dict(shared)
        m.update(prepare_core(inp, core))
        in_maps.append(m)
    res = run_bass_kernel_spmd(nc, in_maps, core_ids=list(range(n_cores)))
    out = np.concatenate([np.asarray(r["out"], dtype=np.float32) for r in res.results], axis=0)
    return out
```
